# Optimizing a Trainium2 kernel written in Bass

```python
import math
import numpy as np
import jax
import jax.numpy as jnp
from jax import lax

D_MODEL = 1024
BATCH = 8
SEQ = 2048
DEPTH = 4

PLE_DIM = 256
D_FF = 2816
EPS = 1e-6
NEG_INF = -1e30
FORCE_SCORE = 1e4

NSA_HEADS = 8
NSA_KV_GROUPS = 2
HEAD_DIM = 64
NSA_WIDTH = NSA_HEADS * HEAD_DIM
KV_WIDTH = NSA_KV_GROUPS * HEAD_DIM
CMP_BLOCK = 32
CMP_STRIDE = 16
CMP_HIDDEN = 256
SEL_BLOCK = 64
SEL_TOPK = 8
WINDOW = 512
Q_BLOCK = 128

REL_BUCKETS = 32
REL_MAX_DIST = 128

SSM_HEADS = 16
SSM_HEAD_DIM = 64
SSM_INNER = SSM_HEADS * SSM_HEAD_DIM
SSM_GROUPS = 2
SSM_STATE = 128
CONV_WIDTH = 4
SSD_CHUNK = 128
CONV_CH = SSM_INNER + 2 * SSM_GROUPS * SSM_STATE

MIX_WIDTH = NSA_WIDTH + SSM_INNER
IN_SIZES = (NSA_WIDTH, KV_WIDTH, KV_WIDTH, KV_WIDTH, KV_WIDTH, KV_WIDTH, KV_WIDTH,
            3 * NSA_HEADS, SSM_INNER, CONV_CH, SSM_HEADS)
IN_WIDTH = sum(IN_SIZES)

kernel_name = "hybrid_nsa_mamba2_macaron_block"


def rmsnorm(x, g):
    xf = x.astype(jnp.float32)
    y = xf * lax.rsqrt(jnp.mean(xf * xf, axis=-1, keepdims=True) + EPS)
    return (y * g.astype(jnp.float32)).astype(x.dtype)


def swiglu(x, w_in, w_out):
    gate, up = jnp.split(x @ w_in, 2, axis=-1)
    return (jax.nn.silu(gate) * up) @ w_out


def t5_bucket(dist):
    n = jnp.maximum(dist, 0)
    exact = REL_BUCKETS // 2
    nf = jnp.maximum(n, exact).astype(jnp.float32)
    large = exact + (jnp.log(nf / exact) / math.log(REL_MAX_DIST / exact)
                     * (REL_BUCKETS - exact)).astype(jnp.int32)
    return jnp.where(n < exact, n, jnp.minimum(large, REL_BUCKETS - 1))


def masked_softmax(logits, mask, axis=-1):
    return jax.nn.softmax(jnp.where(mask, logits.astype(jnp.float32), NEG_INF), axis=axis)


def compress_blocks(k, pos, w1, b1, w2, b2):
    b, s, g, d = k.shape
    nc = (s - CMP_BLOCK) // CMP_STRIDE + 1
    idx = np.arange(nc)[:, None] * CMP_STRIDE + np.arange(CMP_BLOCK)[None, :]
    blocks = k[:, idx] + pos[None, None, :, None, :]
    flat = blocks.transpose(0, 1, 3, 2, 4).reshape(b, nc, g, CMP_BLOCK * d)
    return jax.nn.silu(flat @ w1 + b1) @ w2 + b2


def nsa_attention(q, kc, vc, ks, vs, kw, vw, gate_logits,
                  cmp_pos, cmp_w1, cmp_b1, cmp_w2, cmp_b2, rel_table):
    b, s, _ = q.shape
    G, R, dh = NSA_KV_GROUPS, NSA_HEADS // NSA_KV_GROUPS, HEAD_DIM
    scale = dh ** -0.5
    q = q.reshape(b, s, G, R, dh)
    kc, vc, ks, vs, kw, vw = (a.reshape(b, s, G, dh) for a in (kc, vc, ks, vs, kw, vw))
    t = jnp.arange(s)
    tab = rel_table.reshape(REL_BUCKETS, G, R)

    k_cmp = compress_blocks(kc, cmp_pos[0], cmp_w1[0], cmp_b1[0], cmp_w2[0], cmp_b2[0])
    v_cmp = compress_blocks(vc, cmp_pos[1], cmp_w1[1], cmp_b1[1], cmp_w2[1], cmp_b2[1])
    nc = k_cmp.shape[1]
    ends = jnp.arange(nc) * CMP_STRIDE + CMP_BLOCK - 1
    dist_c = t[:, None] - ends[None, :]
    bias_c = tab[t5_bucket(dist_c)].transpose(2, 3, 0, 1)
    logits_c = jnp.einsum("bsgrd,bcgd->bgrsc", q, k_cmp) * scale + bias_c
    has_block = (t >= CMP_BLOCK - 1).astype(jnp.float32)[:, None]
    p_cmp = masked_softmax(logits_c, dist_c >= 0) * has_block
    o_cmp = jnp.einsum("bgrsc,bcgd->bsgrd", p_cmp, v_cmp)

    nb = s // SEL_BLOCK
    topk = min(SEL_TOPK, nb)
    c_lo = np.arange(nc) * CMP_STRIDE
    c_hi = c_lo + CMP_BLOCK - 1
    s_lo = np.arange(nb) * SEL_BLOCK
    s_hi = s_lo + SEL_BLOCK - 1
    overlap = jnp.asarray(((c_lo[:, None] <= s_hi[None, :]) &
                           (c_hi[:, None] >= s_lo[None, :])).astype(np.float32))
    importance = jnp.einsum("bgrsc,cn->bgsn", p_cmp, overlap)
    blk = jnp.arange(nb)[None, :]
    cur = (t // SEL_BLOCK)[:, None]
    forced = ((blk == 0) | (blk == cur) | (blk == cur - 1)).astype(jnp.float32)
    score = jnp.where(blk <= cur, importance + FORCE_SCORE * forced, -FORCE_SCORE)
    _, sel_idx = lax.top_k(score, topk)

    nq = s // Q_BLOCK
    ks_blk = ks.reshape(b, nb, SEL_BLOCK, G, dh).transpose(0, 3, 1, 2, 4)
    vs_blk = vs.reshape(b, nb, SEL_BLOCK, G, dh).transpose(0, 3, 1, 2, 4)
    kw_pad = jnp.pad(kw, ((0, 0), (WINDOW, 0), (0, 0), (0, 0)))
    vw_pad = jnp.pad(vw, ((0, 0), (WINDOW, 0), (0, 0), (0, 0)))
    kw_len = Q_BLOCK + WINDOW
    gather = jax.vmap(jax.vmap(lambda blocks, ids: blocks[ids]))
    g_idx = jnp.arange(G)[None, :, None, None, None]
    q_blk = q.reshape(b, nq, Q_BLOCK, G, R, dh).transpose(1, 0, 2, 3, 4, 5)
    idx_blk = sel_idx.reshape(b, G, nq, Q_BLOCK, topk).transpose(2, 0, 1, 3, 4)
    starts = jnp.arange(nq) * Q_BLOCK

    def query_block(args):
        qb, ib, q0 = args
        tq = q0 + jnp.arange(Q_BLOCK)
        k_sel = gather(ks_blk, ib)
        v_sel = gather(vs_blk, ib)
        dist_s = tq[None, None, :, None, None] - (ib[..., None] * SEL_BLOCK + jnp.arange(SEL_BLOCK))
        bias_s = tab[t5_bucket(dist_s), g_idx].transpose(0, 1, 2, 5, 3, 4)
        logits_s = jnp.einsum("bqgrd,bgqkld->bgqrkl", qb, k_sel) * scale + bias_s
        p_s = masked_softmax(logits_s, (dist_s >= 0)[:, :, :, None], axis=(-2, -1))
        o_s = jnp.einsum("bgqrkl,bgqkld->bqgrd", p_s, v_sel)
        k_win = lax.dynamic_slice_in_dim(kw_pad, q0, kw_len, axis=1)
        v_win = lax.dynamic_slice_in_dim(vw_pad, q0, kw_len, axis=1)
        kpos = q0 - WINDOW + jnp.arange(kw_len)
        dist_w = tq[:, None] - kpos[None, :]
        valid_w = (dist_w >= 0) & (dist_w < WINDOW) & (kpos[None, :] >= 0)
        bias_w = tab[t5_bucket(dist_w)].transpose(2, 3, 0, 1)
        logits_w = jnp.einsum("bqgrd,bkgd->bgrqk", qb, k_win) * scale + bias_w
        p_w = masked_softmax(logits_w, valid_w)
        o_w = jnp.einsum("bgrqk,bkgd->bqgrd", p_w, v_win)
        return o_s, o_w

    o_sel, o_win = lax.map(query_block, (q_blk, idx_blk, starts))
    unblock = lambda o: o.transpose(1, 0, 2, 3, 4, 5).reshape(b, s, G, R, dh)
    gates = jax.nn.sigmoid(gate_logits.astype(jnp.float32)).reshape(b, s, 3, G, R, 1)
    o = gates[:, :, 0] * o_cmp + gates[:, :, 1] * unblock(o_sel) + gates[:, :, 2] * unblock(o_win)
    return o.reshape(b, s, NSA_WIDTH).astype(q.dtype)


def causal_depthwise_conv(x, w, bias):
    y = lax.conv_general_dilated(
        x, w[:, None, :].astype(x.dtype), window_strides=(1,),
        padding=[(CONV_WIDTH - 1, 0)], dimension_numbers=("NWC", "WIO", "NWC"),
        feature_group_count=x.shape[-1])
    return y + bias


def ssd_scan(x, dt, a, bm, cm, d_skip):
    b, s, h, pd = x.shape
    g, n = bm.shape[2], bm.shape[3]
    r = h // g
    L = SSD_CHUNK
    c = s // L
    xf = x.astype(jnp.float32)
    xg = xf.reshape(b, c, L, g, r, pd)
    xdt = (xf * dt[..., None]).reshape(b, c, L, g, r, pd)
    bc = bm.astype(jnp.float32).reshape(b, c, L, g, n)
    cc = cm.astype(jnp.float32).reshape(b, c, L, g, n)
    da = (dt * a).reshape(b, c, L, g, r).transpose(0, 3, 4, 1, 2)
    cs = jnp.cumsum(da, axis=-1)
    causal = jnp.tril(jnp.ones((L, L), dtype=bool))
    decay_in = jnp.exp(jnp.where(causal, cs[..., :, None] - cs[..., None, :], NEG_INF))
    y_diag = jnp.einsum("bclgn,bcsgn,bgrcls,bcsgrp->bclgrp", cc, bc, decay_in, xdt)
    decay_to_end = jnp.exp(cs[..., -1:] - cs)
    chunk_states = jnp.einsum("bcsgn,bgrcs,bcsgrp->bcgrpn", bc, decay_to_end, xdt)
    chunk_decay = jnp.exp(cs[..., -1])

    def carry_state(h_prev, inp):
        st, dec = inp
        return h_prev * dec[..., None, None] + st, h_prev

    _, h_in = lax.scan(carry_state, jnp.zeros_like(chunk_states[:, 0]),
                       (chunk_states.transpose(1, 0, 2, 3, 4, 5), chunk_decay.transpose(3, 0, 1, 2)))
    h_in = h_in.transpose(1, 0, 2, 3, 4, 5)
    y_off = jnp.einsum("bclgn,bcgrpn,bgrcl->bclgrp", cc, h_in, jnp.exp(cs))
    y = y_diag + y_off + xg * d_skip.astype(jnp.float32).reshape(g, r, 1)
    return y.reshape(b, s, h * pd)


def mamba2_mixer(z, xbc, dt_raw, conv_w, conv_b, dt_bias, a_log, d_skip, norm_g):
    b, s, _ = z.shape
    xbc = jax.nn.silu(causal_depthwise_conv(xbc, conv_w, conv_b))
    bcw = SSM_GROUPS * SSM_STATE
    xs, bm, cm = jnp.split(xbc, [SSM_INNER, SSM_INNER + bcw], axis=-1)
    dt = jax.nn.softplus((dt_raw + dt_bias).astype(jnp.float32))
    a = -jnp.exp(a_log.astype(jnp.float32))
    y = ssd_scan(xs.reshape(b, s, SSM_HEADS, SSM_HEAD_DIM), dt, a,
                 bm.reshape(b, s, SSM_GROUPS, SSM_STATE), cm.reshape(b, s, SSM_GROUPS, SSM_STATE), d_skip)
    y = (y * jax.nn.silu(z.astype(jnp.float32))).reshape(b, s, SSM_GROUPS, SSM_INNER // SSM_GROUPS)
    return rmsnorm(y, norm_g.reshape(SSM_GROUPS, -1)).reshape(b, s, SSM_INNER).astype(z.dtype)


def setup_inputs(seed: int = 0) -> dict:
    key = jax.random.key(seed)
    keys = iter(list(jax.random.split(key, 40)))
    f32 = jnp.float32

    def normal(shape, scale):
        return jax.random.normal(next(keys), shape, f32) * scale

    def gain(shape):
        return 1.0 + normal(shape, 0.01)

    L = DEPTH
    dt0 = jnp.exp(jax.random.uniform(next(keys), (L, SSM_HEADS), f32, math.log(1e-3), math.log(1e-1)))
    return {
        "x": normal((BATCH, SEQ, D_MODEL), 1.0),
        "p": normal((DEPTH, BATCH, SEQ, PLE_DIM), 1.0),
        "ffn1_norm": gain((L, D_MODEL)),
        "ffn1_w_in": normal((L, D_MODEL, 2 * D_FF), D_MODEL ** -0.5),
        "ffn1_w_out": normal((L, D_FF, D_MODEL), D_FF ** -0.5),
        "mix_norm": gain((L, D_MODEL)),
        "w_mix_in": normal((L, D_MODEL, IN_WIDTH), D_MODEL ** -0.5),
        "cmp_pos": normal((L, 2, CMP_BLOCK, HEAD_DIM), 0.1),
        "cmp_w1": normal((L, 2, CMP_BLOCK * HEAD_DIM, CMP_HIDDEN), (CMP_BLOCK * HEAD_DIM) ** -0.5),
        "cmp_b1": normal((L, 2, CMP_HIDDEN), 0.02),
        "cmp_w2": normal((L, 2, CMP_HIDDEN, HEAD_DIM), CMP_HIDDEN ** -0.5),
        "cmp_b2": normal((L, 2, HEAD_DIM), 0.02),
        "rel_table": normal((REL_BUCKETS, NSA_HEADS), 0.5),
        "nsa_out_norm": gain((L, NSA_WIDTH)),
        "conv_w": normal((L, CONV_WIDTH, CONV_CH), CONV_WIDTH ** -0.5),
        "conv_b": normal((L, CONV_CH), 0.02),
        "dt_bias": dt0 + jnp.log(-jnp.expm1(-dt0)),
        "a_log": jnp.log(jax.random.uniform(next(keys), (L, SSM_HEADS), f32, 1.0, 16.0)),
        "d_skip": 1.0 + normal((L, SSM_HEADS), 0.1),
        "ssm_out_norm": gain((L, SSM_INNER)),
        "w_mix_out": normal((L, MIX_WIDTH, D_MODEL), MIX_WIDTH ** -0.5),
        "ffn2_norm": gain((L, D_MODEL)),
        "ffn2_w_in": normal((L, D_MODEL, 2 * D_FF), D_MODEL ** -0.5),
        "ffn2_w_out": normal((L, D_FF, D_MODEL), D_FF ** -0.5),
        "ple_norm": gain((L, D_MODEL)),
        "ple_gate_w": normal((L, D_MODEL, D_MODEL), D_MODEL ** -0.5),
        "ple_proj_w": normal((L, PLE_DIM, D_MODEL), PLE_DIM ** -0.5),
        "final_norm": gain((D_MODEL,)),
    }


def reference(x, p, ffn1_norm, ffn1_w_in, ffn1_w_out, mix_norm, w_mix_in,
              cmp_pos, cmp_w1, cmp_b1, cmp_w2, cmp_b2, rel_table, nsa_out_norm,
              conv_w, conv_b, dt_bias, a_log, d_skip, ssm_out_norm, w_mix_out,
              ffn2_norm, ffn2_w_in, ffn2_w_out, ple_norm, ple_gate_w, ple_proj_w, final_norm):
    offsets = [int(v) for v in np.cumsum(IN_SIZES)[:-1]]
    for i in range(DEPTH):
        x = x + 0.5 * swiglu(rmsnorm(x, ffn1_norm[i]), ffn1_w_in[i], ffn1_w_out[i])
        u = rmsnorm(x, mix_norm[i])
        (q, kc, vc, ks, vs, kw, vw, gate_logits, z, xbc, dt_raw) = jnp.split(
            u @ w_mix_in[i], offsets, axis=-1)
        o_attn = nsa_attention(q, kc, vc, ks, vs, kw, vw, gate_logits,
                               cmp_pos[i], cmp_w1[i], cmp_b1[i], cmp_w2[i], cmp_b2[i], rel_table)
        o_ssm = mamba2_mixer(z, xbc, dt_raw, conv_w[i], conv_b[i], dt_bias[i], a_log[i],
                             d_skip[i], ssm_out_norm[i])
        mixed = jnp.concatenate([rmsnorm(o_attn, nsa_out_norm[i]), o_ssm], axis=-1)
        x = x + mixed @ w_mix_out[i]
        x = x + 0.5 * swiglu(rmsnorm(x, ffn2_norm[i]), ffn2_w_in[i], ffn2_w_out[i])
        gate = jax.nn.sigmoid(rmsnorm(x, ple_norm[i]) @ ple_gate_w[i])
        x = x + gate * (p[i] @ ple_proj_w[i])
    return rmsnorm(x, final_norm)
```

```python
import math
from contextlib import ExitStack
import numpy as np
import concourse.bass as bass
import concourse.mybir as mybir
from concourse.bass_utils import run_bass_kernel_spmd

F32 = mybir.dt.float32
BF16 = mybir.dt.bfloat16
AF = mybir.ActivationFunctionType
ALU = mybir.AluOpType
AX = mybir.AxisListType

ENGS = ("pe", "act", "dve", "pool", "sp")

D_MODEL = 1024
SEQ = 2048
DEPTH = 4
D_FF = 2816
NJ = D_FF // 128
EPS = 1e-6


def I(name, *args, **kw):
    return lambda e: getattr(e, name)(*args, **kw)


class Buf:
    __slots__ = ("name", "w", "r")

    def __init__(self, name=""):
        self.name = name
        self.w = None
        self.r = []


class Prog:
    def __init__(self, nc, n_dma_sems=48):
        self.nc = nc
        self.ops = {e: [] for e in ENGS}
        self.waited_eng = {e: {} for e in ENGS}
        self.waited_dma = {e: {} for e in ENGS}
        self.n_dma_sems = n_dma_sems
        self.dma_val = [0] * n_dma_sems
        self.dma_next = {"pool": 0, "sp": 0}
        self.half = n_dma_sems // 2

    def _need(self, eng, tok, waits):
        if tok is None:
            return
        if tok[0] == "eng":
            _, src, idx = tok
            if src == eng:
                return
            cur = self.waited_eng[eng].get(src, -1)
            if idx <= cur:
                return
            self.waited_eng[eng][src] = idx
            self.ops[src][idx]["sig"] = True
            waits.append(tok)
        else:
            _, s, val = tok
            cur = self.waited_dma[eng].get(s, 0)
            if val <= cur:
                return
            self.waited_dma[eng][s] = val
            waits.append(tok)

    def _need_same(self, eng, tok, waits):
        _, src, idx = tok
        cur = self.waited_eng[eng].get("self", -1)
        if idx <= cur:
            return
        self.waited_eng[eng]["self"] = idx
        self.ops[src][idx]["sig"] = True
        waits.append(tok)

    def _deps(self, eng, reads, writes, same_raw):
        waits = []
        for b in reads:
            if b.w is not None:
                if b.w[0] == "eng" and b.w[1] == eng:
                    if same_raw:
                        self._need_same(eng, b.w, waits)
                else:
                    self._need(eng, b.w, waits)
        for b in writes:
            if b.w is not None:
                if b.w[0] == "eng" and b.w[1] == eng:
                    if same_raw:
                        self._need_same(eng, b.w, waits)
                else:
                    self._need(eng, b.w, waits)
            for t in b.r:
                self._need(eng, t, waits)
        return waits

    def _record(self, tok, reads, writes):
        for b in writes:
            b.w = tok
            b.r = []
        for b in reads:
            if b in writes:
                continue
            b.r = [t for t in b.r if not (t[0] == tok[0] and t[1] == tok[1])]
            b.r.append(tok)

    def op(self, eng, fn, reads=(), writes=()):
        reads = [b for b in reads if b is not None]
        writes = [b for b in writes if b is not None]
        waits = self._deps(eng, reads, writes, same_raw=(eng in ("act", "dve", "pool")))
        idx = len(self.ops[eng])
        self.ops[eng].append({"fn": fn, "waits": waits, "sig": False, "dma": None})
        tok = ("eng", eng, idx)
        self._record(tok, reads, writes)
        return tok

    def dma(self, q, out_ap, in_ap, reads=(), writes=(), **kw):
        reads = [b for b in reads if b is not None]
        writes = [b for b in writes if b is not None]
        waits = self._deps(q, reads, writes, same_raw=False)
        for b in reads:
            if b.w is not None and b.w[0] == "eng" and b.w[1] == q:
                self._need_same(q, b.w, waits)
        for b in writes:
            for t in ([b.w] if b.w is not None else []) + b.r:
                if t[0] == "eng" and t[1] == q:
                    self._need_same(q, t, waits)
        s = self.dma_next[q] + (0 if q == "pool" else self.half)
        self.dma_next[q] = (self.dma_next[q] + 1) % self.half
        if self.dma_val[s] > 0:
            self._need(q, ("dma", s, self.dma_val[s]), waits)
        self.dma_val[s] += 16
        tok = ("dma", s, self.dma_val[s])

        def fn(e, out_ap=out_ap, in_ap=in_ap, kw=kw):
            return e.dma_start(out=out_ap, in_=in_ap, **kw)
        self.ops[q].append({"fn": fn, "waits": waits, "sig": False, "dma": s})
        self._record(tok, reads, writes)
        return tok

    def barrier(self):
        toks = []
        for e in ENGS:
            for i in range(len(self.ops[e]) - 1, -1, -1):
                if self.ops[e][i]["fn"] is not None and self.ops[e][i]["dma"] is None:
                    toks.append(("eng", e, i))
                    break
        for s in range(self.n_dma_sems):
            if self.dma_val[s] > 0:
                toks.append(("dma", s, self.dma_val[s]))
        for e in ENGS:
            waits = []
            for t in toks:
                self._need(e, t, waits)
            if waits:
                self.ops[e].append({"fn": None, "waits": waits, "sig": False, "dma": None})

    def emit(self):
        nc = self.nc
        self.barrier()
        with ExitStack() as st:
            sems = {e: st.enter_context(nc.semaphore("s_" + e)) for e in ENGS}
            dsems = [st.enter_context(nc.semaphore("d_%d" % i)) for i in range(self.n_dma_sems)]
            cum = {}
            for e in ENGS:
                c = 0
                arr = []
                for o in self.ops[e]:
                    if o["sig"]:
                        c += 1
                    arr.append(c)
                cum[e] = arr
            block = st.enter_context(nc.Block())

            def make(e):
                def body(eng):
                    for o in self.ops[e]:
                        for t in o["waits"]:
                            if t[0] == "eng":
                                eng.wait_ge(sems[t[1]], cum[t[1]][t[2]])
                            else:
                                eng.wait_ge(dsems[t[1]], t[2])
                        if o["fn"] is None:
                            continue
                        inst = o["fn"](eng)
                        if o["dma"] is not None:
                            inst.then_inc(dsems[o["dma"]], 16)
                        elif o["sig"]:
                            inst.then_inc(sems[e], 1)
                return body
            block.tensor(make("pe"))
            block.scalar(make("act"))
            block.vector(make("dve"))
            block.gpsimd(make("pool"))
            block.sync(make("sp"))
        return nc


class Ctx:
    dbg = False
    nsa_stop = 99

    def dump(self, P, name, ap, reads):
        if not self.dbg:
            return
        shp = list(ap.shape)
        t = self.nc.dram_tensor("dbg_" + name, shp, F32, kind="ExternalOutput").ap()
        P.dma("pool", t, ap, reads=reads)


ARENA_WORDS = 53000
OFF_XT = 0
OFF_XN = 16384
OFF_CONST = 24576
OFF_PH = 27648


def fview(C, off, n):
    return C.arena[:, off:off + n]


def bview(C, off, nwords):
    return C.arena[:, off:off + nwords].bitcast(BF16)


def rmsnorm_T(P, C, gidx):
    ph = OFF_PH + 18432
    sq = bview(C, ph, 2048).rearrange("p (c t) -> p c t", c=8)
    rstd = fview(C, ph + 2048, 2048)
    for tt in range(4):
        ts = slice(tt * 512, (tt + 1) * 512)
        P.op("act", I("activation", out=sq, in_=C.xT[:, :, ts], func=AF.Square),
             reads=[C.b_xT[tt]], writes=[C.b_sq])
        for c in range(8):
            P.op("pe", I("matmul", C.ps[:, tt, :], lhsT=C.ones_bf, rhs=sq[:, c, :],
                                                      start=(c == 0), stop=(c == 7)),
                 reads=[C.b_sq], writes=[C.b_ps[tt]])
    psall = C.ps[:, 0:4, :].rearrange("p a t -> p (a t)")
    P.op("act", I("activation", out=rstd, in_=psall, func=AF.Ln, bias=C.eps1024[:, 0:1], scale=1.0),
         reads=C.b_ps[0:4], writes=[C.b_rstd])
    P.op("act", I("activation", out=rstd, in_=rstd, func=AF.Exp, scale=-0.5),
         reads=[C.b_rstd], writes=[C.b_rstd])
    for tt in range(4):
        ts = slice(tt * 512, (tt + 1) * 512)
        for c in range(8):
            P.op("dve", I("scalar_tensor_tensor",
                out=C.xnT[:, c, ts], in0=C.xT[:, c, ts], scalar=C.gains[:, gidx + c:gidx + c + 1],
                in1=rstd[:, ts], op0=ALU.mult, op1=ALU.mult),
                reads=[C.b_xT[tt], C.b_rstd], writes=[C.b_xn[tt]])


def ffn_stage(P, C, win_d, wout_d, l, gidx):
    rmsnorm_T(P, C, gidx)
    ph = OFF_PH
    hT = bview(C, ph, 11264).rearrange("p (j t) -> p j t", j=NJ)
    win = [bview(C, ph + 11264 + i * 1024, 1024).rearrange("p (g k m) -> p g k m", g=2, k=8) for i in range(3)]
    wout = [bview(C, ph + 11264 + 3072 + i * 1408, 1408).rearrange("p (j m) -> p j m", j=NJ) for i in range(2)]
    sg = [fview(C, ph + 11264 + 3072 + 2816 + i * 512, 512) for i in range(2)]
    b_win = [Buf("win%d" % i) for i in range(3)]
    b_wout = [Buf("wout%d" % i) for i in range(2)]
    b_sg = [Buf("sg%d" % i) for i in range(2)]
    b_h = [Buf("h%d" % j) for j in range(NJ)]
    step = 0
    for hf in range(2):
        for j in range(NJ):
            s = (hf * NJ + j) % 3
            P.dma("pool", win[s].rearrange("p g k m -> p (g k m)"), win_d[l, j], writes=[b_win[s]])
            for t2 in range(2):
                tt = hf * 2 + t2
                ts = slice(tt * 512, (tt + 1) * 512)
                pg, pu = step % 2, 2 + step % 2
                for g, pb in ((0, pg), (1, pu)):
                    for kc in range(8):
                        P.op("pe", I("matmul",
                            C.ps[:, pb, :], lhsT=win[s][:, g, kc, :], rhs=C.xnT[:, kc, ts],
                            start=(kc == 0), stop=(kc == 7)),
                            reads=[b_win[s], C.b_xn[tt]], writes=[C.b_ps[pb]])
                k = step % 2
                P.op("act", I("activation", out=sg[k], in_=C.ps[:, pg, :], func=AF.Silu),
                     reads=[C.b_ps[pg]], writes=[b_sg[k]])
                P.op("dve", I("tensor_tensor",
                    out=hT[:, j, t2 * 512:(t2 + 1) * 512], in0=sg[k], in1=C.ps[:, pu, :], op=ALU.mult),
                    reads=[b_sg[k], C.b_ps[pu]], writes=[b_h[j]])
                step += 1
        for m in range(8):
            s = (hf * 8 + m) % 2
            P.dma("pool", wout[s][:, 0:11, :].rearrange("p j m -> p (j m)"), wout_d[l, m][:, 0:1408],
                  writes=[b_wout[s]])
            P.dma("pool", wout[s][:, 11:22, :].rearrange("p j m -> p (j m)"), wout_d[l, m][:, 1408:2816],
                  writes=[b_wout[s]])
            for t2 in range(2):
                tt = hf * 2 + t2
                ts = slice(tt * 512, (tt + 1) * 512)
                pb = 4 + step % 2
                for j in range(NJ):
                    P.op("pe", I("matmul",
                        C.ps[:, pb, :], lhsT=wout[s][:, j, :], rhs=hT[:, j, t2 * 512:(t2 + 1) * 512],
                        start=(j == 0), stop=(j == NJ - 1)),
                        reads=[b_wout[s], b_h[j]], writes=[C.b_ps[pb]])
                P.op("dve", I("scalar_tensor_tensor",
                    out=C.xT[:, m, ts], in0=C.ps[:, pb, :], scalar=0.5, in1=C.xT[:, m, ts],
                    op0=ALU.mult, op1=ALU.add),
                    reads=[C.b_ps[pb], C.b_xT[tt]], writes=[C.b_xT[tt]])
                step += 1
    P.barrier()


class Alloc:
    def __init__(self, C, base=None):
        self.C = C
        self.o = OFF_PH if base is None else base

    def f(self, n):
        r = fview(self.C, self.o, n)
        self.o += n
        assert self.o <= ARENA_WORDS, self.o
        return r

    def b(self, nwords):
        r = bview(self.C, self.o, nwords)
        self.o += nwords
        assert self.o <= ARENA_WORDS, self.o
        return r


NFM = 30
FM_Z = 10
FM_XBC = 18
NVEC = 12 * 4 + 12 + 8 + 4 + 4 + 1
V_CW, V_CB, V_SG, V_NG, V_B1, V_B2K = 0, 48, 60, 68, 72, 76


def ssd_phase(P, C, d, l):
    A = Alloc(C)
    zT = A.b(2048).rearrange("p (c t) -> p c t", c=8)
    xbcT = A.b(3072).rearrange("p (c t) -> p c t", c=12)
    yT = A.b(2048).rearrange("p (c t) -> p c t", c=8)
    gz = A.b(2048).rearrange("p (c t) -> p c t", c=8)
    wring = [A.b(512).rearrange("p (k m) -> p k m", k=8) for _ in range(3)]
    raw = [A.f(520) for _ in range(2)]
    t1 = [A.f(512) for _ in range(2)]
    rawhist = A.f(40)[:, 0:36].rearrange("p (c j) -> p c j", c=12)
    E = A.f(1024)
    daU = A.f(1024)
    MT = A.b(512).rearrange("p (i l) -> p i l", i=8)
    GU = A.f(256).rearrange("p (g l) -> p g l", g=2)
    xtok = A.b(512)
    xdt = A.b(512)
    xdtdec = A.b(512)
    Btok = A.b(128).rearrange("p (g n) -> p g n", g=2)
    HT = A.f(1024)
    HTbf = A.b(512)
    tmpA = A.f(1024)
    tmpB = A.f(1024)
    xD = A.f(1024)
    ytok = A.b(512)
    sm = A.f(256)
    wdt = A.b(64).rearrange("p (k n) -> p k n", k=8)
    dtr, dt, da, cs_sb, ecs, edte, coef2, etot = [sm[:, i * 16:(i + 1) * 16] for i in range(8)]
    arow = sm[:, 128:144]
    rstd2 = E.rearrange("p (g t) -> p g t", g=2)
    mixT = zT
    sq = yT
    vec = C.vecs[:, l, :]
    rows = C.rows[:, l, :]
    dtb_row, alog_row, D_row = rows[:, 0:16], rows[:, 16:32], rows[:, 32:48]

    b = {n: Buf(n) for n in ("zT xbcT yT gz E daU MT GU xtok xdt xdtdec Btok HT HTbf tmpA tmpB xD ytok "
                             "dtr dt da cs ecs edte coef2 etot arow wdt rawhist mixT rstd2").split()}
    b_w = [Buf("w%d" % i) for i in range(3)]
    b_raw = [Buf("raw%d" % i) for i in range(2)]
    b_t1 = [Buf("t1%d" % i) for i in range(2)]
    bps = C.b_ps
    psb = lambda k: C.ps[:, k, :]
    psbf = lambda k: C.ps[:, k, :].bitcast(BF16)
    wcount = [0]

    def wload(src):
        s = wcount[0] % 3
        wcount[0] += 1
        P.dma("pool", wring[s].rearrange("p k m -> p (k m)"), src, writes=[b_w[s]])
        return s

    P.op("act", I("activation", out=arow, in_=alog_row, func=AF.Exp), reads=[C.b_const], writes=[b["arow"]])
    P.op("dve", I("tensor_scalar", out=arow, in0=arow, scalar1=-1.0, scalar2=None, op0=ALU.mult),
         reads=[b["arow"]], writes=[b["arow"]])
    P.op("pool", I("memset", HT, 0.0), writes=[b["HT"]])
    P.op("pool", I("memset", HTbf, 0.0), writes=[b["HTbf"]])
    P.op("pool", I("memset", rawhist, 0.0), writes=[b["rawhist"]])
    P.dma("pool", wdt.rearrange("p k n -> p (k n)"), d["wdt"][l], writes=[b["wdt"]])

    step = [0]
    for G in range(4):
        gs = slice(G * 512, (G + 1) * 512)
        for c in range(8):
            s = wload(d["wfm"][l, FM_Z + c])
            pb = 6 + step[0] % 2
            step[0] += 1
            for kc in range(8):
                P.op("pe", I("matmul", psb(pb), lhsT=wring[s][:, kc, :], rhs=C.xnT[:, kc, gs],
                                                                start=(kc == 0), stop=(kc == 7)),
                     reads=[b_w[s], C.b_xn[G]], writes=[bps[pb]])
            P.op("act", I("activation", out=zT[:, c, :], in_=psb(pb), func=AF.Silu),
                 reads=[bps[pb]], writes=[b["zT"]])
        for ch in range(12):
            s = wload(d["wfm"][l, FM_XBC + ch])
            pb = 6 + step[0] % 2
            r = step[0] % 2
            step[0] += 1
            for kc in range(8):
                P.op("pe", I("matmul", psb(pb), lhsT=wring[s][:, kc, :], rhs=C.xnT[:, kc, gs],
                                                                start=(kc == 0), stop=(kc == 7)),
                     reads=[b_w[s], C.b_xn[G]], writes=[bps[pb]])
            P.op("pool", I("tensor_copy", out=raw[r][:, 0:3], in_=rawhist[:, ch, :]),
                 reads=[b["rawhist"]], writes=[b_raw[r]])
            P.op("act", I("activation", out=raw[r][:, 3:515], in_=psb(pb), func=AF.Copy),
                 reads=[bps[pb]], writes=[b_raw[r]])
            P.op("pool", I("tensor_copy", out=rawhist[:, ch, :], in_=raw[r][:, 512:515]),
                 reads=[b_raw[r]], writes=[b["rawhist"]])
            cw = lambda j, ch=ch: vec[:, V_CW + ch * 4 + j:V_CW + ch * 4 + j + 1]
            P.op("dve", I("tensor_scalar", out=t1[r], in0=raw[r][:, 0:512], scalar1=cw(0), scalar2=None,
                                                             op0=ALU.mult), reads=[b_raw[r], C.b_const], writes=[b_t1[r]])
            for j in range(1, 4):
                P.op("dve", I("scalar_tensor_tensor",
                    out=t1[r], in0=raw[r][:, j:j + 512], scalar=cw(j), in1=t1[r], op0=ALU.mult, op1=ALU.add),
                    reads=[b_raw[r], b_t1[r]], writes=[b_t1[r]])
            P.op("act", I("activation", out=xbcT[:, ch, :], in_=t1[r], func=AF.Silu,
                                                          bias=vec[:, V_CB + ch:V_CB + ch + 1]),
                 reads=[b_t1[r]], writes=[b["xbcT"]])
        for c4 in range(4):
            ti = G * 4 + c4
            cs_ = slice(c4 * 128, (c4 + 1) * 128)
            tk = slice(ti * 128, (ti + 1) * 128)
            for kc in range(8):
                P.op("pe", I("matmul", C.ps[:, 0, 0:16], lhsT=C.xnT[:, kc, tk], rhs=wdt[:, kc, :],
                                                            start=(kc == 0), stop=(kc == 7)),
                     reads=[C.b_xn[G], b["wdt"]], writes=[bps[0]])
            P.op("dve", I("tensor_tensor", out=dtr, in0=C.ps[:, 0, 0:16], in1=dtb_row, op=ALU.add),
                 reads=[bps[0], C.b_const], writes=[b["dtr"]])
            P.op("act", I("activation", out=dtr, in_=dtr, func=AF.Exp), reads=[b["dtr"]], writes=[b["dtr"]])
            P.op("act", I("activation", out=dt, in_=dtr, func=AF.Ln, bias=C.one_c[:, 0:1], scale=1.0),
                 reads=[b["dtr"]], writes=[b["dt"]])
            P.op("dve", I("tensor_tensor", out=da, in0=dt, in1=arow, op=ALU.mult),
                 reads=[b["dt"], b["arow"]], writes=[b["da"]])
            P.op("pe", I("matmul", C.ps[:, 0, 16:32], lhsT=C.U_f, rhs=da, start=True, stop=True),
                 reads=[b["da"]], writes=[bps[0]])
            P.op("pe", I("matmul", C.ps[:, 0, 32:48], lhsT=C.SL_f, rhs=da, start=True, stop=True),
                 reads=[b["da"]], writes=[bps[0]])
            P.op("pe", I("matmul", C.ps[:, 0, 48:64], lhsT=C.ones_f, rhs=da, start=True, stop=True),
                 reads=[b["da"]], writes=[bps[0]])
            P.op("act", I("activation", out=ecs, in_=C.ps[:, 0, 16:32], func=AF.Exp), reads=[bps[0]], writes=[b["ecs"]])
            P.op("dve", I("tensor_copy", out=cs_sb, in_=C.ps[:, 0, 16:32]), reads=[bps[0]], writes=[b["cs"]])
            P.op("act", I("activation", out=edte, in_=C.ps[:, 0, 32:48], func=AF.Exp), reads=[bps[0]], writes=[b["edte"]])
            P.op("act", I("activation", out=etot, in_=C.ps[:, 0, 48:64], func=AF.Exp), reads=[bps[0]], writes=[b["etot"]])
            P.op("dve", I("tensor_tensor", out=coef2, in0=dt, in1=edte, op=ALU.mult),
                 reads=[b["dt"], b["edte"]], writes=[b["coef2"]])
            if ti == 0:
                C.dump(P, "dt", dt, [b["dt"]])
                C.dump(P, "cs", cs_sb, [b["cs"]])
                C.dump(P, "xbcT", xbcT, [b["xbcT"]])
                C.dump(P, "zT", zT, [b["zT"]])
            for ch in range(8):
                P.op("pe", I("transpose", psbf(1)[:, ch * 128:(ch + 1) * 128], xbcT[:, ch, cs_], C.ident_bf),
                     reads=[b["xbcT"]], writes=[bps[1]])
            P.op("act", I("activation", out=xtok, in_=psbf(1), func=AF.Copy), reads=[bps[1]], writes=[b["xtok"]])
            for g in range(2):
                P.op("pe", I("transpose", psbf(0)[:, 768 + g * 128:768 + (g + 1) * 128], xbcT[:, 8 + g, cs_], C.ident_bf),
                     reads=[b["xbcT"]], writes=[bps[0]])
            P.op("dve", I("tensor_copy", out=Btok.rearrange("p g n -> p (g n)"), in_=psbf(0)[:, 768:1024]),
                 reads=[bps[0]], writes=[b["Btok"]])
            for g in range(2):
                P.op("pe", I("matmul", C.ps[:, 0, 128 + g * 128:128 + (g + 1) * 128], lhsT=xbcT[:, 8 + g, cs_],
                                                            rhs=xbcT[:, 10 + g, cs_], start=True, stop=True),
                     reads=[b["xbcT"]], writes=[bps[0]])
            P.op("dve", I("tensor_tensor", out=GU, in0=C.ps[:, 0, 128:384].rearrange("p (g l) -> p g l", g=2),
                                                  in1=C.U_f.unsqueeze(1).to_broadcast([128, 2, 128]), op=ALU.mult),
                 reads=[bps[0]], writes=[b["GU"]])
            P.op("dve", I("tensor_tensor", out=xdt.rearrange("p (h q) -> p h q", h=16),
                                                  in0=xtok.rearrange("p (h q) -> p h q", h=16),
                                                  in1=dt.unsqueeze(2).to_broadcast([128, 16, 64]), op=ALU.mult),
                 reads=[b["xtok"], b["dt"]], writes=[b["xdt"]])
            P.op("pool", I("tensor_tensor", out=xdtdec.rearrange("p (h q) -> p h q", h=16),
                                                   in0=xtok.rearrange("p (h q) -> p h q", h=16),
                                                   in1=coef2.unsqueeze(2).to_broadcast([128, 16, 64]), op=ALU.mult),
                 reads=[b["xtok"], b["coef2"]], writes=[b["xdtdec"]])
            for g in range(2):
                P.op("pe", I("matmul", psb(6 + g), lhsT=xbcT[:, 10 + g, cs_], rhs=HTbf[:, g * 512:(g + 1) * 512],
                                                            start=True, stop=True),
                     reads=[b["xbcT"], b["HTbf"]], writes=[bps[6 + g]])
            for hh in range(2):
                P.op("dve", I("tensor_tensor",
                    out=daU.rearrange("p (i l) -> p i l", i=8), in0=C.U_f.unsqueeze(1).to_broadcast([128, 8, 128]),
                    in1=da[:, hh * 8:(hh + 1) * 8].unsqueeze(2).to_broadcast([128, 8, 128]), op=ALU.mult),
                    reads=[b["da"]], writes=[b["daU"]])
                for k in range(2):
                    P.op("pe", I("matmul", psb(2 + k), lhsT=C.ones_f, rhs=daU[:, k * 512:(k + 1) * 512],
                                                       start=True, stop=True),
                         reads=[b["daU"]], writes=[bps[2 + k]])
                P.op("dve", I("tensor_tensor",
                    out=E.rearrange("p (i l) -> p i l", i=8),
                    in0=C.ps[:, 2:4, :].rearrange("p a (i l) -> p (a i) l", l=128),
                    in1=cs_sb[:, hh * 8:(hh + 1) * 8].unsqueeze(2).to_broadcast([128, 8, 128]), op=ALU.subtract),
                    reads=[bps[2], bps[3], b["cs"]], writes=[b["E"]])
                P.op("pool", I("tensor_scalar", out=E, in0=E, scalar1=0.0, scalar2=None, op0=ALU.min),
                     reads=[b["E"]], writes=[b["E"]])
                P.op("act", I("activation", out=E, in_=E, func=AF.Exp), reads=[b["E"]], writes=[b["E"]])
                P.op("dve", I("tensor_tensor",
                    out=MT, in0=E.rearrange("p (i l) -> p i l", i=8),
                    in1=GU[:, hh, :].unsqueeze(1).to_broadcast([128, 8, 128]), op=ALU.mult),
                    reads=[b["E"], b["GU"]], writes=[b["MT"]])
                for i in range(8):
                    h = hh * 8 + i
                    P.op("pe", I("matmul", C.ps[:, 4 + hh, i * 64:(i + 1) * 64], lhsT=MT[:, i, :],
                                                                   rhs=xdt[:, h * 64:(h + 1) * 64], start=True, stop=True),
                         reads=[b["MT"], b["xdt"]], writes=[bps[4 + hh]])
            for g in range(2):
                P.op("pe", I("matmul", psb(2 + g), lhsT=Btok[:, g, :], rhs=xdtdec[:, g * 512:(g + 1) * 512],
                                                   start=True, stop=True),
                     reads=[b["Btok"], b["xdtdec"]], writes=[bps[2 + g]])
            P.op("dve", I("tensor_tensor", out=tmpB.rearrange("p (h q) -> p h q", h=16),
                                                  in0=C.ps[:, 6:8, :].rearrange("p a (i q) -> p (a i) q", q=64),
                                                  in1=ecs.unsqueeze(2).to_broadcast([128, 16, 64]), op=ALU.mult),
                 reads=[bps[6], bps[7], b["ecs"]], writes=[b["tmpB"]])
            P.op("dve", I("tensor_tensor", out=tmpB, in0=tmpB, in1=C.ps[:, 4:6, :].rearrange("p a t -> p (a t)"), op=ALU.add),
                 reads=[b["tmpB"], bps[4], bps[5]], writes=[b["tmpB"]])
            P.op("pool", I("tensor_tensor", out=xD.rearrange("p (h q) -> p h q", h=16),
                                                   in0=xtok.rearrange("p (h q) -> p h q", h=16),
                                                   in1=D_row.unsqueeze(2).to_broadcast([128, 16, 64]), op=ALU.mult),
                 reads=[b["xtok"], C.b_const], writes=[b["xD"]])
            P.op("pool", I("tensor_tensor", out=ytok, in0=tmpB, in1=xD, op=ALU.add),
                 reads=[b["tmpB"], b["xD"]], writes=[b["ytok"]])
            if ti == 0:
                C.dump(P, "xtok", xtok, [b["xtok"]])
                C.dump(P, "ytok", ytok, [b["ytok"]])
                C.dump(P, "MT", MT, [b["MT"]])
                C.dump(P, "GU", GU, [b["GU"]])
                C.dump(P, "tmpB", tmpB, [b["tmpB"]])
            P.op("dve", I("tensor_tensor", out=tmpA.rearrange("p (h q) -> p h q", h=16),
                                                  in0=HT.rearrange("p (h q) -> p h q", h=16),
                                                  in1=etot.unsqueeze(2).to_broadcast([128, 16, 64]), op=ALU.mult),
                 reads=[b["HT"], b["etot"]], writes=[b["tmpA"]])
            P.op("dve", I("tensor_tensor", out=HT, in0=tmpA, in1=C.ps[:, 2:4, :].rearrange("p a t -> p (a t)"), op=ALU.add),
                 reads=[b["tmpA"], bps[2], bps[3]], writes=[b["HT"]])
            P.op("pool", I("tensor_copy", out=HTbf, in_=HT), reads=[b["HT"]], writes=[b["HTbf"]])
            for ch in range(8):
                P.op("pe", I("transpose", psbf(1)[:, ch * 128:(ch + 1) * 128], ytok[:, ch * 128:(ch + 1) * 128], C.ident_bf),
                     reads=[b["ytok"]], writes=[bps[1]])
            P.op("act", I("activation", out=yT[:, :, cs_], in_=psbf(1).rearrange("p (c t) -> p c t", c=8), func=AF.Copy),
                 reads=[bps[1]], writes=[b["yT"]])
        P.op("pool", I("tensor_tensor", out=gz, in0=yT, in1=zT, op=ALU.mult), reads=[b["yT"], b["zT"]], writes=[b["gz"]])
        P.op("act", I("activation", out=sq, in_=gz, func=AF.Square), reads=[b["gz"]], writes=[b["yT"]])
        for sg in range(2):
            for k in range(4):
                P.op("pe", I("matmul", psb(2 + sg), lhsT=C.ones_bf, rhs=sq[:, sg * 4 + k, :],
                                                          start=(k == 0), stop=(k == 3)),
                     reads=[b["yT"]], writes=[bps[2 + sg]])
        P.op("act", I("activation", out=E, in_=C.ps[:, 2:4, :].rearrange("p a t -> p (a t)"), func=AF.Ln,
                                           bias=C.eps512[:, 0:1], scale=1.0),
             reads=[bps[2], bps[3]], writes=[b["E"]])
        P.op("act", I("activation", out=E, in_=E, func=AF.Exp, scale=-0.5), reads=[b["E"]], writes=[b["E"]])
        for ch in range(8):
            P.op("dve", I("scalar_tensor_tensor",
                out=mixT[:, ch, :], in0=gz[:, ch, :], scalar=vec[:, V_SG + ch:V_SG + ch + 1], in1=rstd2[:, ch // 4, :],
                op0=ALU.mult, op1=ALU.mult),
                reads=[b["gz"], b["E"], C.b_const], writes=[b["zT"]])
        if G == 0:
            C.dump(P, "mixT", mixT, [b["zT"]])
        for m in range(8):
            s = wload(d["wmo"][l, m][:, 512:1536])
            pb = 4 + m % 2
            for kc in range(8):
                P.op("pe", I("matmul", psb(pb), lhsT=wring[s][:, kc, :], rhs=mixT[:, kc, :],
                                                                start=(kc == 0), stop=(kc == 7)),
                     reads=[b_w[s], b["zT"]], writes=[bps[pb]])
            P.op("dve", I("tensor_tensor", out=C.xT[:, m, gs], in0=psb(pb), in1=C.xT[:, m, gs], op=ALU.add),
                 reads=[bps[pb], C.b_xT[G]], writes=[C.b_xT[G]])
    P.barrier()


NEG = -30000.0
LC = 4064


def t5_bucket_np(n):
    n = np.maximum(n, 0)
    nf = np.maximum(n, 16).astype(np.float32)
    large = 16 + (np.log(nf / np.float32(16)) / np.float32(math.log(128 / 16)) * np.float32(16)).astype(np.int32)
    return np.where(n < 16, n, np.minimum(large, 31))


def nsa_tables(P, C, d):
    A = Alloc(C)
    tab = A.f(8)
    tabx = A.f(1024).rearrange("p (h m) -> p h m", h=8)
    oht = A.f(383 + 255 + LC)
    wrow = A.f(LC)
    bt = {n: Buf(n) for n in "tab tabx oht wrow scrA scrB scrC D".split()}
    P.dma("sp", tab[0:32, :], d["rel_table"], writes=[bt["tab"]])
    P.dma("sp", oht[0:33, :], d["oht"], writes=[bt["oht"]])
    P.dma("sp", C.tab31, d["rel_table"][31:32, :].partition_broadcast(128), writes=[C.b_const])
    P.op("dve", I("tensor_scalar", out=C.ntab31, in0=C.tab31, scalar1=-1.0, scalar2=None, op0=ALU.mult),
         reads=[C.b_const], writes=[C.b_const])
    P.op("dve", I("memset", tabx[32:33, :, :], NEG), writes=[bt["tabx"]])
    P.op("dve", I("tensor_copy", out=tabx[0:32, :, :], in_=tab[0:32, :].unsqueeze(2).to_broadcast([32, 8, 128])),
         reads=[bt["tab"]], writes=[bt["tabx"]])
    for h in range(8):
        for (nm, o0, n, scr, rel) in (("A", 0, 383, d["scrA"], True), ("B", 383, 255, d["scrB"], True), ("C", 638, LC, d["scrC"], False)):
            for c0 in range(0, n, 508):
                cn = min(508, n - c0)
                P.op("pe", I("matmul", C.ps[:, 0, 0:cn], lhsT=tabx[0:33, h, :], rhs=oht[0:33, o0 + c0:o0 + c0 + cn], start=True, stop=True),
                     reads=[bt["tabx"], bt["oht"]], writes=[C.b_ps[0]])
                if rel:
                    P.op("act", I("activation", out=wrow[:, c0:c0 + cn], in_=C.ps[:, 0, 0:cn], func=AF.Exp, bias=C.ntab31[:, h:h + 1], scale=1.0),
                         reads=[C.b_ps[0], C.b_const], writes=[bt["wrow"]])
                else:
                    P.op("act", I("activation", out=wrow[:, c0:c0 + cn], in_=C.ps[:, 0, 0:cn], func=AF.Exp),
                         reads=[C.b_ps[0]], writes=[bt["wrow"]])
            P.dma("sp", scr[h].rearrange("(p n) -> p n", p=128), wrow[:, 0:n], reads=[bt["wrow"]], writes=[bt["scr" + nm]])
        P.dma("pool", C.Dtab[:, h, 0:256], bass.AP(d["scrA"].tensor, h * 128 * 383 + 127, [[382, 128], [1, 256]]),
              reads=[bt["scrA"]], writes=[C.b_const])
        P.dma("pool", C.Dtab[:, h, 256:384], bass.AP(d["scrB"].tensor, h * 128 * 255 + 127, [[254, 128], [1, 128]]),
              reads=[bt["scrB"]], writes=[C.b_const])
    C.b_scrC = bt["scrC"]
    P.barrier()


def nsa_phase(P, C, d, l, stop=99):
    A = Alloc(C)
    ksT = A.b(2048).rearrange("p (g t) -> p g t", g=2)
    kwT = A.b(2048).rearrange("p (g t) -> p g t", g=2)
    vtok = A.b(2080).rearrange("p (t a d) -> p t a d", t=16, a=4)
    Wq = A.b(2048).rearrange("p (r k m) -> p r k m", r=4, k=8)
    Woa = A.b(2048).rearrange("p (o k m) -> p o k m", o=8, k=4)
    Ind = A.b(1024)
    ovl = A.b(16)
    valid = A.f(512).rearrange("p (t n) -> p t n", t=16)
    addm = A.f(512).rearrange("p (t n) -> p t n", t=16)
    kcmpT = A.b(128).rearrange("p (g c) -> p g c", g=2)
    vcmp = A.b(64).rearrange("p (g d) -> p g d", g=2)
    b2v = A.f(64)
    gts = A.f(384).rearrange("p (t n) -> p t n", t=16)
    base_q = A.o
    kcT = A.b(1024)
    vcT = A.b(1024)
    wvt = A.b(1120).rearrange("p (k n) -> p k n", k=8)
    w1 = A.b(4096)
    posT = A.b(16)
    w2k = A.b(128).rearrange("p (j m) -> p j m", j=2)
    w2v = A.b(64).rearrange("p (j m) -> p j m", j=2)
    hid = A.b(128).rearrange("p (j c) -> p j c", j=2)
    btmp = A.f(8)
    wring = [A.b(512).rearrange("p (k m) -> p k m", k=8) for _ in range(3)]
    A2 = Alloc(C, base_q)
    qTt = A2.b(1024).rearrange("p (r t) -> p r t", r=4)
    Ec = [A2.b(256) for _ in range(2)]
    b_Ec = [Buf("Ec0"), Buf("Ec1")]
    e32 = [A2.f(512) for _ in range(2)]
    pn_off = A2.o
    pn = A2.b(2048).rearrange("p (h t) -> p h t", h=8)
    pn_f32 = fview(C, pn_off, 2048).rearrange("p (j t) -> p j t", j=4)
    rZ = A2.f(512)
    negmT = A2.b(512).rearrange("p (g t) -> p g t", g=2)
    pexp = [A2.b(256) for _ in range(3)]
    oacc = A2.f(2048).rearrange("p (j f) -> p j f", j=4)
    on = A2.b(1024).rearrange("p (j f) -> p j f", j=4)
    mixT = A2.b(1024).rearrange("p (k t) -> p k t", k=4)
    sc = A2.f(256).rearrange("p (s n) -> p s n", s=8)
    mx = A2.f(64).rearrange("p (s n) -> p s n", s=8)
    negm = A2.b(128).rearrange("p (s n) -> p s n", s=8)
    coef = A2.f(8)
    ssq = A2.f(8)
    junk = C.arena[:, 0:0]
    vec = C.vecs[:, l, :]
    rows = C.rows[:, l, :]

    b = {n: Buf(n) for n in ("ksT kwT vtok Wq Woa cst kcmpT vcmp b2v gts kcT vcT wvt w1 posT w2 hid btmp qTt Ec pn rZ "
                             "negmT oacc on mixT sc mx negm coef ssq junk").split()}
    b_w = [Buf("w%d" % i) for i in range(3)]
    b_e32 = [Buf("e32%d" % i) for i in range(2)]
    b_pexp = [Buf("pexp%d" % i) for i in range(3)]
    bps = C.b_ps
    psb = lambda k: C.ps[:, k, :]
    psbf = lambda k: C.ps[:, k, :].bitcast(BF16)
    cnt = {"w": 0, "ev": 0, "pe": 0, "sb": 0}

    def wload(src):
        s = cnt["w"] % 3
        cnt["w"] += 1
        P.dma("pool", wring[s].rearrange("p k m -> p (k m)"), src, writes=[b_w[s]])
        return s

    def evac(out, in_, reads, writes, **kw):
        cnt["ev"] += 1
        if cnt["ev"] % 2:
            P.op("act", I("activation", out=out, in_=in_, func=AF.Copy, **kw), reads=reads, writes=writes)
        else:
            if "scale" in kw:
                P.op("dve", I("tensor_scalar", out=out, in0=in_, scalar1=kw["scale"], scalar2=None, op0=ALU.mult), reads=reads, writes=writes)
            else:
                P.op("dve", I("tensor_copy", out=out, in_=in_), reads=reads, writes=writes)

    P.dma("pool", Ind[0:32, :], d["ind"], writes=[b["cst"]])
    P.dma("pool", ovl[0:127, :], d["ovl"], writes=[b["cst"]])
    P.dma("sp", valid.rearrange("p t n -> p (t n)"), d["valid"], writes=[b["cst"]])
    P.dma("sp", addm.rearrange("p t n -> p (t n)"), d["addm"], writes=[b["cst"]])
    P.dma("sp", b2v, d["b2v"][l:l + 1, :].partition_broadcast(128), writes=[b["b2v"]])
    for pr in range(4):
        P.dma("pool", Wq[:, pr, :, :].rearrange("p k m -> p (k m)"), d["wfm"][l, pr], writes=[b["Wq"]])
    for m in range(8):
        P.dma("pool", Woa[:, m, :, :].rearrange("p k m -> p (k m)"), d["wmo"][l, m][:, 0:512], writes=[b["Woa"]])
    P.dma("pool", wvt[:, 0:4, :].rearrange("p k n -> p (k n)"), d["wv"][l][:, 0:1120], writes=[b["wvt"]])
    P.dma("pool", wvt[:, 4:8, :].rearrange("p k n -> p (k n)"), d["wv"][l][:, 1120:2240], writes=[b["wvt"]])
    P.op("pool", I("memset", vtok[:, :, :, 64:65], 1.0), writes=[b["vtok"]])
    for ci, dst, bn in ((4, ksT[:, 0, :], "ksT"), (5, ksT[:, 1, :], "ksT"), (6, kwT[:, 0, :], "kwT"), (7, kwT[:, 1, :], "kwT"),
                        (8, kcT, "kcT"), (9, vcT, "vcT")):
        s = wload(d["wfm"][l, ci])
        for tt in range(4):
            ts = slice(tt * 512, (tt + 1) * 512)
            pb = 6 + cnt["pe"] % 2
            cnt["pe"] += 1
            for kc in range(8):
                P.op("pe", I("matmul", psb(pb), lhsT=wring[s][:, kc, :], rhs=C.xnT[:, kc, ts], start=(kc == 0), stop=(kc == 7)),
                     reads=[b_w[s], C.b_xn[tt]], writes=[bps[pb]])
            evac(dst[:, ts], psb(pb), [bps[pb]], [b[bn]])
    for ti in range(16):
        tk = slice(ti * 128, (ti + 1) * 128)
        pb = 4 + ti % 2
        for kc in range(8):
            P.op("pe", I("matmul", C.ps[:, pb, 0:280], lhsT=C.xnT[:, kc, tk], rhs=wvt[:, kc, :], start=(kc == 0), stop=(kc == 7)),
                 reads=[b["wvt"], C.b_xn[ti // 4]], writes=[bps[pb]])
        evac(vtok[:, ti, :, 0:64], C.ps[:, pb, 0:256].rearrange("p (a d) -> p a d", a=4), [bps[pb]], [b["vtok"]])
        P.op("act", I("activation", out=gts[:, ti, :], in_=C.ps[:, pb, 256:280], func=AF.Sigmoid), reads=[bps[pb]], writes=[b["gts"]])
    if stop <= 1:
        P.barrier()
        return
    P.dma("pool", w2k.rearrange("p j m -> p (j m)"), d["cw2k"][l], writes=[b["w2"]])
    P.dma("pool", w2v.rearrange("p j m -> p (j m)"), d["cw2v"][l], writes=[b["w2"]])
    for x in range(2):
        src = kcT if x == 0 else vcT
        bsrc = b["kcT"] if x == 0 else b["vcT"]
        for half in range(2):
            for i in range(4):
                P.dma("pool", w1[half * 64:(half + 1) * 64, i * 2048:(i + 1) * 2048], d["cw1"][l, x][:, i * 2048:(i + 1) * 2048], writes=[b["w1"]])
            P.dma("pool", posT[half * 64:(half + 1) * 64, :], d["cpos"][l, x], writes=[b["posT"]])
        for g in range(2):
            hs = slice(g * 64, (g + 1) * 64)
            for jc in range(2):
                pb = 6 + cnt["pe"] % 2
                cnt["pe"] += 1
                for ll in range(32):
                    P.op("pe", I("matmul", C.ps[:, pb, 0:127], lhsT=w1[hs, ll * 256 + jc * 128:ll * 256 + (jc + 1) * 128],
                                 rhs=src[hs, ll:ll + 2017:16], start=(ll == 0), stop=(ll == 31)),
                         reads=[b["w1"], bsrc], writes=[bps[pb]])
                for ll in range(32):
                    P.op("pe", I("matmul", C.ps[:, pb, 128:129], lhsT=w1[hs, ll * 256 + jc * 128:ll * 256 + (jc + 1) * 128],
                                 rhs=posT[hs, ll:ll + 1], start=(ll == 0), stop=(ll == 31)),
                         reads=[b["w1"], b["posT"]], writes=[bps[pb]])
                P.op("dve", I("tensor_tensor", out=btmp[:, 0:1], in0=C.ps[:, pb, 128:129], in1=vec[:, V_B1 + x * 2 + jc:V_B1 + x * 2 + jc + 1], op=ALU.add),
                     reads=[bps[pb], C.b_const], writes=[b["btmp"]])
                P.op("act", I("activation", out=hid[:, jc, 0:127], in_=C.ps[:, pb, 0:127], func=AF.Silu, bias=btmp[:, 0:1], scale=1.0),
                     reads=[bps[pb], b["btmp"]], writes=[b["hid"]])
            pb = 4 + g
            if x == 0:
                for jc in range(2):
                    P.op("pe", I("matmul", C.ps[:, pb, 0:127], lhsT=w2k[:, jc, :], rhs=hid[:, jc, 0:127], start=(jc == 0), stop=(jc == 1)),
                         reads=[b["w2"], b["hid"]], writes=[bps[pb]])
                P.op("dve", I("tensor_scalar", out=kcmpT[:, g, 0:127], in0=C.ps[:, pb, 0:127], scalar1=vec[:, V_B2K:V_B2K + 1], scalar2=None, op0=ALU.add),
                     reads=[bps[pb], C.b_const], writes=[b["kcmpT"]])
            else:
                for jc in range(2):
                    P.op("pe", I("matmul", C.ps[0:127, pb, 0:64], lhsT=hid[:, jc, 0:127], rhs=w2v[:, jc, :], start=(jc == 0), stop=(jc == 1)),
                         reads=[b["w2"], b["hid"]], writes=[bps[pb]])
                P.op("dve", I("tensor_tensor", out=vcmp[0:127, g, :], in0=C.ps[0:127, pb, 0:64], in1=b2v[0:127, :], op=ALU.add),
                     reads=[bps[pb], b["b2v"]], writes=[b["vcmp"]])
    P.barrier()

    if stop <= 2:
        return

    def combine(h, pb, j0, j1, gcol, first, qt):
        po = C.ps[:, pb, 0:260].rearrange("p (j d) -> p j d", j=4)
        gsl = gts[:, qt * 4 + j0:qt * 4 + j1 + 1, gcol + h]
        if gcol == 0:
            cf = gsl
            rd = [b["gts"]]
        else:
            P.op("dve", I("reciprocal", out=coef[:, j0:j1 + 1], in_=po[:, j0:j1 + 1, 64]), reads=[bps[pb]], writes=[b["coef"]])
            P.op("dve", I("tensor_tensor", out=coef[:, j0:j1 + 1], in0=coef[:, j0:j1 + 1], in1=gsl, op=ALU.mult),
                 reads=[b["coef"], b["gts"]], writes=[b["coef"]])
            cf = coef[:, j0:j1 + 1]
            rd = [b["coef"]]
        for j in range(j0, j1 + 1):
            dst = oacc[:, j, h * 64:(h + 1) * 64]
            if first:
                P.op("dve", I("tensor_scalar", out=dst, in0=po[:, j, 0:64], scalar1=cf[:, j - j0:j - j0 + 1], scalar2=None, op0=ALU.mult),
                     reads=[bps[pb]] + rd, writes=[b["oacc"]])
            else:
                P.op("dve", I("scalar_tensor_tensor", out=dst, in0=po[:, j, 0:64], scalar=cf[:, j - j0:j - j0 + 1], in1=dst,
                              op0=ALU.mult, op1=ALU.add), reads=[bps[pb], b["oacc"]] + rd, writes=[b["oacc"]])

    for qt in range(4):
        q0 = qt * 512
        qs = slice(q0, q0 + 512)
        for pr in range(4):
            pb = 6 + cnt["pe"] % 2
            cnt["pe"] += 1
            for kc in range(8):
                P.op("pe", I("matmul", psb(pb), lhsT=Wq[:, pr, kc, :], rhs=C.xnT[:, kc, qs], start=(kc == 0), stop=(kc == 7)),
                     reads=[b["Wq"], C.b_xn[qt]], writes=[bps[pb]])
            evac(qTt[:, pr, :], psb(pb), [bps[pb]], [b["qTt"]], scale=0.125)
        for h in range(8):
            pr, half, g = h // 2, h % 2, h // 4
            hs = slice(half * 64, (half + 1) * 64)
            sb = cnt["sb"] % 2
            cnt["sb"] += 1
            P.dma("pool", Ec[h % 2][:, :], bass.AP(d["scrC"].tensor, h * 128 * LC + q0 + 2016, [[LC - 16, 128], [1, 512]]),
                  reads=[C.b_scrC], writes=[b_Ec[h % 2]])
            P.op("pe", I("matmul", C.ps[0:127, sb, :], lhsT=kcmpT[hs, g, 0:127], rhs=qTt[hs, pr, :], start=True, stop=True),
                 reads=[b["kcmpT"], b["qTt"]], writes=[bps[sb]])
            P.op("act", I("activation", out=e32[sb][0:127, :], in_=C.ps[0:127, sb, :], func=AF.Exp), reads=[bps[sb]], writes=[b_e32[sb]])
            P.op("dve", I("tensor_tensor", out=pn[0:127, h, :], in0=e32[sb][0:127, :], in1=Ec[h % 2][0:127, :], op=ALU.mult),
                 reads=[b_e32[sb], b_Ec[h % 2]], writes=[b["pn"]])
            P.op("pe", I("matmul", psb(5), lhsT=C.ones_bf[0:127, :], rhs=pn[0:127, h, :], start=True, stop=True),
                 reads=[b["pn"]], writes=[bps[5]])
            P.op("act", I("activation", out=rZ, in_=psb(5), func=AF.Ln, bias=C.tiny[:, 0:1], scale=1.0), reads=[bps[5]], writes=[b["rZ"]])
            P.op("act", I("activation", out=rZ, in_=rZ, func=AF.Exp, scale=-1.0), reads=[b["rZ"]], writes=[b["rZ"]])
            P.op("pool", I("tensor_tensor", out=pn[:, h, :], in0=pn[:, h, :], in1=rZ, op=ALU.mult),
                 reads=[b["pn"], b["rZ"]], writes=[b["pn"]])
            pb = 2 + h % 2
            for s4 in range(4):
                P.op("pe", I("matmul", C.ps[:, pb, s4 * 65:s4 * 65 + 64], lhsT=pn[0:127, h, s4 * 128:(s4 + 1) * 128], rhs=vcmp[0:127, g, :],
                             start=True, stop=True), reads=[b["pn"], b["vcmp"]], writes=[bps[pb]])
            combine(h, pb, 0, 3, 0, True, qt)
        for s4 in range(4):
            for g in range(2):
                for hh in range(4):
                    h = g * 4 + hh
                    P.op("pe", I("matmul", C.ps[:, 4, (s4 * 2 + g) * 32:(s4 * 2 + g + 1) * 32], lhsT=pn[0:127, h, s4 * 128:(s4 + 1) * 128],
                                 rhs=ovl[0:127, :], start=(hh == 0), stop=(hh == 3)), reads=[b["pn"], b["cst"]], writes=[bps[4]])

        def attend(h, kT, va, kts, sel):
            pr, half, g = h // 2, h % 2, h // 4
            hs = slice(half * 64, (half + 1) * 64)
            pb = 2 + h % 2
            for kt in kts:
                k0 = kt * 128
                dl = q0 - k0
                jlo = max(0, -(dl // 128))
                jhi = 3 if sel else min(3, (512 - dl) // 128)
                cols = slice(jlo * 128, (jhi + 1) * 128)
                sb = cnt["sb"] % 2
                cnt["sb"] += 1
                P.op("pe", I("matmul", C.ps[:, sb, cols], lhsT=kT[hs, g, k0:k0 + 128], rhs=qTt[hs, pr, cols], start=True, stop=(not sel)),
                     reads=[b["ksT" if sel else "kwT"], b["qTt"]], writes=[bps[sb]])
                if sel:
                    P.op("pe", I("matmul", C.ps[:, sb, cols], lhsT=Ind[0:32, k0:k0 + 128], rhs=negmT[0:32, g, cols], start=False, stop=True),
                         reads=[b["cst"], b["negmT"]], writes=[bps[sb]])
                r = cnt["ev"] % 3
                cnt["ev"] += 1
                P.op("act", I("activation", out=pexp[r][:, cols], in_=C.ps[:, sb, cols], func=AF.Exp, bias=C.tab31[:, h:h + 1], scale=1.0),
                     reads=[bps[sb], C.b_const], writes=[b_pexp[r]])
                for j in range(jlo, jhi + 1):
                    dp = dl + 128 * j
                    tb = {0: 0, 128: 128, 512: 256}.get(dp) if (not sel or dp < 256) else None
                    if tb is not None:
                        eng = "dve" if (j % 2 == 0) else "pool"
                        P.op(eng, I("tensor_tensor", out=pexp[r][:, j * 128:(j + 1) * 128], in0=pexp[r][:, j * 128:(j + 1) * 128],
                                    in1=C.Dtab[:, h, tb:tb + 128], op=ALU.mult), reads=[b_pexp[r], C.b_const], writes=[b_pexp[r]])
                for j in range(jlo, jhi + 1):
                    P.op("pe", I("matmul", C.ps[:, pb, j * 65:(j + 1) * 65], lhsT=pexp[r][:, j * 128:(j + 1) * 128], rhs=vtok[:, kt, va + g, :],
                                 start=(kt == kts[0] and j == jlo), stop=(kt == kts[-1] and j == jhi)),
                         reads=[b_pexp[r], b["vtok"]], writes=[bps[pb]])
            combine(h, pb, 0, 3, 8 if sel else 16, False, qt)

        if stop <= 3:
            break
        for h in range(8):
            attend(h, kwT, 2, range(max(0, 4 * qt - 4), 4 * qt + 4), False)
        if stop <= 4:
            break
        P.op("dve", I("tensor_tensor", out=sc.rearrange("p (s g) n -> p s g n", g=2), in0=C.ps[:, 4, 0:256].rearrange("p (s g n) -> p s g n", s=4, g=2),
                      in1=valid[:, qt * 4:(qt + 1) * 4, :].unsqueeze(2).to_broadcast([128, 4, 2, 32]), op=ALU.mult),
             reads=[bps[4], b["cst"]], writes=[b["sc"]])
        P.op("dve", I("tensor_tensor", out=sc.rearrange("p (s g) n -> p s g n", g=2), in0=sc.rearrange("p (s g) n -> p s g n", g=2),
                      in1=addm[:, qt * 4:(qt + 1) * 4, :].unsqueeze(2).to_broadcast([128, 4, 2, 32]), op=ALU.add),
             reads=[b["sc"], b["cst"]], writes=[b["sc"]])
        for sg in range(8):
            P.op("dve", I("max", out=mx[:, sg, :], in_=sc[:, sg, :]), reads=[b["sc"]], writes=[b["mx"]])
        for sg in range(8):
            P.op("dve", I("tensor_scalar", out=negm[:, sg, :], in0=sc[:, sg, :], scalar1=mx[:, sg, 7:8], scalar2=NEG, op0=ALU.is_lt, op1=ALU.mult),
                 reads=[b["sc"], b["mx"]], writes=[b["negm"]])
        for s4 in range(4):
            for g in range(2):
                P.op("pe", I("transpose", psbf(5)[0:32, g * 512 + s4 * 128:g * 512 + (s4 + 1) * 128], negm[:, s4 * 2 + g, :], C.ident_bf),
                     reads=[b["negm"]], writes=[bps[5]])
        P.op("act", I("activation", out=negmT.rearrange("p g t -> p (g t)")[0:32, :], in_=psbf(5)[0:32, :], func=AF.Copy),
             reads=[bps[5]], writes=[b["negmT"]])
        if stop <= 5:
            break
        for h in range(8):
            attend(h, ksT, 0, range(0, 4 * qt + 4), True)
        for j in range(4):
            P.op("act", I("activation", out=on[:, j, :], in_=oacc[:, j, :], func=AF.Square, accum_out=ssq[:, j:j + 1]),
                 reads=[b["oacc"]], writes=[b["on"], b["ssq"]])
        P.op("act", I("activation", out=ssq[:, 0:4], in_=ssq[:, 0:4], func=AF.Ln, bias=C.eps512[:, 0:1], scale=1.0), reads=[b["ssq"]], writes=[b["ssq"]])
        P.op("act", I("activation", out=ssq[:, 0:4], in_=ssq[:, 0:4], func=AF.Exp, scale=-0.5), reads=[b["ssq"]], writes=[b["ssq"]])
        for j in range(4):
            P.op("dve", I("tensor_scalar", out=on[:, j, :], in0=oacc[:, j, :], scalar1=ssq[:, j:j + 1], scalar2=None, op0=ALU.mult),
                 reads=[b["oacc"], b["ssq"]], writes=[b["on"]])
            pb = 6 + j % 2
            for kc in range(4):
                P.op("pe", I("transpose", psbf(pb)[:, kc * 128:(kc + 1) * 128], on[:, j, kc * 128:(kc + 1) * 128], C.ident_bf),
                     reads=[b["on"]], writes=[bps[pb]])
            P.op("dve", I("tensor_tensor", out=mixT[:, :, j * 128:(j + 1) * 128], in0=psbf(pb)[:, 0:512].rearrange("p (k t) -> p k t", k=4),
                          in1=vec[:, V_NG:V_NG + 4].unsqueeze(2).to_broadcast([128, 4, 128]), op=ALU.mult),
                 reads=[bps[pb], C.b_const], writes=[b["mixT"]])
        for m in range(8):
            pb = 6 + m % 2
            for kc in range(4):
                P.op("pe", I("matmul", psb(pb), lhsT=Woa[:, m, kc, :], rhs=mixT[:, kc, :], start=(kc == 0), stop=(kc == 3)),
                     reads=[b["Woa"], b["mixT"]], writes=[bps[pb]])
            P.op("dve", I("tensor_tensor", out=C.xT[:, m, qs], in0=psb(pb), in1=C.xT[:, m, qs], op=ALU.add),
                 reads=[bps[pb], C.b_xT[qt]], writes=[C.b_xT[qt]])
    P.barrier()


def ple_stage(P, C, d, l, gidx):
    rmsnorm_T(P, C, gidx)
    A = Alloc(C)
    pT = A.b(2048).rearrange("p (k t) -> p k t", k=2)
    Wp = A.b(1024).rearrange("p (m k n) -> p m k n", m=8, k=2)
    wring = [A.b(512).rearrange("p (k m) -> p k m", k=8) for _ in range(3)]
    sg = [A.f(512) for _ in range(2)]
    b_pT, b_Wp = Buf("pT"), Buf("Wp")
    b_w = [Buf("w%d" % i) for i in range(3)]
    b_sg = [Buf("sg%d" % i) for i in range(2)]
    bps = C.b_ps
    for k in range(2):
        P.dma("pool", pT[:, k, :], d["pT"][l][:, k, :], writes=[b_pT])
    P.dma("pool", Wp.rearrange("p m k n -> p (m k n)"), d["wp"][l], writes=[b_Wp])
    step = 0
    for m in range(8):
        s = m % 3
        P.dma("pool", wring[s].rearrange("p k m -> p (k m)"), d["wg"][l, m], writes=[b_w[s]])
        for tt in range(4):
            ts = slice(tt * 512, (tt + 1) * 512)
            pg, pp = step % 2, 2 + step % 2
            k = step % 2
            step += 1
            for kc in range(8):
                P.op("pe", I("matmul", C.ps[:, pg, :], lhsT=wring[s][:, kc, :], rhs=C.xnT[:, kc, ts], start=(kc == 0), stop=(kc == 7)),
                     reads=[b_w[s], C.b_xn[tt]], writes=[bps[pg]])
            for kc in range(2):
                P.op("pe", I("matmul", C.ps[:, pp, :], lhsT=Wp[:, m, kc, :], rhs=pT[:, kc, ts], start=(kc == 0), stop=(kc == 1)),
                     reads=[b_Wp, b_pT], writes=[bps[pp]])
            P.op("act", I("activation", out=sg[k], in_=C.ps[:, pg, :], func=AF.Sigmoid), reads=[bps[pg]], writes=[b_sg[k]])
            P.op("dve", I("tensor_tensor", out=sg[k], in0=sg[k], in1=C.ps[:, pp, :], op=ALU.mult), reads=[b_sg[k], bps[pp]], writes=[b_sg[k]])
            P.op("dve", I("tensor_tensor", out=C.xT[:, m, ts], in0=sg[k], in1=C.xT[:, m, ts], op=ALU.add),
                 reads=[b_sg[k], C.b_xT[tt]], writes=[C.b_xT[tt]])
    P.barrier()


def final_norm_store(P, C, gidx, outT_d):
    ph = OFF_PH + 18432
    sq = bview(C, ph, 2048).rearrange("p (c t) -> p c t", c=8)
    rstd = fview(C, ph + 2048, 2048)
    for tt in range(4):
        ts = slice(tt * 512, (tt + 1) * 512)
        P.op("act", I("activation", out=sq, in_=C.xT[:, :, ts], func=AF.Square),
             reads=[C.b_xT[tt]], writes=[C.b_sq])
        for c in range(8):
            P.op("pe", I("matmul", C.ps[:, tt, :], lhsT=C.ones_bf, rhs=sq[:, c, :],
                                                      start=(c == 0), stop=(c == 7)),
                 reads=[C.b_sq], writes=[C.b_ps[tt]])
    psall = C.ps[:, 0:4, :].rearrange("p a t -> p (a t)")
    P.op("act", I("activation", out=rstd, in_=psall, func=AF.Ln, bias=C.eps1024[:, 0:1], scale=1.0),
         reads=C.b_ps[0:4], writes=[C.b_rstd])
    P.op("act", I("activation", out=rstd, in_=rstd, func=AF.Exp, scale=-0.5),
         reads=[C.b_rstd], writes=[C.b_rstd])
    for tt in range(4):
        ts = slice(tt * 512, (tt + 1) * 512)
        for c in range(8):
            P.op("dve", I("scalar_tensor_tensor",
                out=C.xT[:, c, ts], in0=C.xT[:, c, ts], scalar=C.gains[:, gidx + c:gidx + c + 1],
                in1=rstd[:, ts], op0=ALU.mult, op1=ALU.mult),
                reads=[C.b_xT[tt], C.b_rstd], writes=[C.b_xT[tt]])
        P.dma("sp", outT_d[:, :, ts], C.xT[:, :, ts], reads=[C.b_xT[tt]])


NG_PER_LAYER = 4 * 8


def build_nc(n_layers=DEPTH, stages=("ffn1", "mix", "ffn2", "ple"), final=True, dbg=False, nsa_stop=99):
    nc = bass.Bass("TRN2", target_bir_lowering=False)
    P = Prog(nc)
    C = Ctx()
    C.nsa_stop = nsa_stop
    C.nc = nc
    C.dbg = dbg
    L = DEPTH
    d = {}
    d["xT"] = nc.dram_tensor("xT", [128, 8, SEQ], F32, kind="ExternalInput").ap()
    d["gains"] = nc.dram_tensor("gains", [128, L * NG_PER_LAYER + 8], F32, kind="ExternalInput").ap()
    d["f1_win"] = nc.dram_tensor("f1_win", [L, NJ, 128, 2048], F32, kind="ExternalInput").ap()
    d["f1_wout"] = nc.dram_tensor("f1_wout", [L, 8, 128, D_FF], F32, kind="ExternalInput").ap()
    d["f2_win"] = nc.dram_tensor("f2_win", [L, NJ, 128, 2048], F32, kind="ExternalInput").ap()
    d["f2_wout"] = nc.dram_tensor("f2_wout", [L, 8, 128, D_FF], F32, kind="ExternalInput").ap()
    d["wfm"] = nc.dram_tensor("wfm", [L, NFM, 128, 1024], F32, kind="ExternalInput").ap()
    d["wdt"] = nc.dram_tensor("wdt", [L, 128, 128], F32, kind="ExternalInput").ap()
    d["wmo"] = nc.dram_tensor("wmo", [L, 8, 128, 1536], F32, kind="ExternalInput").ap()
    d["vecs"] = nc.dram_tensor("vecs", [128, L, NVEC], F32, kind="ExternalInput").ap()
    d["rows"] = nc.dram_tensor("rows", [1, L * 48], F32, kind="ExternalInput").ap()
    d["cst"] = nc.dram_tensor("cst", [128, 512], F32, kind="ExternalInput").ap()
    d["rel_table"] = nc.dram_tensor("rel_table", [32, 8], F32, kind="ExternalInput").ap()
    d["oht"] = nc.dram_tensor("oht", [33, 383 + 255 + LC], F32, kind="ExternalInput").ap()
    d["ind"] = nc.dram_tensor("ind", [32, SEQ], F32, kind="ExternalInput").ap()
    d["ovl"] = nc.dram_tensor("ovl", [127, 32], F32, kind="ExternalInput").ap()
    d["valid"] = nc.dram_tensor("valid", [128, 512], F32, kind="ExternalInput").ap()
    d["addm"] = nc.dram_tensor("addm", [128, 512], F32, kind="ExternalInput").ap()
    d["b2v"] = nc.dram_tensor("b2v", [L, 64], F32, kind="ExternalInput").ap()
    d["wv"] = nc.dram_tensor("wv", [L, 128, 2240], F32, kind="ExternalInput").ap()
    d["cw1"] = nc.dram_tensor("cw1", [L, 2, 64, 8192], F32, kind="ExternalInput").ap()
    d["cpos"] = nc.dram_tensor("cpos", [L, 2, 64, 32], F32, kind="ExternalInput").ap()
    d["cw2k"] = nc.dram_tensor("cw2k", [L, 128, 256], F32, kind="ExternalInput").ap()
    d["cw2v"] = nc.dram_tensor("cw2v", [L, 128, 128], F32, kind="ExternalInput").ap()
    d["pT"] = nc.dram_tensor("pT", [L, 128, 2, SEQ], F32, kind="ExternalInput").ap()
    d["wg"] = nc.dram_tensor("wg", [L, 8, 128, 1024], F32, kind="ExternalInput").ap()
    d["wp"] = nc.dram_tensor("wp", [L, 128, 2048], F32, kind="ExternalInput").ap()
    d["scrA"] = nc.dram_tensor("scrA", [8, 128 * 383], F32).ap()
    d["scrB"] = nc.dram_tensor("scrB", [8, 128 * 255], F32).ap()
    d["scrC"] = nc.dram_tensor("scrC", [8, 128 * LC], F32).ap()
    outT = nc.dram_tensor("outT", [128, 8, SEQ], F32, kind="ExternalOutput").ap()

    with ExitStack() as st:
        C.arena = st.enter_context(nc.sbuf_tensor("arena", [128, ARENA_WORDS], F32))
        C.ps = st.enter_context(nc.psum_tensor("ps", [128, 8, 512], F32))
        C.xT = C.arena[:, OFF_XT:OFF_XT + 16384].rearrange("p (c t) -> p c t", c=8)
        C.xnT = bview(C, OFF_XN, 8192).rearrange("p (c t) -> p c t", c=8)
        ngc = L * NG_PER_LAYER + 8
        C.gains = fview(C, OFF_CONST, ngc)
        C.eps1024 = fview(C, OFF_CONST + ngc, 1)
        C.eps512 = fview(C, OFF_CONST + ngc + 1, 1)
        C.one_c = fview(C, OFF_CONST + ngc + 2, 1)
        C.ones_bf = bview(C, OFF_CONST + ngc + 8, 64)
        co = Alloc(C, OFF_CONST + ngc + 8 + 64)
        C.ident_bf = co.b(64)
        C.U_f = co.f(128)
        C.SL_f = co.f(128)
        C.ones_f = co.f(128)
        C.vecs = co.f(L * NVEC).rearrange("p (l v) -> p l v", l=L)
        C.rows = co.f(L * 48).rearrange("p (l v) -> p l v", l=L)
        C.tab31 = co.f(8)
        C.ntab31 = co.f(8)
        C.tiny = co.f(1)
        C.Dtab = co.b(1536).rearrange("p (h m) -> p h m", h=8)
        assert co.o <= OFF_PH, co.o
        C.b_xT = [Buf("xT%d" % i) for i in range(4)]
        C.b_xn = [Buf("xn%d" % i) for i in range(4)]
        C.b_ps = [Buf("ps%d" % i) for i in range(8)]
        C.b_sq = Buf("sq")
        C.b_rstd = Buf("rstd")
        b_const = Buf("const")
        C.b_const = b_const

        for tt in range(4):
            ts = slice(tt * 512, (tt + 1) * 512)
            P.dma("sp", C.xT[:, :, ts], d["xT"][:, :, ts], writes=[C.b_xT[tt]])
        P.dma("sp", C.gains, d["gains"], writes=[b_const])
        P.op("dve", I("tensor_scalar", out=C.gains, in0=C.gains, scalar1=32.0, scalar2=None, op0=ALU.mult),
             reads=[b_const], writes=[b_const])
        P.op("dve", I("memset", C.eps1024, 1024.0 * EPS), writes=[b_const])
        P.op("dve", I("memset", C.ones_bf, 1.0), writes=[b_const])
        P.op("dve", I("memset", C.eps512, 512.0 * EPS), writes=[b_const])
        P.op("dve", I("memset", C.one_c, 1.0), writes=[b_const])
        P.op("dve", I("memset", C.tiny, 1e-30), writes=[b_const])
        P.dma("sp", C.arena[:, OFF_CONST + ngc + 8 + 64 + 64:OFF_CONST + ngc + 8 + 64 + 64 + 384], d["cst"][:, 0:384], writes=[b_const])
        tmp_id = fview(C, OFF_PH, 128)
        P.dma("sp", tmp_id, d["cst"][:, 384:512], writes=[b_const])
        P.op("dve", I("tensor_copy", out=C.ident_bf, in_=tmp_id), reads=[b_const], writes=[b_const])
        P.dma("sp", C.vecs.rearrange("p l v -> p (l v)"), d["vecs"].rearrange("p l v -> p (l v)"), writes=[b_const])
        P.dma("sp", C.rows.rearrange("p l v -> p (l v)"), d["rows"].partition_broadcast(128), writes=[b_const])
        s512 = math.sqrt(512.0)
        P.op("dve", I("tensor_scalar", out=C.vecs[:, :, V_SG:V_SG + 12], in0=C.vecs[:, :, V_SG:V_SG + 12], scalar1=s512,
                                              scalar2=None, op0=ALU.mult), reads=[b_const], writes=[b_const])
        P.barrier()
        if "mix" in stages or "nsa" in stages or "nsatab" in stages:
            nsa_tables(P, C, d)

        for l in range(n_layers):
            g0 = l * NG_PER_LAYER
            if "ffn1" in stages:
                ffn_stage(P, C, d["f1_win"], d["f1_wout"], l, g0 + 0)
            if "mix" in stages or "ssd" in stages or "nsa" in stages:
                rmsnorm_T(P, C, g0 + 8)
                P.barrier()
            if "mix" in stages or "nsa" in stages:
                nsa_phase(P, C, d, l, stop=C.nsa_stop)
            if "mix" in stages or "ssd" in stages:
                ssd_phase(P, C, d, l)
            if "ffn2" in stages:
                ffn_stage(P, C, d["f2_win"], d["f2_wout"], l, g0 + 16)
            if "ple" in stages:
                ple_stage(P, C, d, l, g0 + 24)
        if final:
            final_norm_store(P, C, L * NG_PER_LAYER, outT)
        else:
            for tt in range(4):
                ts = slice(tt * 512, (tt + 1) * 512)
                P.dma("sp", outT[:, :, ts], C.xT[:, :, ts], reads=[C.b_xT[tt]])
        P.emit()
    return nc


def _fm(v):
    return np.ascontiguousarray(v.reshape(-1, 128).T)


def prep_shared(inp):
    L = DEPTH
    sh = {}
    g = np.zeros((128, L * NG_PER_LAYER + 8), np.float32)
    for l in range(L):
        for k, nm in enumerate(("ffn1_norm", "mix_norm", "ffn2_norm", "ple_norm")):
            g[:, l * NG_PER_LAYER + k * 8:l * NG_PER_LAYER + k * 8 + 8] = _fm(np.asarray(inp[nm][l]))
    g[:, L * NG_PER_LAYER:] = _fm(np.asarray(inp["final_norm"]))
    sh["gains"] = g
    for pre, a, b in (("f1", "ffn1_w_in", "ffn1_w_out"), ("f2", "ffn2_w_in", "ffn2_w_out")):
        wi = np.asarray(inp[a])
        wi = wi.reshape(L, 8, 128, 2, NJ, 128)
        sh[pre + "_win"] = np.ascontiguousarray(wi.transpose(0, 4, 2, 3, 1, 5)).reshape(L, NJ, 128, 2048)
        wo = np.asarray(inp[b])
        wo = wo.reshape(L, NJ, 128, 8, 128)
        sh[pre + "_wout"] = np.ascontiguousarray(wo.transpose(0, 3, 2, 1, 4)).reshape(L, 8, 128, D_FF)
    W = np.asarray(inp["w_mix_in"])
    cols = []
    for pr in range(4):
        cols.append(np.arange(pr * 128, (pr + 1) * 128))
    for base in (768, 1024):
        for g in range(2):
            c = np.arange(base + g * 64, base + (g + 1) * 64)
            cols.append(np.concatenate([c, c]))
    cols.append(np.arange(512, 640))
    cols.append(np.arange(640, 768))
    for c in range(8):
        cols.append(np.arange(1304 + c * 128, 1304 + (c + 1) * 128))
    for c in range(12):
        cols.append(np.arange(2328 + c * 128, 2328 + (c + 1) * 128))
    cols = np.stack(cols, 0)
    Wk = W.reshape(L, 8, 128, 3880)
    wfm = Wk[:, :, :, cols]
    sh["wfm"] = np.ascontiguousarray(wfm.transpose(0, 3, 2, 1, 4)).reshape(L, NFM, 128, 1024)
    sh["wdt"] = np.ascontiguousarray(Wk[:, :, :, 3864:3880].transpose(0, 2, 1, 3)).reshape(L, 128, 128)
    wo = np.asarray(inp["w_mix_out"]).reshape(L, 12, 128, 8, 128)
    sh["wmo"] = np.ascontiguousarray(wo.transpose(0, 3, 2, 1, 4)).reshape(L, 8, 128, 1536)
    vecs = np.zeros((128, L, NVEC), np.float32)
    for l in range(L):
        cw = np.asarray(inp["conv_w"][l])
        vecs[:, l, V_CW:V_CW + 48] = cw.reshape(4, 12, 128).transpose(2, 1, 0).reshape(128, 48)
        vecs[:, l, V_CB:V_CB + 12] = np.asarray(inp["conv_b"][l]).reshape(12, 128).T
        vecs[:, l, V_SG:V_SG + 8] = np.asarray(inp["ssm_out_norm"][l]).reshape(8, 128).T
        vecs[:, l, V_NG:V_NG + 4] = np.asarray(inp["nsa_out_norm"][l]).reshape(4, 128).T
        vecs[:, l, V_B1:V_B1 + 4] = np.asarray(inp["cmp_b1"][l]).reshape(4, 128).T
    sh["vecs"] = vecs
    rows = np.zeros((1, L * 48), np.float32)
    for l in range(L):
        rows[0, l * 48:l * 48 + 16] = np.asarray(inp["dt_bias"][l])
        rows[0, l * 48 + 16:l * 48 + 32] = np.asarray(inp["a_log"][l])
        rows[0, l * 48 + 32:l * 48 + 48] = np.asarray(inp["d_skip"][l])
    sh["rows"] = rows
    ii = np.arange(128)
    cst = np.zeros((128, 512), np.float32)
    cst[:, 0:128] = (ii[:, None] <= ii[None, :])
    cst[:, 128:256] = (ii[:, None] > ii[None, :])
    cst[:, 256:384] = 1.0
    cst[:, 384:512] = np.eye(128)
    sh["cst"] = cst
    sh["rel_table"] = np.ascontiguousarray(np.asarray(inp["rel_table"], np.float32))
    def onehot(dvals, validm):
        o = np.zeros((33, len(dvals)), np.float32)
        bk = t5_bucket_np(dvals)
        for n in range(len(dvals)):
            if validm[n]:
                o[bk[n], n] = 1.0
            else:
                o[32, n] = 1.0
        return o
    dA = np.arange(383) - 127
    dB = np.arange(255) + 385
    dC = np.arange(LC) - 2047
    sh["oht"] = np.concatenate([onehot(dA, dA >= 0), onehot(dB, dB < 512), onehot(dC, dC >= 0)], axis=1)
    kk = np.arange(SEQ)
    sh["ind"] = (kk[None, :] // 64 == np.arange(32)[:, None]).astype(np.float32)
    cc = np.arange(127)
    nn = np.arange(32)
    sh["ovl"] = ((16 * cc[:, None] <= 64 * nn[None, :] + 63) & (16 * cc[:, None] + 31 >= 64 * nn[None, :])).astype(np.float32)
    t = (np.arange(16)[None, :] * 128 + np.arange(128)[:, None])
    cur = (t // 64)[:, :, None]
    blk = nn[None, None, :]
    vld = (blk <= cur)
    forced = ((blk == 0) | (blk == cur) | (blk == cur - 1))
    sh["valid"] = vld.astype(np.float32).reshape(128, 512)
    sh["addm"] = np.where(vld, np.where(forced, 1e4, 0.0), -1e4).astype(np.float32).reshape(128, 512)
    sh["b2v"] = np.ascontiguousarray(np.asarray(inp["cmp_b2"])[:, 1, :])
    tcols = np.concatenate([np.arange(896, 1024), np.arange(1152, 1280), np.arange(1280, 1304)])
    sh["wv"] = np.ascontiguousarray(Wk[:, :, :, tcols].transpose(0, 2, 1, 3)).reshape(L, 128, 2240)
    w1 = np.asarray(inp["cmp_w1"]).reshape(L, 2, 32, 64, 256)
    sh["cw1"] = np.ascontiguousarray(w1.transpose(0, 1, 3, 2, 4)).reshape(L, 2, 64, 8192)
    sh["cpos"] = np.ascontiguousarray(np.asarray(inp["cmp_pos"]).transpose(0, 1, 3, 2))
    w2 = np.asarray(inp["cmp_w2"]).reshape(L, 2, 2, 128, 64)
    w2k = w2[:, 0].transpose(0, 2, 1, 3)
    sh["cw2k"] = np.ascontiguousarray(np.concatenate([w2k, w2k], axis=-1)).reshape(L, 128, 256)
    sh["cw2v"] = np.ascontiguousarray(w2[:, 1].transpose(0, 2, 1, 3)).reshape(L, 128, 128)
    b2k = np.asarray(inp["cmp_b2"])[:, 0, :]
    for l in range(L):
        sh["vecs"][:, l, V_B2K] = np.concatenate([b2k[l], b2k[l]])
    wg = np.asarray(inp["ple_gate_w"]).reshape(L, 8, 128, 8, 128)
    sh["wg"] = np.ascontiguousarray(wg.transpose(0, 3, 2, 1, 4)).reshape(L, 8, 128, 1024)
    wp = np.asarray(inp["ple_proj_w"]).reshape(L, 2, 128, 8, 128)
    sh["wp"] = np.ascontiguousarray(wp.transpose(0, 2, 3, 1, 4)).reshape(L, 128, 2048)
    return sh


def prep_core(inp, b):
    x = np.asarray(inp["x"][b])
    xT = np.ascontiguousarray(x.T.reshape(8, 128, SEQ).transpose(1, 0, 2))
    p = np.asarray(inp["p"][:, b])
    pT = np.ascontiguousarray(p.transpose(0, 2, 1).reshape(DEPTH, 2, 128, SEQ).transpose(0, 2, 1, 3))
    return {"xT": xT, "pT": pT}


_NC_CACHE = {}


def kernel(**inputs):
    key = "full"
    if key not in _NC_CACHE:
        _NC_CACHE[key] = build_nc()
    nc = _NC_CACHE[key]
    sh = prep_shared(inputs)
    in_maps = []
    for b in range(8):
        m = dict(sh)
        m.update(prep_core(inputs, b))
        in_maps.append(m)
    res = run_bass_kernel_spmd(nc, in_maps, core_ids=list(range(8)))
    outs = []
    for b in range(8):
        oT = np.asarray(res.results[b]["outT"])
        outs.append(oT.transpose(2, 1, 0).reshape(SEQ, D_MODEL))
    return np.stack(outs, 0).astype(np.float32)
```

```python
import math
from contextlib import ExitStack
import numpy as np
import concourse.bass as bass
import concourse.mybir as mybir
from concourse.bass_utils import run_bass_kernel_spmd

F32 = mybir.dt.float32
BF16 = mybir.dt.bfloat16
AF = mybir.ActivationFunctionType
ALU = mybir.AluOpType
AX = mybir.AxisListType

ENGS = ("pe", "act", "dve", "pool", "sp")

D_MODEL = 1024
SEQ = 2048
DEPTH = 4
D_FF = 2816
NJ = D_FF // 128
EPS = 1e-6


def I(name, *args, **kw):
    return lambda e: getattr(e, name)(*args, **kw)


class Buf:
    __slots__ = ("name", "w", "r")

    def __init__(self, name=""):
        self.name = name
        self.w = None
        self.r = []


class Prog:
    def __init__(self, nc, n_dma_sems=48):
        self.nc = nc
        self.ops = {e: [] for e in ENGS}
        self.waited_eng = {e: {} for e in ENGS}
        self.waited_dma = {e: {} for e in ENGS}
        self.n_dma_sems = n_dma_sems
        self.dma_val = [0] * n_dma_sems
        self.dma_next = {"pool": 0, "sp": 0}
        self.half = n_dma_sems // 2

    def _need(self, eng, tok, waits):
        if tok is None:
            return
        if tok[0] == "eng":
            _, src, idx = tok
            if src == eng:
                return
            cur = self.waited_eng[eng].get(src, -1)
            if idx <= cur:
                return
            self.waited_eng[eng][src] = idx
            self.ops[src][idx]["sig"] = True
            waits.append(tok)
        else:
            _, s, val = tok
            cur = self.waited_dma[eng].get(s, 0)
            if val <= cur:
                return
            self.waited_dma[eng][s] = val
            waits.append(tok)

    def _need_same(self, eng, tok, waits):
        _, src, idx = tok
        cur = self.waited_eng[eng].get("self", -1)
        if idx <= cur:
            return
        self.waited_eng[eng]["self"] = idx
        self.ops[src][idx]["sig"] = True
        waits.append(tok)

    def _deps(self, eng, reads, writes, same_raw):
        waits = []
        for b in reads:
            if b.w is not None:
                if b.w[0] == "eng" and b.w[1] == eng:
                    if same_raw:
                        self._need_same(eng, b.w, waits)
                else:
                    self._need(eng, b.w, waits)
        for b in writes:
            if b.w is not None:
                if b.w[0] == "eng" and b.w[1] == eng:
                    if same_raw:
                        self._need_same(eng, b.w, waits)
                else:
                    self._need(eng, b.w, waits)
            for t in b.r:
                self._need(eng, t, waits)
        return waits

    def _record(self, tok, reads, writes):
        for b in writes:
            b.w = tok
            b.r = []
        for b in reads:
            if b in writes:
                continue
            b.r = [t for t in b.r if not (t[0] == tok[0] and t[1] == tok[1])]
            b.r.append(tok)

    def op(self, eng, fn, reads=(), writes=()):
        reads = [b for b in reads if b is not None]
        writes = [b for b in writes if b is not None]
        waits = self._deps(eng, reads, writes, same_raw=(eng in ("act", "dve", "pool")))
        idx = len(self.ops[eng])
        self.ops[eng].append({"fn": fn, "waits": waits, "sig": False, "dma": None})
        tok = ("eng", eng, idx)
        self._record(tok, reads, writes)
        return tok

    def dma(self, q, out_ap, in_ap, reads=(), writes=(), **kw):
        reads = [b for b in reads if b is not None]
        writes = [b for b in writes if b is not None]
        waits = self._deps(q, reads, writes, same_raw=False)
        for b in reads:
            if b.w is not None and b.w[0] == "eng" and b.w[1] == q:
                self._need_same(q, b.w, waits)
        for b in writes:
            for t in ([b.w] if b.w is not None else []) + b.r:
                if t[0] == "eng" and t[1] == q:
                    self._need_same(q, t, waits)
        s = self.dma_next[q] + (0 if q == "pool" else self.half)
        self.dma_next[q] = (self.dma_next[q] + 1) % self.half
        if self.dma_val[s] > 0:
            self._need(q, ("dma", s, self.dma_val[s]), waits)
        self.dma_val[s] += 16
        tok = ("dma", s, self.dma_val[s])

        def fn(e, out_ap=out_ap, in_ap=in_ap, kw=kw):
            return e.dma_start(out=out_ap, in_=in_ap, **kw)
        self.ops[q].append({"fn": fn, "waits": waits, "sig": False, "dma": s})
        self._record(tok, reads, writes)
        return tok

    def barrier(self):
        toks = []
        for e in ENGS:
            for i in range(len(self.ops[e]) - 1, -1, -1):
                if self.ops[e][i]["fn"] is not None and self.ops[e][i]["dma"] is None:
                    toks.append(("eng", e, i))
                    break
        for s in range(self.n_dma_sems):
            if self.dma_val[s] > 0:
                toks.append(("dma", s, self.dma_val[s]))
        for e in ENGS:
            waits = []
            for t in toks:
                self._need(e, t, waits)
            if waits:
                self.ops[e].append({"fn": None, "waits": waits, "sig": False, "dma": None})

    def emit(self):
        nc = self.nc
        self.barrier()
        with ExitStack() as st:
            sems = {e: st.enter_context(nc.semaphore("s_" + e)) for e in ENGS}
            dsems = [st.enter_context(nc.semaphore("d_%d" % i)) for i in range(self.n_dma_sems)]
            cum = {}
            for e in ENGS:
                c = 0
                arr = []
                for o in self.ops[e]:
                    if o["sig"]:
                        c += 1
                    arr.append(c)
                cum[e] = arr
            block = st.enter_context(nc.Block())

            def make(e):
                def body(eng):
                    for o in self.ops[e]:
                        for t in o["waits"]:
                            if t[0] == "eng":
                                eng.wait_ge(sems[t[1]], cum[t[1]][t[2]])
                            else:
                                eng.wait_ge(dsems[t[1]], t[2])
                        if o["fn"] is None:
                            continue
                        inst = o["fn"](eng)
                        if o["dma"] is not None:
                            inst.then_inc(dsems[o["dma"]], 16)
                        elif o["sig"]:
                            inst.then_inc(sems[e], 1)
                return body
            block.tensor(make("pe"))
            block.scalar(make("act"))
            block.vector(make("dve"))
            block.gpsimd(make("pool"))
            block.sync(make("sp"))
        return nc


class Ctx:
    dbg = False
    nsa_stop = 99

    def dump(self, P, name, ap, reads):
        if not self.dbg:
            return
        shp = list(ap.shape)
        t = self.nc.dram_tensor("dbg_" + name, shp, F32, kind="ExternalOutput").ap()
        P.dma("pool", t, ap, reads=reads)


ARENA_WORDS = 53000
OFF_XT = 0
OFF_XN = 16384
OFF_CONST = 24576
OFF_PH = 27648


def fview(C, off, n):
    return C.arena[:, off:off + n]


def bview(C, off, nwords):
    return C.arena[:, off:off + nwords].bitcast(BF16)


def rmsnorm_T(P, C, gidx):
    ph = OFF_PH + 18432
    sq = bview(C, ph, 2048).rearrange("p (c t) -> p c t", c=8)
    rstd = fview(C, ph + 2048, 2048)
    for tt in range(4):
        ts = slice(tt * 512, (tt + 1) * 512)
        P.op("act", I("activation", out=sq, in_=C.xT[:, :, ts], func=AF.Square),
             reads=[C.b_xT[tt]], writes=[C.b_sq])
        for c in range(8):
            P.op("pe", I("matmul", C.ps[:, tt, :], lhsT=C.ones_bf, rhs=sq[:, c, :],
                                                      start=(c == 0), stop=(c == 7)),
                 reads=[C.b_sq], writes=[C.b_ps[tt]])
    psall = C.ps[:, 0:4, :].rearrange("p a t -> p (a t)")
    P.op("act", I("activation", out=rstd, in_=psall, func=AF.Ln, bias=C.eps1024[:, 0:1], scale=1.0),
         reads=C.b_ps[0:4], writes=[C.b_rstd])
    P.op("act", I("activation", out=rstd, in_=rstd, func=AF.Exp, scale=-0.5),
         reads=[C.b_rstd], writes=[C.b_rstd])
    for tt in range(4):
        ts = slice(tt * 512, (tt + 1) * 512)
        for c in range(8):
            P.op("dve", I("scalar_tensor_tensor",
                out=C.xnT[:, c, ts], in0=C.xT[:, c, ts], scalar=C.gains[:, gidx + c:gidx + c + 1],
                in1=rstd[:, ts], op0=ALU.mult, op1=ALU.mult),
                reads=[C.b_xT[tt], C.b_rstd], writes=[C.b_xn[tt]])


def ffn_stage(P, C, win_d, wout_d, l, gidx):
    rmsnorm_T(P, C, gidx)
    ph = OFF_PH
    hT = bview(C, ph, 11264).rearrange("p (j t) -> p j t", j=NJ)
    win = [bview(C, ph + 11264 + i * 1024, 1024).rearrange("p (g k m) -> p g k m", g=2, k=8) for i in range(3)]
    wout = [bview(C, ph + 11264 + 3072 + i * 1408, 1408).rearrange("p (j m) -> p j m", j=NJ) for i in range(2)]
    sg = [fview(C, ph + 11264 + 3072 + 2816 + i * 512, 512) for i in range(2)]
    b_win = [Buf("win%d" % i) for i in range(3)]
    b_wout = [Buf("wout%d" % i) for i in range(2)]
    b_sg = [Buf("sg%d" % i) for i in range(2)]
    b_h = [Buf("h%d" % j) for j in range(NJ)]
    step = 0
    for hf in range(2):
        for j in range(NJ):
            s = (hf * NJ + j) % 3
            P.dma("pool", win[s].rearrange("p g k m -> p (g k m)"), win_d[l, j], writes=[b_win[s]])
            for t2 in range(2):
                tt = hf * 2 + t2
                ts = slice(tt * 512, (tt + 1) * 512)
                pg, pu = step % 2, 2 + step % 2
                for g, pb in ((0, pg), (1, pu)):
                    for kc in range(8):
                        P.op("pe", I("matmul",
                            C.ps[:, pb, :], lhsT=win[s][:, g, kc, :], rhs=C.xnT[:, kc, ts],
                            start=(kc == 0), stop=(kc == 7)),
                            reads=[b_win[s], C.b_xn[tt]], writes=[C.b_ps[pb]])
                k = step % 2
                P.op("act", I("activation", out=sg[k], in_=C.ps[:, pg, :], func=AF.Silu),
                     reads=[C.b_ps[pg]], writes=[b_sg[k]])
                P.op("dve", I("tensor_tensor",
                    out=hT[:, j, t2 * 512:(t2 + 1) * 512], in0=sg[k], in1=C.ps[:, pu, :], op=ALU.mult),
                    reads=[b_sg[k], C.b_ps[pu]], writes=[b_h[j]])
                step += 1
        for m in range(8):
            s = (hf * 8 + m) % 2
            P.dma("pool", wout[s][:, 0:11, :].rearrange("p j m -> p (j m)"), wout_d[l, m][:, 0:1408],
                  writes=[b_wout[s]])
            P.dma("pool", wout[s][:, 11:22, :].rearrange("p j m -> p (j m)"), wout_d[l, m][:, 1408:2816],
                  writes=[b_wout[s]])
            for t2 in range(2):
                tt = hf * 2 + t2
                ts = slice(tt * 512, (tt + 1) * 512)
                pb = 4 + step % 2
                for j in range(NJ):
                    P.op("pe", I("matmul",
                        C.ps[:, pb, :], lhsT=wout[s][:, j, :], rhs=hT[:, j, t2 * 512:(t2 + 1) * 512],
                        start=(j == 0), stop=(j == NJ - 1)),
                        reads=[b_wout[s], b_h[j]], writes=[C.b_ps[pb]])
                P.op("dve", I("scalar_tensor_tensor",
                    out=C.xT[:, m, ts], in0=C.ps[:, pb, :], scalar=0.5, in1=C.xT[:, m, ts],
                    op0=ALU.mult, op1=ALU.add),
                    reads=[C.b_ps[pb], C.b_xT[tt]], writes=[C.b_xT[tt]])
                step += 1
    P.barrier()


class Alloc:
    def __init__(self, C, base=None):
        self.C = C
        self.o = OFF_PH if base is None else base

    def f(self, n):
        r = fview(self.C, self.o, n)
        self.o += n
        assert self.o <= ARENA_WORDS, self.o
        return r

    def b(self, nwords):
        r = bview(self.C, self.o, nwords)
        self.o += nwords
        assert self.o <= ARENA_WORDS, self.o
        return r


NFM = 30
FM_Z = 10
FM_XBC = 18
NVEC = 12 * 4 + 12 + 8 + 4 + 4 + 1
V_CW, V_CB, V_SG, V_NG, V_B1, V_B2K = 0, 48, 60, 68, 72, 76


def ssd_phase(P, C, d, l):
    A = Alloc(C)
    zT = A.b(2048).rearrange("p (c t) -> p c t", c=8)
    xbcT = A.b(3072).rearrange("p (c t) -> p c t", c=12)
    yT = A.b(2048).rearrange("p (c t) -> p c t", c=8)
    gz = A.b(2048).rearrange("p (c t) -> p c t", c=8)
    wring = [A.b(512).rearrange("p (k m) -> p k m", k=8) for _ in range(3)]
    raw = [A.f(520) for _ in range(2)]
    t1 = [A.f(512) for _ in range(2)]
    rawhist = A.f(40)[:, 0:36].rearrange("p (c j) -> p c j", c=12)
    E = A.f(1024)
    daU = A.f(1024)
    MT = A.b(512).rearrange("p (i l) -> p i l", i=8)
    GU = A.f(256).rearrange("p (g l) -> p g l", g=2)
    xtok = A.b(512)
    xdt = A.b(512)
    xdtdec = A.b(512)
    Btok = A.b(128).rearrange("p (g n) -> p g n", g=2)
    HT = A.f(1024)
    HTbf = A.b(512)
    tmpA = A.f(1024)
    tmpB = A.f(1024)
    xD = A.f(1024)
    ytok = A.b(512)
    sm = A.f(256)
    wdt = A.b(64).rearrange("p (k n) -> p k n", k=8)
    dtr, dt, da, cs_sb, ecs, edte, coef2, etot = [sm[:, i * 16:(i + 1) * 16] for i in range(8)]
    arow = sm[:, 128:144]
    rstd2 = E.rearrange("p (g t) -> p g t", g=2)
    mixT = zT
    sq = yT
    vec = C.vecs[:, l, :]
    rows = C.rows[:, l, :]
    dtb_row, alog_row, D_row = rows[:, 0:16], rows[:, 16:32], rows[:, 32:48]

    b = {n: Buf(n) for n in ("zT xbcT yT gz E daU MT GU xtok xdt xdtdec Btok HT HTbf tmpA tmpB xD ytok "
                             "dtr dt da cs ecs edte coef2 etot arow wdt rawhist mixT rstd2").split()}
    b_w = [Buf("w%d" % i) for i in range(3)]
    b_raw = [Buf("raw%d" % i) for i in range(2)]
    b_t1 = [Buf("t1%d" % i) for i in range(2)]
    bps = C.b_ps
    psb = lambda k: C.ps[:, k, :]
    psbf = lambda k: C.ps[:, k, :].bitcast(BF16)
    wcount = [0]

    def wload(src):
        s = wcount[0] % 3
        wcount[0] += 1
        P.dma("pool", wring[s].rearrange("p k m -> p (k m)"), src, writes=[b_w[s]])
        return s

    P.op("act", I("activation", out=arow, in_=alog_row, func=AF.Exp), reads=[C.b_const], writes=[b["arow"]])
    P.op("dve", I("tensor_scalar", out=arow, in0=arow, scalar1=-1.0, scalar2=None, op0=ALU.mult),
         reads=[b["arow"]], writes=[b["arow"]])
    P.op("pool", I("memset", HT, 0.0), writes=[b["HT"]])
    P.op("pool", I("memset", HTbf, 0.0), writes=[b["HTbf"]])
    P.op("pool", I("memset", rawhist, 0.0), writes=[b["rawhist"]])
    P.dma("pool", wdt.rearrange("p k n -> p (k n)"), d["wdt"][l], writes=[b["wdt"]])

    step = [0]
    for G in range(4):
        gs = slice(G * 512, (G + 1) * 512)
        for c in range(8):
            s = wload(d["wfm"][l, FM_Z + c])
            pb = 6 + step[0] % 2
            step[0] += 1
            for kc in range(8):
                P.op("pe", I("matmul", psb(pb), lhsT=wring[s][:, kc, :], rhs=C.xnT[:, kc, gs],
                                                                start=(kc == 0), stop=(kc == 7)),
                     reads=[b_w[s], C.b_xn[G]], writes=[bps[pb]])
            P.op("act", I("activation", out=zT[:, c, :], in_=psb(pb), func=AF.Silu),
                 reads=[bps[pb]], writes=[b["zT"]])
        for ch in range(12):
            s = wload(d["wfm"][l, FM_XBC + ch])
            pb = 6 + step[0] % 2
            r = step[0] % 2
            step[0] += 1
            for kc in range(8):
                P.op("pe", I("matmul", psb(pb), lhsT=wring[s][:, kc, :], rhs=C.xnT[:, kc, gs],
                                                                start=(kc == 0), stop=(kc == 7)),
                     reads=[b_w[s], C.b_xn[G]], writes=[bps[pb]])
            P.op("dve", I("tensor_copy", out=raw[r][:, 0:3], in_=rawhist[:, ch, :]),
                 reads=[b["rawhist"]], writes=[b_raw[r]])
            P.op("act", I("activation", out=raw[r][:, 3:515], in_=psb(pb), func=AF.Copy),
                 reads=[bps[pb]], writes=[b_raw[r]])
            P.op("dve", I("tensor_copy", out=rawhist[:, ch, :], in_=raw[r][:, 512:515]),
                 reads=[b_raw[r]], writes=[b["rawhist"]])
            cw = lambda j, ch=ch: vec[:, V_CW + ch * 4 + j:V_CW + ch * 4 + j + 1]
            P.op("dve", I("tensor_scalar", out=t1[r], in0=raw[r][:, 0:512], scalar1=cw(0), scalar2=None,
                                                             op0=ALU.mult), reads=[b_raw[r], C.b_const], writes=[b_t1[r]])
            for j in range(1, 4):
                P.op("dve", I("scalar_tensor_tensor",
                    out=t1[r], in0=raw[r][:, j:j + 512], scalar=cw(j), in1=t1[r], op0=ALU.mult, op1=ALU.add),
                    reads=[b_raw[r], b_t1[r]], writes=[b_t1[r]])
            P.op("act", I("activation", out=xbcT[:, ch, :], in_=t1[r], func=AF.Silu,
                                                          bias=vec[:, V_CB + ch:V_CB + ch + 1]),
                 reads=[b_t1[r]], writes=[b["xbcT"]])
        for c4 in range(4):
            ti = G * 4 + c4
            cs_ = slice(c4 * 128, (c4 + 1) * 128)
            tk = slice(ti * 128, (ti + 1) * 128)
            for kc in range(8):
                P.op("pe", I("matmul", C.ps[:, 0, 0:16], lhsT=C.xnT[:, kc, tk], rhs=wdt[:, kc, :],
                                                            start=(kc == 0), stop=(kc == 7)),
                     reads=[C.b_xn[G], b["wdt"]], writes=[bps[0]])
            P.op("dve", I("tensor_tensor", out=dtr, in0=C.ps[:, 0, 0:16], in1=dtb_row, op=ALU.add),
                 reads=[bps[0], C.b_const], writes=[b["dtr"]])
            P.op("act", I("activation", out=dtr, in_=dtr, func=AF.Exp), reads=[b["dtr"]], writes=[b["dtr"]])
            P.op("act", I("activation", out=dt, in_=dtr, func=AF.Ln, bias=C.one_c[:, 0:1], scale=1.0),
                 reads=[b["dtr"]], writes=[b["dt"]])
            P.op("dve", I("tensor_tensor", out=da, in0=dt, in1=arow, op=ALU.mult),
                 reads=[b["dt"], b["arow"]], writes=[b["da"]])
            P.op("pe", I("matmul", C.ps[:, 0, 16:32], lhsT=C.U_f, rhs=da, start=True, stop=True),
                 reads=[b["da"]], writes=[bps[0]])
            P.op("pe", I("matmul", C.ps[:, 0, 32:48], lhsT=C.SL_f, rhs=da, start=True, stop=True),
                 reads=[b["da"]], writes=[bps[0]])
            P.op("pe", I("matmul", C.ps[:, 0, 48:64], lhsT=C.ones_f, rhs=da, start=True, stop=True),
                 reads=[b["da"]], writes=[bps[0]])
            P.op("act", I("activation", out=ecs, in_=C.ps[:, 0, 16:32], func=AF.Exp), reads=[bps[0]], writes=[b["ecs"]])
            P.op("dve", I("tensor_copy", out=cs_sb, in_=C.ps[:, 0, 16:32]), reads=[bps[0]], writes=[b["cs"]])
            P.op("act", I("activation", out=edte, in_=C.ps[:, 0, 32:48], func=AF.Exp), reads=[bps[0]], writes=[b["edte"]])
            P.op("act", I("activation", out=etot, in_=C.ps[:, 0, 48:64], func=AF.Exp), reads=[bps[0]], writes=[b["etot"]])
            P.op("dve", I("tensor_tensor", out=coef2, in0=dt, in1=edte, op=ALU.mult),
                 reads=[b["dt"], b["edte"]], writes=[b["coef2"]])
            if ti == 0:
                C.dump(P, "dt", dt, [b["dt"]])
                C.dump(P, "cs", cs_sb, [b["cs"]])
                C.dump(P, "xbcT", xbcT, [b["xbcT"]])
                C.dump(P, "zT", zT, [b["zT"]])
            for ch in range(8):
                P.op("pe", I("transpose", psbf(1)[:, ch * 128:(ch + 1) * 128], xbcT[:, ch, cs_], C.ident_bf),
                     reads=[b["xbcT"]], writes=[bps[1]])
            P.op("act", I("activation", out=xtok, in_=psbf(1), func=AF.Copy), reads=[bps[1]], writes=[b["xtok"]])
            for g in range(2):
                P.op("pe", I("transpose", psbf(0)[:, 768 + g * 128:768 + (g + 1) * 128], xbcT[:, 8 + g, cs_], C.ident_bf),
                     reads=[b["xbcT"]], writes=[bps[0]])
            P.op("dve", I("tensor_copy", out=Btok.rearrange("p g n -> p (g n)"), in_=psbf(0)[:, 768:1024]),
                 reads=[bps[0]], writes=[b["Btok"]])
            for g in range(2):
                P.op("pe", I("matmul", C.ps[:, 0, 128 + g * 128:128 + (g + 1) * 128], lhsT=xbcT[:, 8 + g, cs_],
                                                            rhs=xbcT[:, 10 + g, cs_], start=True, stop=True),
                     reads=[b["xbcT"]], writes=[bps[0]])
            P.op("dve", I("tensor_tensor", out=GU, in0=C.ps[:, 0, 128:384].rearrange("p (g l) -> p g l", g=2),
                                                  in1=C.U_f.unsqueeze(1).to_broadcast([128, 2, 128]), op=ALU.mult),
                 reads=[bps[0]], writes=[b["GU"]])
            P.op("dve", I("tensor_tensor", out=xdt.rearrange("p (h q) -> p h q", h=16),
                                                  in0=xtok.rearrange("p (h q) -> p h q", h=16),
                                                  in1=dt.unsqueeze(2).to_broadcast([128, 16, 64]), op=ALU.mult),
                 reads=[b["xtok"], b["dt"]], writes=[b["xdt"]])
            P.op("dve", I("tensor_tensor", out=xdtdec.rearrange("p (h q) -> p h q", h=16),
                                                   in0=xtok.rearrange("p (h q) -> p h q", h=16),
                                                   in1=coef2.unsqueeze(2).to_broadcast([128, 16, 64]), op=ALU.mult),
                 reads=[b["xtok"], b["coef2"]], writes=[b["xdtdec"]])
            for g in range(2):
                P.op("pe", I("matmul", psb(6 + g), lhsT=xbcT[:, 10 + g, cs_], rhs=HTbf[:, g * 512:(g + 1) * 512],
                                                            start=True, stop=True),
                     reads=[b["xbcT"], b["HTbf"]], writes=[bps[6 + g]])
            for hh in range(2):
                P.op("dve", I("tensor_tensor",
                    out=daU.rearrange("p (i l) -> p i l", i=8), in0=C.U_f.unsqueeze(1).to_broadcast([128, 8, 128]),
                    in1=da[:, hh * 8:(hh + 1) * 8].unsqueeze(2).to_broadcast([128, 8, 128]), op=ALU.mult),
                    reads=[b["da"]], writes=[b["daU"]])
                for k in range(2):
                    P.op("pe", I("matmul", psb(2 + k), lhsT=C.ones_f, rhs=daU[:, k * 512:(k + 1) * 512],
                                                       start=True, stop=True),
                         reads=[b["daU"]], writes=[bps[2 + k]])
                for i in range(8):
                    P.op("dve", I("tensor_scalar", out=E[:, i * 128:(i + 1) * 128], in0=C.ps[:, 2 + i // 4, (i % 4) * 128:(i % 4 + 1) * 128],
                                  scalar1=cs_sb[:, hh * 8 + i:hh * 8 + i + 1], scalar2=0.0, op0=ALU.subtract, op1=ALU.min),
                         reads=[bps[2 + i // 4], b["cs"]], writes=[b["E"]])
                P.op("act", I("activation", out=E, in_=E, func=AF.Exp), reads=[b["E"]], writes=[b["E"]])
                P.op("dve", I("tensor_tensor",
                    out=MT, in0=E.rearrange("p (i l) -> p i l", i=8),
                    in1=GU[:, hh, :].unsqueeze(1).to_broadcast([128, 8, 128]), op=ALU.mult),
                    reads=[b["E"], b["GU"]], writes=[b["MT"]])
                for i in range(8):
                    h = hh * 8 + i
                    P.op("pe", I("matmul", C.ps[:, 4 + hh, i * 64:(i + 1) * 64], lhsT=MT[:, i, :],
                                                                   rhs=xdt[:, h * 64:(h + 1) * 64], start=True, stop=True),
                         reads=[b["MT"], b["xdt"]], writes=[bps[4 + hh]])
            for g in range(2):
                P.op("pe", I("matmul", psb(2 + g), lhsT=Btok[:, g, :], rhs=xdtdec[:, g * 512:(g + 1) * 512],
                                                   start=True, stop=True),
                     reads=[b["Btok"], b["xdtdec"]], writes=[bps[2 + g]])
            P.op("dve", I("tensor_tensor", out=tmpB.rearrange("p (h q) -> p h q", h=16),
                                                  in0=C.ps[:, 6:8, :].rearrange("p a (i q) -> p (a i) q", q=64),
                                                  in1=ecs.unsqueeze(2).to_broadcast([128, 16, 64]), op=ALU.mult),
                 reads=[bps[6], bps[7], b["ecs"]], writes=[b["tmpB"]])
            P.op("dve", I("tensor_tensor", out=tmpB, in0=tmpB, in1=C.ps[:, 4:6, :].rearrange("p a t -> p (a t)"), op=ALU.add),
                 reads=[b["tmpB"], bps[4], bps[5]], writes=[b["tmpB"]])
            P.op("dve", I("tensor_tensor", out=xD.rearrange("p (h q) -> p h q", h=16),
                                                   in0=xtok.rearrange("p (h q) -> p h q", h=16),
                                                   in1=D_row.unsqueeze(2).to_broadcast([128, 16, 64]), op=ALU.mult),
                 reads=[b["xtok"], C.b_const], writes=[b["xD"]])
            P.op("dve", I("tensor_tensor", out=ytok, in0=tmpB, in1=xD, op=ALU.add),
                 reads=[b["tmpB"], b["xD"]], writes=[b["ytok"]])
            if ti == 0:
                C.dump(P, "xtok", xtok, [b["xtok"]])
                C.dump(P, "ytok", ytok, [b["ytok"]])
                C.dump(P, "MT", MT, [b["MT"]])
                C.dump(P, "GU", GU, [b["GU"]])
                C.dump(P, "tmpB", tmpB, [b["tmpB"]])
            P.op("dve", I("tensor_tensor", out=tmpA.rearrange("p (h q) -> p h q", h=16),
                                                  in0=HT.rearrange("p (h q) -> p h q", h=16),
                                                  in1=etot.unsqueeze(2).to_broadcast([128, 16, 64]), op=ALU.mult),
                 reads=[b["HT"], b["etot"]], writes=[b["tmpA"]])
            P.op("dve", I("tensor_tensor", out=HT, in0=tmpA, in1=C.ps[:, 2:4, :].rearrange("p a t -> p (a t)"), op=ALU.add),
                 reads=[b["tmpA"], bps[2], bps[3]], writes=[b["HT"]])
            P.op("act", I("activation", out=HTbf, in_=HT, func=AF.Copy), reads=[b["HT"]], writes=[b["HTbf"]])
            for ch in range(8):
                P.op("pe", I("transpose", psbf(1)[:, ch * 128:(ch + 1) * 128], ytok[:, ch * 128:(ch + 1) * 128], C.ident_bf),
                     reads=[b["ytok"]], writes=[bps[1]])
            P.op("act", I("activation", out=yT[:, :, cs_], in_=psbf(1).rearrange("p (c t) -> p c t", c=8), func=AF.Copy),
                 reads=[bps[1]], writes=[b["yT"]])
        P.op("dve", I("tensor_tensor", out=gz, in0=yT, in1=zT, op=ALU.mult), reads=[b["yT"], b["zT"]], writes=[b["gz"]])
        P.op("act", I("activation", out=sq, in_=gz, func=AF.Square), reads=[b["gz"]], writes=[b["yT"]])
        for sg in range(2):
            for k in range(4):
                P.op("pe", I("matmul", psb(2 + sg), lhsT=C.ones_bf, rhs=sq[:, sg * 4 + k, :],
                                                          start=(k == 0), stop=(k == 3)),
                     reads=[b["yT"]], writes=[bps[2 + sg]])
        P.op("act", I("activation", out=E, in_=C.ps[:, 2:4, :].rearrange("p a t -> p (a t)"), func=AF.Ln,
                                           bias=C.eps512[:, 0:1], scale=1.0),
             reads=[bps[2], bps[3]], writes=[b["E"]])
        P.op("act", I("activation", out=E, in_=E, func=AF.Exp, scale=-0.5), reads=[b["E"]], writes=[b["E"]])
        for ch in range(8):
            P.op("dve", I("scalar_tensor_tensor",
                out=mixT[:, ch, :], in0=gz[:, ch, :], scalar=vec[:, V_SG + ch:V_SG + ch + 1], in1=rstd2[:, ch // 4, :],
                op0=ALU.mult, op1=ALU.mult),
                reads=[b["gz"], b["E"], C.b_const], writes=[b["zT"]])
        if G == 0:
            C.dump(P, "mixT", mixT, [b["zT"]])
        for m in range(8):
            s = wload(d["wmo"][l, m][:, 512:1536])
            pb = 4 + m % 2
            for kc in range(8):
                P.op("pe", I("matmul", psb(pb), lhsT=wring[s][:, kc, :], rhs=mixT[:, kc, :],
                                                                start=(kc == 0), stop=(kc == 7)),
                     reads=[b_w[s], b["zT"]], writes=[bps[pb]])
            P.op("dve", I("tensor_tensor", out=C.xT[:, m, gs], in0=psb(pb), in1=C.xT[:, m, gs], op=ALU.add),
                 reads=[bps[pb], C.b_xT[G]], writes=[C.b_xT[G]])
    P.barrier()


NEG = -30000.0
LC = 4064


def t5_bucket_np(n):
    n = np.maximum(n, 0)
    nf = np.maximum(n, 16).astype(np.float32)
    large = 16 + (np.log(nf / np.float32(16)) / np.float32(math.log(128 / 16)) * np.float32(16)).astype(np.int32)
    return np.where(n < 16, n, np.minimum(large, 31))


def nsa_tables(P, C, d):
    A = Alloc(C)
    tab = A.f(8)
    tabx = A.f(1024).rearrange("p (h m) -> p h m", h=8)
    oht = A.f(383 + 255 + LC)
    wrow = A.f(LC)
    bt = {n: Buf(n) for n in "tab tabx oht wrow scrA scrB scrC D".split()}
    P.dma("sp", tab[0:32, :], d["rel_table"], writes=[bt["tab"]])
    P.dma("sp", oht[0:33, :], d["oht"], writes=[bt["oht"]])
    P.dma("sp", C.tab31, d["rel_table"][31:32, :].partition_broadcast(128), writes=[C.b_const])
    P.op("dve", I("tensor_scalar", out=C.ntab31, in0=C.tab31, scalar1=-1.0, scalar2=None, op0=ALU.mult),
         reads=[C.b_const], writes=[C.b_const])
    P.op("dve", I("memset", tabx[32:33, :, :], NEG), writes=[bt["tabx"]])
    P.op("dve", I("tensor_copy", out=tabx[0:32, :, :], in_=tab[0:32, :].unsqueeze(2).to_broadcast([32, 8, 128])),
         reads=[bt["tab"]], writes=[bt["tabx"]])
    for h in range(8):
        for (nm, o0, n, scr, rel) in (("A", 0, 383, d["scrA"], True), ("B", 383, 255, d["scrB"], True), ("C", 638, LC, d["scrC"], False)):
            for c0 in range(0, n, 508):
                cn = min(508, n - c0)
                P.op("pe", I("matmul", C.ps[:, 0, 0:cn], lhsT=tabx[0:33, h, :], rhs=oht[0:33, o0 + c0:o0 + c0 + cn], start=True, stop=True),
                     reads=[bt["tabx"], bt["oht"]], writes=[C.b_ps[0]])
                if rel:
                    P.op("act", I("activation", out=wrow[:, c0:c0 + cn], in_=C.ps[:, 0, 0:cn], func=AF.Exp, bias=C.ntab31[:, h:h + 1], scale=1.0),
                         reads=[C.b_ps[0], C.b_const], writes=[bt["wrow"]])
                else:
                    P.op("act", I("activation", out=wrow[:, c0:c0 + cn], in_=C.ps[:, 0, 0:cn], func=AF.Exp),
                         reads=[C.b_ps[0]], writes=[bt["wrow"]])
            P.dma("sp", scr[h].rearrange("(p n) -> p n", p=128), wrow[:, 0:n], reads=[bt["wrow"]], writes=[bt["scr" + nm]])
        P.dma("pool", C.Dtab[:, h, 0:256], bass.AP(d["scrA"].tensor, h * 128 * 383 + 127, [[382, 128], [1, 256]]),
              reads=[bt["scrA"]], writes=[C.b_const])
        P.dma("pool", C.Dtab[:, h, 256:384], bass.AP(d["scrB"].tensor, h * 128 * 255 + 127, [[254, 128], [1, 128]]),
              reads=[bt["scrB"]], writes=[C.b_const])
    C.b_scrC = bt["scrC"]
    P.barrier()


def nsa_phase(P, C, d, l, stop=99):
    A = Alloc(C)
    ksT = A.b(2048).rearrange("p (g t) -> p g t", g=2)
    kwT = A.b(2048).rearrange("p (g t) -> p g t", g=2)
    vtok = A.b(2080).rearrange("p (t a d) -> p t a d", t=16, a=4)
    Wq = A.b(2048).rearrange("p (r k m) -> p r k m", r=4, k=8)
    Woa = A.b(2048).rearrange("p (o k m) -> p o k m", o=8, k=4)
    Ind = A.b(1024)
    ovl = A.b(16)
    valid = A.f(512).rearrange("p (t n) -> p t n", t=16)
    addm = A.f(512).rearrange("p (t n) -> p t n", t=16)
    kcmpT = A.b(128).rearrange("p (g c) -> p g c", g=2)
    vcmp = A.b(64).rearrange("p (g d) -> p g d", g=2)
    b2v = A.f(64)
    gts = A.f(384).rearrange("p (t n) -> p t n", t=16)
    base_q = A.o
    kcT = A.b(1024)
    vcT = A.b(1024)
    wvt = A.b(1120).rearrange("p (k n) -> p k n", k=8)
    w1 = A.b(4096)
    posT = A.b(16)
    w2k = A.b(128).rearrange("p (j m) -> p j m", j=2)
    w2v = A.b(64).rearrange("p (j m) -> p j m", j=2)
    hid = A.b(128).rearrange("p (j c) -> p j c", j=2)
    btmp = A.f(8)
    wring = [A.b(512).rearrange("p (k m) -> p k m", k=8) for _ in range(3)]
    A2 = Alloc(C, base_q)
    qTt = A2.b(1024).rearrange("p (r t) -> p r t", r=4)
    Ec = [A2.b(256) for _ in range(2)]
    b_Ec = [Buf("Ec0"), Buf("Ec1")]
    e32 = [A2.f(512) for _ in range(2)]
    pn_off = A2.o
    pn = A2.b(2048).rearrange("p (h t) -> p h t", h=8)
    pn_f32 = fview(C, pn_off, 2048).rearrange("p (j t) -> p j t", j=4)
    rZ = A2.f(512)
    negmT = A2.b(512).rearrange("p (g t) -> p g t", g=2)
    pexp = [A2.b(256) for _ in range(3)]
    oacc = A2.f(2048).rearrange("p (j f) -> p j f", j=4)
    on = A2.b(1024).rearrange("p (j f) -> p j f", j=4)
    mixT = A2.b(1024).rearrange("p (k t) -> p k t", k=4)
    sc = A2.f(256).rearrange("p (s n) -> p s n", s=8)
    mx = A2.f(64).rearrange("p (s n) -> p s n", s=8)
    negm = A2.b(128).rearrange("p (s n) -> p s n", s=8)
    coef = A2.f(8)
    ssq = A2.f(8)
    junk = C.arena[:, 0:0]
    vec = C.vecs[:, l, :]
    rows = C.rows[:, l, :]

    b = {n: Buf(n) for n in ("ksT kwT vtok Wq Woa cst kcmpT vcmp b2v gts kcT vcT wvt w1 posT w2 hid btmp qTt Ec pn rZ "
                             "negmT oacc on mixT sc mx negm coef ssq junk").split()}
    b_w = [Buf("w%d" % i) for i in range(3)]
    b_e32 = [Buf("e32%d" % i) for i in range(2)]
    b_pexp = [Buf("pexp%d" % i) for i in range(3)]
    bps = C.b_ps
    psb = lambda k: C.ps[:, k, :]
    psbf = lambda k: C.ps[:, k, :].bitcast(BF16)
    cnt = {"w": 0, "ev": 0, "pe": 0, "sb": 0}

    def wload(src):
        s = cnt["w"] % 3
        cnt["w"] += 1
        P.dma("pool", wring[s].rearrange("p k m -> p (k m)"), src, writes=[b_w[s]])
        return s

    def evac(out, in_, reads, writes, **kw):
        cnt["ev"] += 1
        if cnt["ev"] % 2:
            P.op("act", I("activation", out=out, in_=in_, func=AF.Copy, **kw), reads=reads, writes=writes)
        else:
            if "scale" in kw:
                P.op("dve", I("tensor_scalar", out=out, in0=in_, scalar1=kw["scale"], scalar2=None, op0=ALU.mult), reads=reads, writes=writes)
            else:
                P.op("dve", I("tensor_copy", out=out, in_=in_), reads=reads, writes=writes)

    P.dma("pool", Ind[0:32, :], d["ind"], writes=[b["cst"]])
    P.dma("pool", ovl[0:127, :], d["ovl"], writes=[b["cst"]])
    P.dma("sp", valid.rearrange("p t n -> p (t n)"), d["valid"], writes=[b["cst"]])
    P.dma("sp", addm.rearrange("p t n -> p (t n)"), d["addm"], writes=[b["cst"]])
    P.dma("sp", b2v, d["b2v"][l:l + 1, :].partition_broadcast(128), writes=[b["b2v"]])
    for pr in range(4):
        P.dma("pool", Wq[:, pr, :, :].rearrange("p k m -> p (k m)"), d["wfm"][l, pr], writes=[b["Wq"]])
    for m in range(8):
        P.dma("pool", Woa[:, m, :, :].rearrange("p k m -> p (k m)"), d["wmo"][l, m][:, 0:512], writes=[b["Woa"]])
    P.dma("pool", wvt[:, 0:4, :].rearrange("p k n -> p (k n)"), d["wv"][l][:, 0:1120], writes=[b["wvt"]])
    P.dma("pool", wvt[:, 4:8, :].rearrange("p k n -> p (k n)"), d["wv"][l][:, 1120:2240], writes=[b["wvt"]])
    P.op("pool", I("memset", vtok[:, :, :, 64:65], 1.0), writes=[b["vtok"]])
    for ci, dst, bn in ((4, ksT[:, 0, :], "ksT"), (5, ksT[:, 1, :], "ksT"), (6, kwT[:, 0, :], "kwT"), (7, kwT[:, 1, :], "kwT"),
                        (8, kcT, "kcT"), (9, vcT, "vcT")):
        s = wload(d["wfm"][l, ci])
        for tt in range(4):
            ts = slice(tt * 512, (tt + 1) * 512)
            pb = 6 + cnt["pe"] % 2
            cnt["pe"] += 1
            for kc in range(8):
                P.op("pe", I("matmul", psb(pb), lhsT=wring[s][:, kc, :], rhs=C.xnT[:, kc, ts], start=(kc == 0), stop=(kc == 7)),
                     reads=[b_w[s], C.b_xn[tt]], writes=[bps[pb]])
            evac(dst[:, ts], psb(pb), [bps[pb]], [b[bn]])
    for ti in range(16):
        tk = slice(ti * 128, (ti + 1) * 128)
        pb = 4 + ti % 2
        for kc in range(8):
            P.op("pe", I("matmul", C.ps[:, pb, 0:280], lhsT=C.xnT[:, kc, tk], rhs=wvt[:, kc, :], start=(kc == 0), stop=(kc == 7)),
                 reads=[b["wvt"], C.b_xn[ti // 4]], writes=[bps[pb]])
        evac(vtok[:, ti, :, 0:64], C.ps[:, pb, 0:256].rearrange("p (a d) -> p a d", a=4), [bps[pb]], [b["vtok"]])
        P.op("act", I("activation", out=gts[:, ti, :], in_=C.ps[:, pb, 256:280], func=AF.Sigmoid), reads=[bps[pb]], writes=[b["gts"]])
    if stop <= 1:
        P.barrier()
        return
    P.dma("pool", w2k.rearrange("p j m -> p (j m)"), d["cw2k"][l], writes=[b["w2"]])
    P.dma("pool", w2v.rearrange("p j m -> p (j m)"), d["cw2v"][l], writes=[b["w2"]])
    for x in range(2):
        src = kcT if x == 0 else vcT
        bsrc = b["kcT"] if x == 0 else b["vcT"]
        for half in range(2):
            for i in range(4):
                P.dma("pool", w1[half * 64:(half + 1) * 64, i * 2048:(i + 1) * 2048], d["cw1"][l, x][:, i * 2048:(i + 1) * 2048], writes=[b["w1"]])
            P.dma("pool", posT[half * 64:(half + 1) * 64, :], d["cpos"][l, x], writes=[b["posT"]])
        for g in range(2):
            hs = slice(g * 64, (g + 1) * 64)
            for jc in range(2):
                pb = 6 + cnt["pe"] % 2
                cnt["pe"] += 1
                for ll in range(32):
                    P.op("pe", I("matmul", C.ps[:, pb, 0:127], lhsT=w1[hs, ll * 256 + jc * 128:ll * 256 + (jc + 1) * 128],
                                 rhs=src[hs, ll:ll + 2017:16], start=(ll == 0), stop=(ll == 31)),
                         reads=[b["w1"], bsrc], writes=[bps[pb]])
                for ll in range(32):
                    P.op("pe", I("matmul", C.ps[:, pb, 128:129], lhsT=w1[hs, ll * 256 + jc * 128:ll * 256 + (jc + 1) * 128],
                                 rhs=posT[hs, ll:ll + 1], start=(ll == 0), stop=(ll == 31)),
                         reads=[b["w1"], b["posT"]], writes=[bps[pb]])
                P.op("dve", I("tensor_tensor", out=btmp[:, 0:1], in0=C.ps[:, pb, 128:129], in1=vec[:, V_B1 + x * 2 + jc:V_B1 + x * 2 + jc + 1], op=ALU.add),
                     reads=[bps[pb], C.b_const], writes=[b["btmp"]])
                P.op("act", I("activation", out=hid[:, jc, 0:127], in_=C.ps[:, pb, 0:127], func=AF.Silu, bias=btmp[:, 0:1], scale=1.0),
                     reads=[bps[pb], b["btmp"]], writes=[b["hid"]])
            pb = 4 + g
            if x == 0:
                for jc in range(2):
                    P.op("pe", I("matmul", C.ps[:, pb, 0:127], lhsT=w2k[:, jc, :], rhs=hid[:, jc, 0:127], start=(jc == 0), stop=(jc == 1)),
                         reads=[b["w2"], b["hid"]], writes=[bps[pb]])
                P.op("dve", I("tensor_scalar", out=kcmpT[:, g, 0:127], in0=C.ps[:, pb, 0:127], scalar1=vec[:, V_B2K:V_B2K + 1], scalar2=None, op0=ALU.add),
                     reads=[bps[pb], C.b_const], writes=[b["kcmpT"]])
            else:
                for jc in range(2):
                    P.op("pe", I("matmul", C.ps[0:127, pb, 0:64], lhsT=hid[:, jc, 0:127], rhs=w2v[:, jc, :], start=(jc == 0), stop=(jc == 1)),
                         reads=[b["w2"], b["hid"]], writes=[bps[pb]])
                P.op("dve", I("tensor_tensor", out=vcmp[0:127, g, :], in0=C.ps[0:127, pb, 0:64], in1=b2v[0:127, :], op=ALU.add),
                     reads=[bps[pb], b["b2v"]], writes=[b["vcmp"]])
    P.barrier()

    if stop <= 2:
        return

    def combine(h, pb, j0, j1, gcol, first, qt):
        po = C.ps[:, pb, 0:260].rearrange("p (j d) -> p j d", j=4)
        gsl = gts[:, qt * 4 + j0:qt * 4 + j1 + 1, gcol + h]
        if gcol == 0:
            cf = gsl
            rd = [b["gts"]]
        else:
            P.op("dve", I("reciprocal", out=coef[:, j0:j1 + 1], in_=po[:, j0:j1 + 1, 64]), reads=[bps[pb]], writes=[b["coef"]])
            P.op("dve", I("tensor_tensor", out=coef[:, j0:j1 + 1], in0=coef[:, j0:j1 + 1], in1=gsl, op=ALU.mult),
                 reads=[b["coef"], b["gts"]], writes=[b["coef"]])
            cf = coef[:, j0:j1 + 1]
            rd = [b["coef"]]
        for j in range(j0, j1 + 1):
            dst = oacc[:, j, h * 64:(h + 1) * 64]
            if first:
                P.op("dve", I("tensor_scalar", out=dst, in0=po[:, j, 0:64], scalar1=cf[:, j - j0:j - j0 + 1], scalar2=None, op0=ALU.mult),
                     reads=[bps[pb]] + rd, writes=[b["oacc"]])
            else:
                P.op("dve", I("scalar_tensor_tensor", out=dst, in0=po[:, j, 0:64], scalar=cf[:, j - j0:j - j0 + 1], in1=dst,
                              op0=ALU.mult, op1=ALU.add), reads=[bps[pb], b["oacc"]] + rd, writes=[b["oacc"]])

    for qt in range(4):
        q0 = qt * 512
        qs = slice(q0, q0 + 512)
        for pr in range(4):
            pb = 6 + cnt["pe"] % 2
            cnt["pe"] += 1
            for kc in range(8):
                P.op("pe", I("matmul", psb(pb), lhsT=Wq[:, pr, kc, :], rhs=C.xnT[:, kc, qs], start=(kc == 0), stop=(kc == 7)),
                     reads=[b["Wq"], C.b_xn[qt]], writes=[bps[pb]])
            evac(qTt[:, pr, :], psb(pb), [bps[pb]], [b["qTt"]], scale=0.125)
        for h in range(8):
            pr, half, g = h // 2, h % 2, h // 4
            hs = slice(half * 64, (half + 1) * 64)
            sb = cnt["sb"] % 2
            cnt["sb"] += 1
            P.dma("pool", Ec[h % 2][:, :], bass.AP(d["scrC"].tensor, h * 128 * LC + q0 + 2016, [[LC - 16, 128], [1, 512]]),
                  reads=[C.b_scrC], writes=[b_Ec[h % 2]])
            P.op("pe", I("matmul", C.ps[0:127, sb, :], lhsT=kcmpT[hs, g, 0:127], rhs=qTt[hs, pr, :], start=True, stop=True),
                 reads=[b["kcmpT"], b["qTt"]], writes=[bps[sb]])
            P.op("act", I("activation", out=e32[sb][0:127, :], in_=C.ps[0:127, sb, :], func=AF.Exp), reads=[bps[sb]], writes=[b_e32[sb]])
            P.op("dve", I("tensor_tensor", out=pn[0:127, h, :], in0=e32[sb][0:127, :], in1=Ec[h % 2][0:127, :], op=ALU.mult),
                 reads=[b_e32[sb], b_Ec[h % 2]], writes=[b["pn"]])
            P.op("pe", I("matmul", psb(5), lhsT=C.ones_bf[0:127, :], rhs=pn[0:127, h, :], start=True, stop=True),
                 reads=[b["pn"]], writes=[bps[5]])
            P.op("act", I("activation", out=rZ, in_=psb(5), func=AF.Ln, bias=C.tiny[:, 0:1], scale=1.0), reads=[bps[5]], writes=[b["rZ"]])
            P.op("act", I("activation", out=rZ, in_=rZ, func=AF.Exp, scale=-1.0), reads=[b["rZ"]], writes=[b["rZ"]])
            P.op("dve", I("tensor_tensor", out=pn[:, h, :], in0=pn[:, h, :], in1=rZ, op=ALU.mult),
                 reads=[b["pn"], b["rZ"]], writes=[b["pn"]])
            pb = 2 + h % 2
            for s4 in range(4):
                P.op("pe", I("matmul", C.ps[:, pb, s4 * 65:s4 * 65 + 64], lhsT=pn[0:127, h, s4 * 128:(s4 + 1) * 128], rhs=vcmp[0:127, g, :],
                             start=True, stop=True), reads=[b["pn"], b["vcmp"]], writes=[bps[pb]])
            combine(h, pb, 0, 3, 0, True, qt)
        for s4 in range(4):
            for g in range(2):
                for hh in range(4):
                    h = g * 4 + hh
                    P.op("pe", I("matmul", C.ps[:, 4, (s4 * 2 + g) * 32:(s4 * 2 + g + 1) * 32], lhsT=pn[0:127, h, s4 * 128:(s4 + 1) * 128],
                                 rhs=ovl[0:127, :], start=(hh == 0), stop=(hh == 3)), reads=[b["pn"], b["cst"]], writes=[bps[4]])

        def attend(h, kT, va, kts, sel):
            pr, half, g = h // 2, h % 2, h // 4
            hs = slice(half * 64, (half + 1) * 64)
            pb = 2 + h % 2
            for kt in kts:
                k0 = kt * 128
                dl = q0 - k0
                jlo = max(0, -(dl // 128))
                jhi = 3 if sel else min(3, (512 - dl) // 128)
                cols = slice(jlo * 128, (jhi + 1) * 128)
                sb = cnt["sb"] % 2
                cnt["sb"] += 1
                P.op("pe", I("matmul", C.ps[:, sb, cols], lhsT=kT[hs, g, k0:k0 + 128], rhs=qTt[hs, pr, cols], start=True, stop=(not sel)),
                     reads=[b["ksT" if sel else "kwT"], b["qTt"]], writes=[bps[sb]])
                if sel:
                    P.op("pe", I("matmul", C.ps[:, sb, cols], lhsT=Ind[0:32, k0:k0 + 128], rhs=negmT[0:32, g, cols], start=False, stop=True),
                         reads=[b["cst"], b["negmT"]], writes=[bps[sb]])
                r = cnt["ev"] % 3
                cnt["ev"] += 1
                P.op("act", I("activation", out=pexp[r][:, cols], in_=C.ps[:, sb, cols], func=AF.Exp, bias=C.tab31[:, h:h + 1], scale=1.0),
                     reads=[bps[sb], C.b_const], writes=[b_pexp[r]])
                for j in range(jlo, jhi + 1):
                    dp = dl + 128 * j
                    tb = {0: 0, 128: 128, 512: 256}.get(dp) if (not sel or dp < 256) else None
                    if tb is not None:
                        eng = "dve"
                        P.op(eng, I("tensor_tensor", out=pexp[r][:, j * 128:(j + 1) * 128], in0=pexp[r][:, j * 128:(j + 1) * 128],
                                    in1=C.Dtab[:, h, tb:tb + 128], op=ALU.mult), reads=[b_pexp[r], C.b_const], writes=[b_pexp[r]])
                for j in range(jlo, jhi + 1):
                    P.op("pe", I("matmul", C.ps[:, pb, j * 65:(j + 1) * 65], lhsT=pexp[r][:, j * 128:(j + 1) * 128], rhs=vtok[:, kt, va + g, :],
                                 start=(kt == kts[0] and j == jlo), stop=(kt == kts[-1] and j == jhi)),
                         reads=[b_pexp[r], b["vtok"]], writes=[bps[pb]])
            combine(h, pb, 0, 3, 8 if sel else 16, False, qt)

        if stop <= 3:
            break
        for h in range(8):
            attend(h, kwT, 2, range(max(0, 4 * qt - 4), 4 * qt + 4), False)
        if stop <= 4:
            break
        P.op("dve", I("tensor_tensor", out=sc.rearrange("p (s g) n -> p s g n", g=2), in0=C.ps[:, 4, 0:256].rearrange("p (s g n) -> p s g n", s=4, g=2),
                      in1=valid[:, qt * 4:(qt + 1) * 4, :].unsqueeze(2).to_broadcast([128, 4, 2, 32]), op=ALU.mult),
             reads=[bps[4], b["cst"]], writes=[b["sc"]])
        P.op("dve", I("tensor_tensor", out=sc.rearrange("p (s g) n -> p s g n", g=2), in0=sc.rearrange("p (s g) n -> p s g n", g=2),
                      in1=addm[:, qt * 4:(qt + 1) * 4, :].unsqueeze(2).to_broadcast([128, 4, 2, 32]), op=ALU.add),
             reads=[b["sc"], b["cst"]], writes=[b["sc"]])
        for sg in range(8):
            P.op("dve", I("max", out=mx[:, sg, :], in_=sc[:, sg, :]), reads=[b["sc"]], writes=[b["mx"]])
        for sg in range(8):
            P.op("dve", I("tensor_scalar", out=negm[:, sg, :], in0=sc[:, sg, :], scalar1=mx[:, sg, 7:8], scalar2=NEG, op0=ALU.is_lt, op1=ALU.mult),
                 reads=[b["sc"], b["mx"]], writes=[b["negm"]])
        for s4 in range(4):
            for g in range(2):
                P.op("pe", I("transpose", psbf(5)[0:32, g * 512 + s4 * 128:g * 512 + (s4 + 1) * 128], negm[:, s4 * 2 + g, :], C.ident_bf),
                     reads=[b["negm"]], writes=[bps[5]])
        P.op("act", I("activation", out=negmT.rearrange("p g t -> p (g t)")[0:32, :], in_=psbf(5)[0:32, :], func=AF.Copy),
             reads=[bps[5]], writes=[b["negmT"]])
        if stop <= 5:
            break
        for h in range(8):
            attend(h, ksT, 0, range(0, 4 * qt + 4), True)
        for j in range(4):
            P.op("act", I("activation", out=on[:, j, :], in_=oacc[:, j, :], func=AF.Square, accum_out=ssq[:, j:j + 1]),
                 reads=[b["oacc"]], writes=[b["on"], b["ssq"]])
        P.op("act", I("activation", out=ssq[:, 0:4], in_=ssq[:, 0:4], func=AF.Ln, bias=C.eps512[:, 0:1], scale=1.0), reads=[b["ssq"]], writes=[b["ssq"]])
        P.op("act", I("activation", out=ssq[:, 0:4], in_=ssq[:, 0:4], func=AF.Exp, scale=-0.5), reads=[b["ssq"]], writes=[b["ssq"]])
        for j in range(4):
            P.op("dve", I("tensor_scalar", out=on[:, j, :], in0=oacc[:, j, :], scalar1=ssq[:, j:j + 1], scalar2=None, op0=ALU.mult),
                 reads=[b["oacc"], b["ssq"]], writes=[b["on"]])
            pb = 6 + j % 2
            for kc in range(4):
                P.op("pe", I("transpose", psbf(pb)[:, kc * 128:(kc + 1) * 128], on[:, j, kc * 128:(kc + 1) * 128], C.ident_bf),
                     reads=[b["on"]], writes=[bps[pb]])
            P.op("dve", I("tensor_tensor", out=mixT[:, :, j * 128:(j + 1) * 128], in0=psbf(pb)[:, 0:512].rearrange("p (k t) -> p k t", k=4),
                          in1=vec[:, V_NG:V_NG + 4].unsqueeze(2).to_broadcast([128, 4, 128]), op=ALU.mult),
                 reads=[bps[pb], C.b_const], writes=[b["mixT"]])
        for m in range(8):
            pb = 6 + m % 2
            for kc in range(4):
                P.op("pe", I("matmul", psb(pb), lhsT=Woa[:, m, kc, :], rhs=mixT[:, kc, :], start=(kc == 0), stop=(kc == 3)),
                     reads=[b["Woa"], b["mixT"]], writes=[bps[pb]])
            P.op("dve", I("tensor_tensor", out=C.xT[:, m, qs], in0=psb(pb), in1=C.xT[:, m, qs], op=ALU.add),
                 reads=[bps[pb], C.b_xT[qt]], writes=[C.b_xT[qt]])
    P.barrier()


def ple_stage(P, C, d, l, gidx):
    rmsnorm_T(P, C, gidx)
    A = Alloc(C)
    pT = A.b(2048).rearrange("p (k t) -> p k t", k=2)
    Wp = A.b(1024).rearrange("p (m k n) -> p m k n", m=8, k=2)
    wring = [A.b(512).rearrange("p (k m) -> p k m", k=8) for _ in range(3)]
    sg = [A.f(512) for _ in range(2)]
    b_pT, b_Wp = Buf("pT"), Buf("Wp")
    b_w = [Buf("w%d" % i) for i in range(3)]
    b_sg = [Buf("sg%d" % i) for i in range(2)]
    bps = C.b_ps
    for k in range(2):
        P.dma("pool", pT[:, k, :], d["pT"][l][:, k, :], writes=[b_pT])
    P.dma("pool", Wp.rearrange("p m k n -> p (m k n)"), d["wp"][l], writes=[b_Wp])
    step = 0
    for m in range(8):
        s = m % 3
        P.dma("pool", wring[s].rearrange("p k m -> p (k m)"), d["wg"][l, m], writes=[b_w[s]])
        for tt in range(4):
            ts = slice(tt * 512, (tt + 1) * 512)
            pg, pp = step % 2, 2 + step % 2
            k = step % 2
            step += 1
            for kc in range(8):
                P.op("pe", I("matmul", C.ps[:, pg, :], lhsT=wring[s][:, kc, :], rhs=C.xnT[:, kc, ts], start=(kc == 0), stop=(kc == 7)),
                     reads=[b_w[s], C.b_xn[tt]], writes=[bps[pg]])
            for kc in range(2):
                P.op("pe", I("matmul", C.ps[:, pp, :], lhsT=Wp[:, m, kc, :], rhs=pT[:, kc, ts], start=(kc == 0), stop=(kc == 1)),
                     reads=[b_Wp, b_pT], writes=[bps[pp]])
            P.op("act", I("activation", out=sg[k], in_=C.ps[:, pg, :], func=AF.Sigmoid), reads=[bps[pg]], writes=[b_sg[k]])
            P.op("dve", I("tensor_tensor", out=sg[k], in0=sg[k], in1=C.ps[:, pp, :], op=ALU.mult), reads=[b_sg[k], bps[pp]], writes=[b_sg[k]])
            P.op("dve", I("tensor_tensor", out=C.xT[:, m, ts], in0=sg[k], in1=C.xT[:, m, ts], op=ALU.add),
                 reads=[b_sg[k], C.b_xT[tt]], writes=[C.b_xT[tt]])
    P.barrier()


def final_norm_store(P, C, gidx, outT_d):
    ph = OFF_PH + 18432
    sq = bview(C, ph, 2048).rearrange("p (c t) -> p c t", c=8)
    rstd = fview(C, ph + 2048, 2048)
    for tt in range(4):
        ts = slice(tt * 512, (tt + 1) * 512)
        P.op("act", I("activation", out=sq, in_=C.xT[:, :, ts], func=AF.Square),
             reads=[C.b_xT[tt]], writes=[C.b_sq])
        for c in range(8):
            P.op("pe", I("matmul", C.ps[:, tt, :], lhsT=C.ones_bf, rhs=sq[:, c, :],
                                                      start=(c == 0), stop=(c == 7)),
                 reads=[C.b_sq], writes=[C.b_ps[tt]])
    psall = C.ps[:, 0:4, :].rearrange("p a t -> p (a t)")
    P.op("act", I("activation", out=rstd, in_=psall, func=AF.Ln, bias=C.eps1024[:, 0:1], scale=1.0),
         reads=C.b_ps[0:4], writes=[C.b_rstd])
    P.op("act", I("activation", out=rstd, in_=rstd, func=AF.Exp, scale=-0.5),
         reads=[C.b_rstd], writes=[C.b_rstd])
    for tt in range(4):
        ts = slice(tt * 512, (tt + 1) * 512)
        for c in range(8):
            P.op("dve", I("scalar_tensor_tensor",
                out=C.xT[:, c, ts], in0=C.xT[:, c, ts], scalar=C.gains[:, gidx + c:gidx + c + 1],
                in1=rstd[:, ts], op0=ALU.mult, op1=ALU.mult),
                reads=[C.b_xT[tt], C.b_rstd], writes=[C.b_xT[tt]])
        P.dma("sp", outT_d[:, :, ts], C.xT[:, :, ts], reads=[C.b_xT[tt]])


NG_PER_LAYER = 4 * 8


def build_nc(n_layers=DEPTH, stages=("ffn1", "mix", "ffn2", "ple"), final=True, dbg=False, nsa_stop=99):
    nc = bass.Bass("TRN2", target_bir_lowering=False)
    P = Prog(nc)
    C = Ctx()
    C.nsa_stop = nsa_stop
    C.nc = nc
    C.dbg = dbg
    L = DEPTH
    d = {}
    d["xT"] = nc.dram_tensor("xT", [128, 8, SEQ], F32, kind="ExternalInput").ap()
    d["gains"] = nc.dram_tensor("gains", [128, L * NG_PER_LAYER + 8], F32, kind="ExternalInput").ap()
    d["f1_win"] = nc.dram_tensor("f1_win", [L, NJ, 128, 2048], F32, kind="ExternalInput").ap()
    d["f1_wout"] = nc.dram_tensor("f1_wout", [L, 8, 128, D_FF], F32, kind="ExternalInput").ap()
    d["f2_win"] = nc.dram_tensor("f2_win", [L, NJ, 128, 2048], F32, kind="ExternalInput").ap()
    d["f2_wout"] = nc.dram_tensor("f2_wout", [L, 8, 128, D_FF], F32, kind="ExternalInput").ap()
    d["wfm"] = nc.dram_tensor("wfm", [L, NFM, 128, 1024], F32, kind="ExternalInput").ap()
    d["wdt"] = nc.dram_tensor("wdt", [L, 128, 128], F32, kind="ExternalInput").ap()
    d["wmo"] = nc.dram_tensor("wmo", [L, 8, 128, 1536], F32, kind="ExternalInput").ap()
    d["vecs"] = nc.dram_tensor("vecs", [128, L, NVEC], F32, kind="ExternalInput").ap()
    d["rows"] = nc.dram_tensor("rows", [1, L * 48], F32, kind="ExternalInput").ap()
    d["cst"] = nc.dram_tensor("cst", [128, 512], F32, kind="ExternalInput").ap()
    d["rel_table"] = nc.dram_tensor("rel_table", [32, 8], F32, kind="ExternalInput").ap()
    d["oht"] = nc.dram_tensor("oht", [33, 383 + 255 + LC], F32, kind="ExternalInput").ap()
    d["ind"] = nc.dram_tensor("ind", [32, SEQ], F32, kind="ExternalInput").ap()
    d["ovl"] = nc.dram_tensor("ovl", [127, 32], F32, kind="ExternalInput").ap()
    d["valid"] = nc.dram_tensor("valid", [128, 512], F32, kind="ExternalInput").ap()
    d["addm"] = nc.dram_tensor("addm", [128, 512], F32, kind="ExternalInput").ap()
    d["b2v"] = nc.dram_tensor("b2v", [L, 64], F32, kind="ExternalInput").ap()
    d["wv"] = nc.dram_tensor("wv", [L, 128, 2240], F32, kind="ExternalInput").ap()
    d["cw1"] = nc.dram_tensor("cw1", [L, 2, 64, 8192], F32, kind="ExternalInput").ap()
    d["cpos"] = nc.dram_tensor("cpos", [L, 2, 64, 32], F32, kind="ExternalInput").ap()
    d["cw2k"] = nc.dram_tensor("cw2k", [L, 128, 256], F32, kind="ExternalInput").ap()
    d["cw2v"] = nc.dram_tensor("cw2v", [L, 128, 128], F32, kind="ExternalInput").ap()
    d["pT"] = nc.dram_tensor("pT", [L, 128, 2, SEQ], F32, kind="ExternalInput").ap()
    d["wg"] = nc.dram_tensor("wg", [L, 8, 128, 1024], F32, kind="ExternalInput").ap()
    d["wp"] = nc.dram_tensor("wp", [L, 128, 2048], F32, kind="ExternalInput").ap()
    d["scrA"] = nc.dram_tensor("scrA", [8, 128 * 383], F32).ap()
    d["scrB"] = nc.dram_tensor("scrB", [8, 128 * 255], F32).ap()
    d["scrC"] = nc.dram_tensor("scrC", [8, 128 * LC], F32).ap()
    outT = nc.dram_tensor("outT", [128, 8, SEQ], F32, kind="ExternalOutput").ap()

    with ExitStack() as st:
        C.arena = st.enter_context(nc.sbuf_tensor("arena", [128, ARENA_WORDS], F32))
        C.ps = st.enter_context(nc.psum_tensor("ps", [128, 8, 512], F32))
        C.xT = C.arena[:, OFF_XT:OFF_XT + 16384].rearrange("p (c t) -> p c t", c=8)
        C.xnT = bview(C, OFF_XN, 8192).rearrange("p (c t) -> p c t", c=8)
        ngc = L * NG_PER_LAYER + 8
        C.gains = fview(C, OFF_CONST, ngc)
        C.eps1024 = fview(C, OFF_CONST + ngc, 1)
        C.eps512 = fview(C, OFF_CONST + ngc + 1, 1)
        C.one_c = fview(C, OFF_CONST + ngc + 2, 1)
        C.ones_bf = bview(C, OFF_CONST + ngc + 8, 64)
        co = Alloc(C, OFF_CONST + ngc + 8 + 64)
        C.ident_bf = co.b(64)
        C.U_f = co.f(128)
        C.SL_f = co.f(128)
        C.ones_f = co.f(128)
        C.vecs = co.f(L * NVEC).rearrange("p (l v) -> p l v", l=L)
        C.rows = co.f(L * 48).rearrange("p (l v) -> p l v", l=L)
        C.tab31 = co.f(8)
        C.ntab31 = co.f(8)
        C.tiny = co.f(1)
        C.Dtab = co.b(1536).rearrange("p (h m) -> p h m", h=8)
        assert co.o <= OFF_PH, co.o
        C.b_xT = [Buf("xT%d" % i) for i in range(4)]
        C.b_xn = [Buf("xn%d" % i) for i in range(4)]
        C.b_ps = [Buf("ps%d" % i) for i in range(8)]
        C.b_sq = Buf("sq")
        C.b_rstd = Buf("rstd")
        b_const = Buf("const")
        C.b_const = b_const

        for tt in range(4):
            ts = slice(tt * 512, (tt + 1) * 512)
            P.dma("sp", C.xT[:, :, ts], d["xT"][:, :, ts], writes=[C.b_xT[tt]])
        P.dma("sp", C.gains, d["gains"], writes=[b_const])
        P.op("dve", I("tensor_scalar", out=C.gains, in0=C.gains, scalar1=32.0, scalar2=None, op0=ALU.mult),
             reads=[b_const], writes=[b_const])
        P.op("dve", I("memset", C.eps1024, 1024.0 * EPS), writes=[b_const])
        P.op("dve", I("memset", C.ones_bf, 1.0), writes=[b_const])
        P.op("dve", I("memset", C.eps512, 512.0 * EPS), writes=[b_const])
        P.op("dve", I("memset", C.one_c, 1.0), writes=[b_const])
        P.op("dve", I("memset", C.tiny, 1e-30), writes=[b_const])
        P.dma("sp", C.arena[:, OFF_CONST + ngc + 8 + 64 + 64:OFF_CONST + ngc + 8 + 64 + 64 + 384], d["cst"][:, 0:384], writes=[b_const])
        tmp_id = fview(C, OFF_PH, 128)
        P.dma("sp", tmp_id, d["cst"][:, 384:512], writes=[b_const])
        P.op("dve", I("tensor_copy", out=C.ident_bf, in_=tmp_id), reads=[b_const], writes=[b_const])
        P.dma("sp", C.vecs.rearrange("p l v -> p (l v)"), d["vecs"].rearrange("p l v -> p (l v)"), writes=[b_const])
        P.dma("sp", C.rows.rearrange("p l v -> p (l v)"), d["rows"].partition_broadcast(128), writes=[b_const])
        s512 = math.sqrt(512.0)
        P.op("dve", I("tensor_scalar", out=C.vecs[:, :, V_SG:V_SG + 12], in0=C.vecs[:, :, V_SG:V_SG + 12], scalar1=s512,
                                              scalar2=None, op0=ALU.mult), reads=[b_const], writes=[b_const])
        P.barrier()
        if "mix" in stages or "nsa" in stages or "nsatab" in stages:
            nsa_tables(P, C, d)

        for l in range(n_layers):
            g0 = l * NG_PER_LAYER
            if "ffn1" in stages:
                ffn_stage(P, C, d["f1_win"], d["f1_wout"], l, g0 + 0)
            if "mix" in stages or "ssd" in stages or "nsa" in stages:
                rmsnorm_T(P, C, g0 + 8)
                P.barrier()
            if "mix" in stages or "nsa" in stages:
                nsa_phase(P, C, d, l, stop=C.nsa_stop)
            if "mix" in stages or "ssd" in stages:
                ssd_phase(P, C, d, l)
            if "ffn2" in stages:
                ffn_stage(P, C, d["f2_win"], d["f2_wout"], l, g0 + 16)
            if "ple" in stages:
                ple_stage(P, C, d, l, g0 + 24)
        if final:
            final_norm_store(P, C, L * NG_PER_LAYER, outT)
        else:
            for tt in range(4):
                ts = slice(tt * 512, (tt + 1) * 512)
                P.dma("sp", outT[:, :, ts], C.xT[:, :, ts], reads=[C.b_xT[tt]])
        P.emit()
    return nc


def _fm(v):
    return np.ascontiguousarray(v.reshape(-1, 128).T)


def prep_shared(inp):
    L = DEPTH
    sh = {}
    g = np.zeros((128, L * NG_PER_LAYER + 8), np.float32)
    for l in range(L):
        for k, nm in enumerate(("ffn1_norm", "mix_norm", "ffn2_norm", "ple_norm")):
            g[:, l * NG_PER_LAYER + k * 8:l * NG_PER_LAYER + k * 8 + 8] = _fm(np.asarray(inp[nm][l]))
    g[:, L * NG_PER_LAYER:] = _fm(np.asarray(inp["final_norm"]))
    sh["gains"] = g
    for pre, a, b in (("f1", "ffn1_w_in", "ffn1_w_out"), ("f2", "ffn2_w_in", "ffn2_w_out")):
        wi = np.asarray(inp[a])
        wi = wi.reshape(L, 8, 128, 2, NJ, 128)
        sh[pre + "_win"] = np.ascontiguousarray(wi.transpose(0, 4, 2, 3, 1, 5)).reshape(L, NJ, 128, 2048)
        wo = np.asarray(inp[b])
        wo = wo.reshape(L, NJ, 128, 8, 128)
        sh[pre + "_wout"] = np.ascontiguousarray(wo.transpose(0, 3, 2, 1, 4)).reshape(L, 8, 128, D_FF)
    W = np.asarray(inp["w_mix_in"])
    cols = []
    for pr in range(4):
        cols.append(np.arange(pr * 128, (pr + 1) * 128))
    for base in (768, 1024):
        for g in range(2):
            c = np.arange(base + g * 64, base + (g + 1) * 64)
            cols.append(np.concatenate([c, c]))
    cols.append(np.arange(512, 640))
    cols.append(np.arange(640, 768))
    for c in range(8):
        cols.append(np.arange(1304 + c * 128, 1304 + (c + 1) * 128))
    for c in range(12):
        cols.append(np.arange(2328 + c * 128, 2328 + (c + 1) * 128))
    cols = np.stack(cols, 0)
    Wk = W.reshape(L, 8, 128, 3880)
    wfm = Wk[:, :, :, cols]
    sh["wfm"] = np.ascontiguousarray(wfm.transpose(0, 3, 2, 1, 4)).reshape(L, NFM, 128, 1024)
    sh["wdt"] = np.ascontiguousarray(Wk[:, :, :, 3864:3880].transpose(0, 2, 1, 3)).reshape(L, 128, 128)
    wo = np.asarray(inp["w_mix_out"]).reshape(L, 12, 128, 8, 128)
    sh["wmo"] = np.ascontiguousarray(wo.transpose(0, 3, 2, 1, 4)).reshape(L, 8, 128, 1536)
    vecs = np.zeros((128, L, NVEC), np.float32)
    for l in range(L):
        cw = np.asarray(inp["conv_w"][l])
        vecs[:, l, V_CW:V_CW + 48] = cw.reshape(4, 12, 128).transpose(2, 1, 0).reshape(128, 48)
        vecs[:, l, V_CB:V_CB + 12] = np.asarray(inp["conv_b"][l]).reshape(12, 128).T
        vecs[:, l, V_SG:V_SG + 8] = np.asarray(inp["ssm_out_norm"][l]).reshape(8, 128).T
        vecs[:, l, V_NG:V_NG + 4] = np.asarray(inp["nsa_out_norm"][l]).reshape(4, 128).T
        vecs[:, l, V_B1:V_B1 + 4] = np.asarray(inp["cmp_b1"][l]).reshape(4, 128).T
    sh["vecs"] = vecs
    rows = np.zeros((1, L * 48), np.float32)
    for l in range(L):
        rows[0, l * 48:l * 48 + 16] = np.asarray(inp["dt_bias"][l])
        rows[0, l * 48 + 16:l * 48 + 32] = np.asarray(inp["a_log"][l])
        rows[0, l * 48 + 32:l * 48 + 48] = np.asarray(inp["d_skip"][l])
    sh["rows"] = rows
    ii = np.arange(128)
    cst = np.zeros((128, 512), np.float32)
    cst[:, 0:128] = (ii[:, None] <= ii[None, :])
    cst[:, 128:256] = (ii[:, None] > ii[None, :])
    cst[:, 256:384] = 1.0
    cst[:, 384:512] = np.eye(128)
    sh["cst"] = cst
    sh["rel_table"] = np.ascontiguousarray(np.asarray(inp["rel_table"], np.float32))
    def onehot(dvals, validm):
        o = np.zeros((33, len(dvals)), np.float32)
        bk = t5_bucket_np(dvals)
        for n in range(len(dvals)):
            if validm[n]:
                o[bk[n], n] = 1.0
            else:
                o[32, n] = 1.0
        return o
    dA = np.arange(383) - 127
    dB = np.arange(255) + 385
    dC = np.arange(LC) - 2047
    sh["oht"] = np.concatenate([onehot(dA, dA >= 0), onehot(dB, dB < 512), onehot(dC, dC >= 0)], axis=1)
    kk = np.arange(SEQ)
    sh["ind"] = (kk[None, :] // 64 == np.arange(32)[:, None]).astype(np.float32)
    cc = np.arange(127)
    nn = np.arange(32)
    sh["ovl"] = ((16 * cc[:, None] <= 64 * nn[None, :] + 63) & (16 * cc[:, None] + 31 >= 64 * nn[None, :])).astype(np.float32)
    t = (np.arange(16)[None, :] * 128 + np.arange(128)[:, None])
    cur = (t // 64)[:, :, None]
    blk = nn[None, None, :]
    vld = (blk <= cur)
    forced = ((blk == 0) | (blk == cur) | (blk == cur - 1))
    sh["valid"] = vld.astype(np.float32).reshape(128, 512)
    sh["addm"] = np.where(vld, np.where(forced, 1e4, 0.0), -1e4).astype(np.float32).reshape(128, 512)
    sh["b2v"] = np.ascontiguousarray(np.asarray(inp["cmp_b2"])[:, 1, :])
    tcols = np.concatenate([np.arange(896, 1024), np.arange(1152, 1280), np.arange(1280, 1304)])
    sh["wv"] = np.ascontiguousarray(Wk[:, :, :, tcols].transpose(0, 2, 1, 3)).reshape(L, 128, 2240)
    w1 = np.asarray(inp["cmp_w1"]).reshape(L, 2, 32, 64, 256)
    sh["cw1"] = np.ascontiguousarray(w1.transpose(0, 1, 3, 2, 4)).reshape(L, 2, 64, 8192)
    sh["cpos"] = np.ascontiguousarray(np.asarray(inp["cmp_pos"]).transpose(0, 1, 3, 2))
    w2 = np.asarray(inp["cmp_w2"]).reshape(L, 2, 2, 128, 64)
    w2k = w2[:, 0].transpose(0, 2, 1, 3)
    sh["cw2k"] = np.ascontiguousarray(np.concatenate([w2k, w2k], axis=-1)).reshape(L, 128, 256)
    sh["cw2v"] = np.ascontiguousarray(w2[:, 1].transpose(0, 2, 1, 3)).reshape(L, 128, 128)
    b2k = np.asarray(inp["cmp_b2"])[:, 0, :]
    for l in range(L):
        sh["vecs"][:, l, V_B2K] = np.concatenate([b2k[l], b2k[l]])
    wg = np.asarray(inp["ple_gate_w"]).reshape(L, 8, 128, 8, 128)
    sh["wg"] = np.ascontiguousarray(wg.transpose(0, 3, 2, 1, 4)).reshape(L, 8, 128, 1024)
    wp = np.asarray(inp["ple_proj_w"]).reshape(L, 2, 128, 8, 128)
    sh["wp"] = np.ascontiguousarray(wp.transpose(0, 2, 3, 1, 4)).reshape(L, 128, 2048)
    return sh


def prep_core(inp, b):
    x = np.asarray(inp["x"][b])
    xT = np.ascontiguousarray(x.T.reshape(8, 128, SEQ).transpose(1, 0, 2))
    p = np.asarray(inp["p"][:, b])
    pT = np.ascontiguousarray(p.transpose(0, 2, 1).reshape(DEPTH, 2, 128, SEQ).transpose(0, 2, 1, 3))
    return {"xT": xT, "pT": pT}


_NC_CACHE = {}


def kernel(**inputs):
    key = "full"
    if key not in _NC_CACHE:
        _NC_CACHE[key] = build_nc()
    nc = _NC_CACHE[key]
    sh = prep_shared(inputs)
    in_maps = []
    for b in range(8):
        m = dict(sh)
        m.update(prep_core(inputs, b))
        in_maps.append(m)
    res = run_bass_kernel_spmd(nc, in_maps, core_ids=list(range(8)))
    outs = []
    for b in range(8):
        oT = np.asarray(res.results[b]["outT"])
        outs.append(oT.transpose(2, 1, 0).reshape(SEQ, D_MODEL))
    return np.stack(outs, 0).astype(np.float32)
```

```python
import math
from contextlib import ExitStack
import numpy as np
import concourse.bass as bass
import concourse.mybir as mybir
from concourse.bass_utils import run_bass_kernel_spmd

F32 = mybir.dt.float32
BF16 = mybir.dt.bfloat16
AF = mybir.ActivationFunctionType
ALU = mybir.AluOpType
AX = mybir.AxisListType

ENGS = ("pe", "act", "dve", "pool", "sp")

D_MODEL = 1024
SEQ = 2048
DEPTH = 4
D_FF = 2816
NJ = D_FF // 128
EPS = 1e-6


def I(name, *args, **kw):
    return lambda e: getattr(e, name)(*args, **kw)


class Buf:
    __slots__ = ("name", "w", "r")

    def __init__(self, name=""):
        self.name = name
        self.w = None
        self.r = []


class Prog:
    def __init__(self, nc, n_dma_sems=48):
        self.nc = nc
        self.ops = {e: [] for e in ENGS}
        self.waited_eng = {e: {} for e in ENGS}
        self.waited_dma = {e: {} for e in ENGS}
        self.n_dma_sems = n_dma_sems
        self.dma_val = [0] * n_dma_sems
        self.dma_next = {"pool": 0, "sp": 0}
        self.half = n_dma_sems // 2

    def _need(self, eng, tok, waits):
        if tok is None:
            return
        if tok[0] == "eng":
            _, src, idx = tok
            if src == eng:
                return
            cur = self.waited_eng[eng].get(src, -1)
            if idx <= cur:
                return
            self.waited_eng[eng][src] = idx
            self.ops[src][idx]["sig"] = True
            waits.append(tok)
        else:
            _, s, val = tok
            cur = self.waited_dma[eng].get(s, 0)
            if val <= cur:
                return
            self.waited_dma[eng][s] = val
            waits.append(tok)

    def _need_same(self, eng, tok, waits):
        _, src, idx = tok
        cur = self.waited_eng[eng].get("self", -1)
        if idx <= cur:
            return
        self.waited_eng[eng]["self"] = idx
        self.ops[src][idx]["sig"] = True
        waits.append(tok)

    def _deps(self, eng, reads, writes, same_raw):
        waits = []
        for b in reads:
            if b.w is not None:
                if b.w[0] == "eng" and b.w[1] == eng:
                    if same_raw:
                        self._need_same(eng, b.w, waits)
                else:
                    self._need(eng, b.w, waits)
        for b in writes:
            if b.w is not None:
                if b.w[0] == "eng" and b.w[1] == eng:
                    if same_raw:
                        self._need_same(eng, b.w, waits)
                else:
                    self._need(eng, b.w, waits)
            for t in b.r:
                self._need(eng, t, waits)
        return waits

    def _record(self, tok, reads, writes):
        for b in writes:
            b.w = tok
            b.r = []
        for b in reads:
            if b in writes:
                continue
            b.r = [t for t in b.r if not (t[0] == tok[0] and t[1] == tok[1])]
            b.r.append(tok)

    def op(self, eng, fn, reads=(), writes=()):
        reads = [b for b in reads if b is not None]
        writes = [b for b in writes if b is not None]
        waits = self._deps(eng, reads, writes, same_raw=(eng in ("act", "dve", "pool")))
        idx = len(self.ops[eng])
        self.ops[eng].append({"fn": fn, "waits": waits, "sig": False, "dma": None})
        tok = ("eng", eng, idx)
        self._record(tok, reads, writes)
        return tok

    def dma(self, q, out_ap, in_ap, reads=(), writes=(), **kw):
        reads = [b for b in reads if b is not None]
        writes = [b for b in writes if b is not None]
        waits = self._deps(q, reads, writes, same_raw=False)
        for b in reads:
            if b.w is not None and b.w[0] == "eng" and b.w[1] == q:
                self._need_same(q, b.w, waits)
        for b in writes:
            for t in ([b.w] if b.w is not None else []) + b.r:
                if t[0] == "eng" and t[1] == q:
                    self._need_same(q, t, waits)
        s = self.dma_next[q] + (0 if q == "pool" else self.half)
        self.dma_next[q] = (self.dma_next[q] + 1) % self.half
        if self.dma_val[s] > 0:
            self._need(q, ("dma", s, self.dma_val[s]), waits)
        self.dma_val[s] += 16
        tok = ("dma", s, self.dma_val[s])

        def fn(e, out_ap=out_ap, in_ap=in_ap, kw=kw):
            return e.dma_start(out=out_ap, in_=in_ap, **kw)
        self.ops[q].append({"fn": fn, "waits": waits, "sig": False, "dma": s})
        self._record(tok, reads, writes)
        return tok

    def barrier(self):
        toks = []
        for e in ENGS:
            for i in range(len(self.ops[e]) - 1, -1, -1):
                if self.ops[e][i]["fn"] is not None and self.ops[e][i]["dma"] is None:
                    toks.append(("eng", e, i))
                    break
        for s in range(self.n_dma_sems):
            if self.dma_val[s] > 0:
                toks.append(("dma", s, self.dma_val[s]))
        for e in ENGS:
            waits = []
            for t in toks:
                self._need(e, t, waits)
            if waits:
                self.ops[e].append({"fn": None, "waits": waits, "sig": False, "dma": None})

    def emit(self):
        nc = self.nc
        self.barrier()
        with ExitStack() as st:
            sems = {e: st.enter_context(nc.semaphore("s_" + e)) for e in ENGS}
            dsems = [st.enter_context(nc.semaphore("d_%d" % i)) for i in range(self.n_dma_sems)]
            cum = {}
            for e in ENGS:
                c = 0
                arr = []
                for o in self.ops[e]:
                    if o["sig"]:
                        c += 1
                    arr.append(c)
                cum[e] = arr
            block = st.enter_context(nc.Block())

            def make(e):
                def body(eng):
                    for o in self.ops[e]:
                        for t in o["waits"]:
                            if t[0] == "eng":
                                eng.wait_ge(sems[t[1]], cum[t[1]][t[2]])
                            else:
                                eng.wait_ge(dsems[t[1]], t[2])
                        if o["fn"] is None:
                            continue
                        inst = o["fn"](eng)
                        if o["dma"] is not None:
                            inst.then_inc(dsems[o["dma"]], 16)
                        elif o["sig"]:
                            inst.then_inc(sems[e], 1)
                return body
            block.tensor(make("pe"))
            block.scalar(make("act"))
            block.vector(make("dve"))
            block.gpsimd(make("pool"))
            block.sync(make("sp"))
        return nc


class Ctx:
    dbg = False
    nsa_stop = 99

    def dump(self, P, name, ap, reads):
        if not self.dbg:
            return
        shp = list(ap.shape)
        t = self.nc.dram_tensor("dbg_" + name, shp, F32, kind="ExternalOutput").ap()
        P.dma("pool", t, ap, reads=reads)


ARENA_WORDS = 53000
OFF_XT = 0
OFF_XN = 16384
OFF_CONST = 24576
OFF_PH = 27648


def fview(C, off, n):
    return C.arena[:, off:off + n]


def bview(C, off, nwords):
    return C.arena[:, off:off + nwords].bitcast(BF16)


def rmsnorm_T(P, C, gidx):
    ph = OFF_PH + 18432
    sq = bview(C, ph, 2048).rearrange("p (c t) -> p c t", c=8)
    rstd = fview(C, ph + 2048, 2048)
    for tt in range(4):
        ts = slice(tt * 512, (tt + 1) * 512)
        P.op("act", I("activation", out=sq, in_=C.xT[:, :, ts], func=AF.Square),
             reads=[C.b_xT[tt]], writes=[C.b_sq])
        for c in range(8):
            P.op("pe", I("matmul", C.ps[:, tt, :], lhsT=C.ones_bf, rhs=sq[:, c, :],
                                                      start=(c == 0), stop=(c == 7)),
                 reads=[C.b_sq], writes=[C.b_ps[tt]])
    psall = C.ps[:, 0:4, :].rearrange("p a t -> p (a t)")
    P.op("act", I("activation", out=rstd, in_=psall, func=AF.Ln, bias=C.eps1024[:, 0:1], scale=1.0),
         reads=C.b_ps[0:4], writes=[C.b_rstd])
    P.op("act", I("activation", out=rstd, in_=rstd, func=AF.Exp, scale=-0.5),
         reads=[C.b_rstd], writes=[C.b_rstd])
    for tt in range(4):
        ts = slice(tt * 512, (tt + 1) * 512)
        for c in range(8):
            P.op("dve", I("scalar_tensor_tensor",
                out=C.xnT[:, c, ts], in0=C.xT[:, c, ts], scalar=C.gains[:, gidx + c:gidx + c + 1],
                in1=rstd[:, ts], op0=ALU.mult, op1=ALU.mult),
                reads=[C.b_xT[tt], C.b_rstd], writes=[C.b_xn[tt]])


def ffn_stage(P, C, win_d, wout_d, l, gidx):
    rmsnorm_T(P, C, gidx)
    ph = OFF_PH
    hT = bview(C, ph, 11264).rearrange("p (j t) -> p j t", j=NJ)
    win = [bview(C, ph + 11264 + i * 1024, 1024).rearrange("p (g k m) -> p g k m", g=2, k=8) for i in range(3)]
    wout = [bview(C, ph + 11264 + 3072 + i * 1408, 1408).rearrange("p (j m) -> p j m", j=NJ) for i in range(2)]
    sg = [fview(C, ph + 11264 + 3072 + 2816 + i * 512, 512) for i in range(2)]
    b_win = [Buf("win%d" % i) for i in range(3)]
    b_wout = [Buf("wout%d" % i) for i in range(2)]
    b_sg = [Buf("sg%d" % i) for i in range(2)]
    b_h = [Buf("h%d" % j) for j in range(NJ)]
    step = 0
    for hf in range(2):
        for j in range(NJ):
            s = (hf * NJ + j) % 3
            P.dma("pool", win[s].rearrange("p g k m -> p (g k m)"), win_d[l, j], writes=[b_win[s]])
            for t2 in range(2):
                tt = hf * 2 + t2
                ts = slice(tt * 512, (tt + 1) * 512)
                pg, pu = step % 2, 2 + step % 2
                for g, pb in ((0, pg), (1, pu)):
                    for kc in range(8):
                        P.op("pe", I("matmul",
                            C.ps[:, pb, :], lhsT=win[s][:, g, kc, :], rhs=C.xnT[:, kc, ts],
                            start=(kc == 0), stop=(kc == 7)),
                            reads=[b_win[s], C.b_xn[tt]], writes=[C.b_ps[pb]])
                k = step % 2
                P.op("act", I("activation", out=sg[k], in_=C.ps[:, pg, :], func=AF.Silu),
                     reads=[C.b_ps[pg]], writes=[b_sg[k]])
                P.op("dve", I("tensor_tensor",
                    out=hT[:, j, t2 * 512:(t2 + 1) * 512], in0=sg[k], in1=C.ps[:, pu, :], op=ALU.mult),
                    reads=[b_sg[k], C.b_ps[pu]], writes=[b_h[j]])
                step += 1
        for m in range(8):
            s = (hf * 8 + m) % 2
            P.dma("pool", wout[s][:, 0:11, :].rearrange("p j m -> p (j m)"), wout_d[l, m][:, 0:1408],
                  writes=[b_wout[s]])
            P.dma("pool", wout[s][:, 11:22, :].rearrange("p j m -> p (j m)"), wout_d[l, m][:, 1408:2816],
                  writes=[b_wout[s]])
            for t2 in range(2):
                tt = hf * 2 + t2
                ts = slice(tt * 512, (tt + 1) * 512)
                pb = 4 + step % 2
                for j in range(NJ):
                    P.op("pe", I("matmul",
                        C.ps[:, pb, :], lhsT=wout[s][:, j, :], rhs=hT[:, j, t2 * 512:(t2 + 1) * 512],
                        start=(j == 0), stop=(j == NJ - 1)),
                        reads=[b_wout[s], b_h[j]], writes=[C.b_ps[pb]])
                P.op("dve", I("scalar_tensor_tensor",
                    out=C.xT[:, m, ts], in0=C.ps[:, pb, :], scalar=0.5, in1=C.xT[:, m, ts],
                    op0=ALU.mult, op1=ALU.add),
                    reads=[C.b_ps[pb], C.b_xT[tt]], writes=[C.b_xT[tt]])
                step += 1
    P.barrier()


class Alloc:
    def __init__(self, C, base=None):
        self.C = C
        self.o = OFF_PH if base is None else base

    def f(self, n):
        r = fview(self.C, self.o, n)
        self.o += n
        assert self.o <= ARENA_WORDS, self.o
        return r

    def b(self, nwords):
        r = bview(self.C, self.o, nwords)
        self.o += nwords
        assert self.o <= ARENA_WORDS, self.o
        return r


NFM = 30
FM_Z = 10
FM_XBC = 18
NVEC = 12 * 4 + 12 + 8 + 4 + 4 + 1
V_CW, V_CB, V_SG, V_NG, V_B1, V_B2K = 0, 48, 60, 68, 72, 76


def ssd_phase(P, C, d, l):
    A = Alloc(C)
    zT = A.b(2048).rearrange("p (c t) -> p c t", c=8)
    xbcT = A.b(3072).rearrange("p (c t) -> p c t", c=12)
    yT = A.b(2048).rearrange("p (c t) -> p c t", c=8)
    gz = A.b(2048).rearrange("p (c t) -> p c t", c=8)
    wring = [A.b(512).rearrange("p (k m) -> p k m", k=8) for _ in range(3)]
    raw = [A.f(520) for _ in range(2)]
    t1 = [A.f(512) for _ in range(2)]
    rawhist = A.f(40)[:, 0:36].rearrange("p (c j) -> p c j", c=12)
    E = A.f(1024)
    daU = A.f(1024)
    MT = A.b(512).rearrange("p (i l) -> p i l", i=8)
    GU = A.f(256).rearrange("p (g l) -> p g l", g=2)
    xtok = A.b(512)
    xdt = A.b(512)
    xdtdec = A.b(512)
    Btok = A.b(128).rearrange("p (g n) -> p g n", g=2)
    HT = A.f(1024)
    HTbf = A.b(512)
    tmpA = A.f(1024)
    tmpB = A.f(1024)
    xD = A.f(1024)
    ytok = A.b(512)
    sm = A.f(256)
    wdt = A.b(64).rearrange("p (k n) -> p k n", k=8)
    dtr, dt, da, cs_sb, ecs, edte, coef2, etot = [sm[:, i * 16:(i + 1) * 16] for i in range(8)]
    arow = sm[:, 128:144]
    rstd2 = E.rearrange("p (g t) -> p g t", g=2)
    mixT = zT
    sq = yT
    vec = C.vecs[:, l, :]
    rows = C.rows[:, l, :]
    dtb_row, alog_row, D_row = rows[:, 0:16], rows[:, 16:32], rows[:, 32:48]

    b = {n: Buf(n) for n in ("zT xbcT yT gz E daU MT GU xtok xdt xdtdec Btok HT HTbf tmpA tmpB xD ytok "
                             "dtr dt da cs ecs edte coef2 etot arow wdt rawhist mixT rstd2").split()}
    b_w = [Buf("w%d" % i) for i in range(3)]
    b_raw = [Buf("raw%d" % i) for i in range(2)]
    b_t1 = [Buf("t1%d" % i) for i in range(2)]
    bps = C.b_ps
    psb = lambda k: C.ps[:, k, :]
    psbf = lambda k: C.ps[:, k, :].bitcast(BF16)
    wcount = [0]

    def wload(src):
        s = wcount[0] % 3
        wcount[0] += 1
        P.dma("pool", wring[s].rearrange("p k m -> p (k m)"), src, writes=[b_w[s]])
        return s

    P.op("act", I("activation", out=arow, in_=alog_row, func=AF.Exp), reads=[C.b_const], writes=[b["arow"]])
    P.op("dve", I("tensor_scalar", out=arow, in0=arow, scalar1=-1.0, scalar2=None, op0=ALU.mult),
         reads=[b["arow"]], writes=[b["arow"]])
    P.op("pool", I("memset", HT, 0.0), writes=[b["HT"]])
    P.op("pool", I("memset", HTbf, 0.0), writes=[b["HTbf"]])
    P.op("pool", I("memset", rawhist, 0.0), writes=[b["rawhist"]])
    P.dma("pool", wdt.rearrange("p k n -> p (k n)"), d["wdt"][l], writes=[b["wdt"]])

    step = [0]
    for G in range(4):
        gs = slice(G * 512, (G + 1) * 512)
        for c in range(8):
            s = wload(d["wfm"][l, FM_Z + c])
            pb = 6 + step[0] % 2
            step[0] += 1
            for kc in range(8):
                P.op("pe", I("matmul", psb(pb), lhsT=wring[s][:, kc, :], rhs=C.xnT[:, kc, gs],
                                                                start=(kc == 0), stop=(kc == 7)),
                     reads=[b_w[s], C.b_xn[G]], writes=[bps[pb]])
            P.op("act", I("activation", out=zT[:, c, :], in_=psb(pb), func=AF.Silu),
                 reads=[bps[pb]], writes=[b["zT"]])
        for ch in range(12):
            s = wload(d["wfm"][l, FM_XBC + ch])
            pb = 6 + step[0] % 2
            r = step[0] % 2
            step[0] += 1
            for kc in range(8):
                P.op("pe", I("matmul", psb(pb), lhsT=wring[s][:, kc, :], rhs=C.xnT[:, kc, gs],
                                                                start=(kc == 0), stop=(kc == 7)),
                     reads=[b_w[s], C.b_xn[G]], writes=[bps[pb]])
            P.op("dve", I("tensor_copy", out=raw[r][:, 0:3], in_=rawhist[:, ch, :]),
                 reads=[b["rawhist"]], writes=[b_raw[r]])
            P.op("act", I("activation", out=raw[r][:, 3:515], in_=psb(pb), func=AF.Copy),
                 reads=[bps[pb]], writes=[b_raw[r]])
            P.op("dve", I("tensor_copy", out=rawhist[:, ch, :], in_=raw[r][:, 512:515]),
                 reads=[b_raw[r]], writes=[b["rawhist"]])
            cw = lambda j, ch=ch: vec[:, V_CW + ch * 4 + j:V_CW + ch * 4 + j + 1]
            P.op("dve", I("tensor_scalar", out=t1[r], in0=raw[r][:, 0:512], scalar1=cw(0), scalar2=None,
                                                             op0=ALU.mult), reads=[b_raw[r], C.b_const], writes=[b_t1[r]])
            for j in range(1, 4):
                P.op("dve", I("scalar_tensor_tensor",
                    out=t1[r], in0=raw[r][:, j:j + 512], scalar=cw(j), in1=t1[r], op0=ALU.mult, op1=ALU.add),
                    reads=[b_raw[r], b_t1[r]], writes=[b_t1[r]])
            P.op("act", I("activation", out=xbcT[:, ch, :], in_=t1[r], func=AF.Silu,
                                                          bias=vec[:, V_CB + ch:V_CB + ch + 1]),
                 reads=[b_t1[r]], writes=[b["xbcT"]])
        for c4 in range(4):
            ti = G * 4 + c4
            cs_ = slice(c4 * 128, (c4 + 1) * 128)
            tk = slice(ti * 128, (ti + 1) * 128)
            for kc in range(8):
                P.op("pe", I("matmul", C.ps[:, 0, 0:16], lhsT=C.xnT[:, kc, tk], rhs=wdt[:, kc, :],
                                                            start=(kc == 0), stop=(kc == 7)),
                     reads=[C.b_xn[G], b["wdt"]], writes=[bps[0]])
            P.op("dve", I("tensor_tensor", out=dtr, in0=C.ps[:, 0, 0:16], in1=dtb_row, op=ALU.add),
                 reads=[bps[0], C.b_const], writes=[b["dtr"]])
            P.op("act", I("activation", out=dtr, in_=dtr, func=AF.Exp), reads=[b["dtr"]], writes=[b["dtr"]])
            P.op("act", I("activation", out=dt, in_=dtr, func=AF.Ln, bias=C.one_c[:, 0:1], scale=1.0),
                 reads=[b["dtr"]], writes=[b["dt"]])
            P.op("dve", I("tensor_tensor", out=da, in0=dt, in1=arow, op=ALU.mult),
                 reads=[b["dt"], b["arow"]], writes=[b["da"]])
            P.op("pe", I("matmul", C.ps[:, 0, 16:32], lhsT=C.U_f, rhs=da, start=True, stop=True),
                 reads=[b["da"]], writes=[bps[0]])
            P.op("pe", I("matmul", C.ps[:, 0, 32:48], lhsT=C.SL_f, rhs=da, start=True, stop=True),
                 reads=[b["da"]], writes=[bps[0]])
            P.op("pe", I("matmul", C.ps[:, 0, 48:64], lhsT=C.ones_f, rhs=da, start=True, stop=True),
                 reads=[b["da"]], writes=[bps[0]])
            P.op("act", I("activation", out=ecs, in_=C.ps[:, 0, 16:32], func=AF.Exp), reads=[bps[0]], writes=[b["ecs"]])
            P.op("dve", I("tensor_copy", out=cs_sb, in_=C.ps[:, 0, 16:32]), reads=[bps[0]], writes=[b["cs"]])
            P.op("act", I("activation", out=edte, in_=C.ps[:, 0, 32:48], func=AF.Exp), reads=[bps[0]], writes=[b["edte"]])
            P.op("act", I("activation", out=etot, in_=C.ps[:, 0, 48:64], func=AF.Exp), reads=[bps[0]], writes=[b["etot"]])
            P.op("dve", I("tensor_tensor", out=coef2, in0=dt, in1=edte, op=ALU.mult),
                 reads=[b["dt"], b["edte"]], writes=[b["coef2"]])
            if ti == 0:
                C.dump(P, "dt", dt, [b["dt"]])
                C.dump(P, "cs", cs_sb, [b["cs"]])
                C.dump(P, "xbcT", xbcT, [b["xbcT"]])
                C.dump(P, "zT", zT, [b["zT"]])
            for ch in range(8):
                P.op("pe", I("transpose", psbf(1)[:, ch * 128:(ch + 1) * 128], xbcT[:, ch, cs_], C.ident_bf),
                     reads=[b["xbcT"]], writes=[bps[1]])
            P.op("act", I("activation", out=xtok, in_=psbf(1), func=AF.Copy), reads=[bps[1]], writes=[b["xtok"]])
            for g in range(2):
                P.op("pe", I("transpose", psbf(0)[:, 768 + g * 128:768 + (g + 1) * 128], xbcT[:, 8 + g, cs_], C.ident_bf),
                     reads=[b["xbcT"]], writes=[bps[0]])
            P.op("dve", I("tensor_copy", out=Btok.rearrange("p g n -> p (g n)"), in_=psbf(0)[:, 768:1024]),
                 reads=[bps[0]], writes=[b["Btok"]])
            for g in range(2):
                P.op("pe", I("matmul", C.ps[:, 0, 128 + g * 128:128 + (g + 1) * 128], lhsT=xbcT[:, 8 + g, cs_],
                                                            rhs=xbcT[:, 10 + g, cs_], start=True, stop=True),
                     reads=[b["xbcT"]], writes=[bps[0]])
            P.op("dve", I("tensor_tensor", out=GU, in0=C.ps[:, 0, 128:384].rearrange("p (g l) -> p g l", g=2),
                                                  in1=C.U_f.unsqueeze(1).to_broadcast([128, 2, 128]), op=ALU.mult),
                 reads=[bps[0]], writes=[b["GU"]])
            P.op("dve", I("tensor_tensor", out=xdt.rearrange("p (h q) -> p h q", h=16),
                                                  in0=xtok.rearrange("p (h q) -> p h q", h=16),
                                                  in1=dt.unsqueeze(2).to_broadcast([128, 16, 64]), op=ALU.mult),
                 reads=[b["xtok"], b["dt"]], writes=[b["xdt"]])
            P.op("dve", I("tensor_tensor", out=xdtdec.rearrange("p (h q) -> p h q", h=16),
                                                   in0=xtok.rearrange("p (h q) -> p h q", h=16),
                                                   in1=coef2.unsqueeze(2).to_broadcast([128, 16, 64]), op=ALU.mult),
                 reads=[b["xtok"], b["coef2"]], writes=[b["xdtdec"]])
            for g in range(2):
                P.op("pe", I("matmul", psb(6 + g), lhsT=xbcT[:, 10 + g, cs_], rhs=HTbf[:, g * 512:(g + 1) * 512],
                                                            start=True, stop=True),
                     reads=[b["xbcT"], b["HTbf"]], writes=[bps[6 + g]])
            for hh in range(2):
                P.op("dve", I("tensor_tensor",
                    out=daU.rearrange("p (i l) -> p i l", i=8), in0=C.U_f.unsqueeze(1).to_broadcast([128, 8, 128]),
                    in1=da[:, hh * 8:(hh + 1) * 8].unsqueeze(2).to_broadcast([128, 8, 128]), op=ALU.mult),
                    reads=[b["da"]], writes=[b["daU"]])
                for k in range(2):
                    P.op("pe", I("matmul", psb(2 + k), lhsT=C.ones_f, rhs=daU[:, k * 512:(k + 1) * 512],
                                                       start=True, stop=True),
                         reads=[b["daU"]], writes=[bps[2 + k]])
                for i in range(8):
                    P.op("dve", I("tensor_scalar", out=E[:, i * 128:(i + 1) * 128], in0=C.ps[:, 2 + i // 4, (i % 4) * 128:(i % 4 + 1) * 128],
                                  scalar1=cs_sb[:, hh * 8 + i:hh * 8 + i + 1], scalar2=0.0, op0=ALU.subtract, op1=ALU.min),
                         reads=[bps[2 + i // 4], b["cs"]], writes=[b["E"]])
                P.op("act", I("activation", out=E, in_=E, func=AF.Exp), reads=[b["E"]], writes=[b["E"]])
                P.op("dve", I("tensor_tensor",
                    out=MT, in0=E.rearrange("p (i l) -> p i l", i=8),
                    in1=GU[:, hh, :].unsqueeze(1).to_broadcast([128, 8, 128]), op=ALU.mult),
                    reads=[b["E"], b["GU"]], writes=[b["MT"]])
                for i in range(8):
                    h = hh * 8 + i
                    P.op("pe", I("matmul", C.ps[:, 4 + hh, i * 64:(i + 1) * 64], lhsT=MT[:, i, :],
                                                                   rhs=xdt[:, h * 64:(h + 1) * 64], start=True, stop=True),
                         reads=[b["MT"], b["xdt"]], writes=[bps[4 + hh]])
            for g in range(2):
                P.op("pe", I("matmul", psb(2 + g), lhsT=Btok[:, g, :], rhs=xdtdec[:, g * 512:(g + 1) * 512],
                                                   start=True, stop=True),
                     reads=[b["Btok"], b["xdtdec"]], writes=[bps[2 + g]])
            P.op("dve", I("tensor_tensor", out=tmpB.rearrange("p (h q) -> p h q", h=16),
                                                  in0=C.ps[:, 6:8, :].rearrange("p a (i q) -> p (a i) q", q=64),
                                                  in1=ecs.unsqueeze(2).to_broadcast([128, 16, 64]), op=ALU.mult),
                 reads=[bps[6], bps[7], b["ecs"]], writes=[b["tmpB"]])
            P.op("dve", I("tensor_tensor", out=tmpB, in0=tmpB, in1=C.ps[:, 4:6, :].rearrange("p a t -> p (a t)"), op=ALU.add),
                 reads=[b["tmpB"], bps[4], bps[5]], writes=[b["tmpB"]])
            P.op("dve", I("tensor_tensor", out=xD.rearrange("p (h q) -> p h q", h=16),
                                                   in0=xtok.rearrange("p (h q) -> p h q", h=16),
                                                   in1=D_row.unsqueeze(2).to_broadcast([128, 16, 64]), op=ALU.mult),
                 reads=[b["xtok"], C.b_const], writes=[b["xD"]])
            P.op("dve", I("tensor_tensor", out=ytok, in0=tmpB, in1=xD, op=ALU.add),
                 reads=[b["tmpB"], b["xD"]], writes=[b["ytok"]])
            if ti == 0:
                C.dump(P, "xtok", xtok, [b["xtok"]])
                C.dump(P, "ytok", ytok, [b["ytok"]])
                C.dump(P, "MT", MT, [b["MT"]])
                C.dump(P, "GU", GU, [b["GU"]])
                C.dump(P, "tmpB", tmpB, [b["tmpB"]])
            P.op("dve", I("tensor_tensor", out=tmpA.rearrange("p (h q) -> p h q", h=16),
                                                  in0=HT.rearrange("p (h q) -> p h q", h=16),
                                                  in1=etot.unsqueeze(2).to_broadcast([128, 16, 64]), op=ALU.mult),
                 reads=[b["HT"], b["etot"]], writes=[b["tmpA"]])
            P.op("dve", I("tensor_tensor", out=HT, in0=tmpA, in1=C.ps[:, 2:4, :].rearrange("p a t -> p (a t)"), op=ALU.add),
                 reads=[b["tmpA"], bps[2], bps[3]], writes=[b["HT"]])
            P.op("act", I("activation", out=HTbf, in_=HT, func=AF.Copy), reads=[b["HT"]], writes=[b["HTbf"]])
            for ch in range(8):
                P.op("pe", I("transpose", psbf(1)[:, ch * 128:(ch + 1) * 128], ytok[:, ch * 128:(ch + 1) * 128], C.ident_bf),
                     reads=[b["ytok"]], writes=[bps[1]])
            P.op("act", I("activation", out=yT[:, :, cs_], in_=psbf(1).rearrange("p (c t) -> p c t", c=8), func=AF.Copy),
                 reads=[bps[1]], writes=[b["yT"]])
        P.op("dve", I("tensor_tensor", out=gz, in0=yT, in1=zT, op=ALU.mult), reads=[b["yT"], b["zT"]], writes=[b["gz"]])
        P.op("act", I("activation", out=sq, in_=gz, func=AF.Square), reads=[b["gz"]], writes=[b["yT"]])
        for sg in range(2):
            for k in range(4):
                P.op("pe", I("matmul", psb(2 + sg), lhsT=C.ones_bf, rhs=sq[:, sg * 4 + k, :],
                                                          start=(k == 0), stop=(k == 3)),
                     reads=[b["yT"]], writes=[bps[2 + sg]])
        P.op("act", I("activation", out=E, in_=C.ps[:, 2:4, :].rearrange("p a t -> p (a t)"), func=AF.Ln,
                                           bias=C.eps512[:, 0:1], scale=1.0),
             reads=[bps[2], bps[3]], writes=[b["E"]])
        P.op("act", I("activation", out=E, in_=E, func=AF.Exp, scale=-0.5), reads=[b["E"]], writes=[b["E"]])
        for ch in range(8):
            P.op("dve", I("scalar_tensor_tensor",
                out=mixT[:, ch, :], in0=gz[:, ch, :], scalar=vec[:, V_SG + ch:V_SG + ch + 1], in1=rstd2[:, ch // 4, :],
                op0=ALU.mult, op1=ALU.mult),
                reads=[b["gz"], b["E"], C.b_const], writes=[b["zT"]])
        if G == 0:
            C.dump(P, "mixT", mixT, [b["zT"]])
        for m in range(8):
            s = wload(d["wmo"][l, m][:, 512:1536])
            pb = 4 + m % 2
            for kc in range(8):
                P.op("pe", I("matmul", psb(pb), lhsT=wring[s][:, kc, :], rhs=mixT[:, kc, :],
                                                                start=(kc == 0), stop=(kc == 7)),
                     reads=[b_w[s], b["zT"]], writes=[bps[pb]])
            P.op("dve", I("tensor_tensor", out=C.xT[:, m, gs], in0=psb(pb), in1=C.xT[:, m, gs], op=ALU.add),
                 reads=[bps[pb], C.b_xT[G]], writes=[C.b_xT[G]])
    P.barrier()


NEG = -30000.0
LC = 4064


def t5_bucket_np(n):
    n = np.maximum(n, 0)
    nf = np.maximum(n, 16).astype(np.float32)
    large = 16 + (np.log(nf / np.float32(16)) / np.float32(math.log(128 / 16)) * np.float32(16)).astype(np.int32)
    return np.where(n < 16, n, np.minimum(large, 31))


def nsa_tables(P, C, d):
    A = Alloc(C)
    tab = A.f(8)
    tabx = A.f(1024).rearrange("p (h m) -> p h m", h=8)
    oht = A.f(383 + 255 + LC)
    wrows = [A.f(LC), A.f(LC)]
    bt = {n: Buf(n) for n in "tab tabx oht".split()}
    b_wrow = [Buf("wrow0"), Buf("wrow1")]
    b_scr = {(nm, h): Buf("scr%s%d" % (nm, h)) for nm in "ABC" for h in range(8)}
    P.dma("sp", tab[0:32, :], d["rel_table"], writes=[bt["tab"]])
    P.dma("sp", oht[0:33, :], d["oht"], writes=[bt["oht"]])
    P.dma("sp", C.tab31, d["rel_table"][31:32, :].partition_broadcast(128), writes=[C.b_const])
    P.op("dve", I("tensor_scalar", out=C.ntab31, in0=C.tab31, scalar1=-1.0, scalar2=None, op0=ALU.mult),
         reads=[C.b_const], writes=[C.b_const])
    P.op("dve", I("memset", tabx[32:33, :, :], NEG), writes=[bt["tabx"]])
    P.op("dve", I("tensor_copy", out=tabx[0:32, :, :], in_=tab[0:32, :].unsqueeze(2).to_broadcast([32, 8, 128])),
         reads=[bt["tab"]], writes=[bt["tabx"]])
    k = 0
    pk = 0
    for h in range(8):
        for (nm, o0, n, scr, rel) in (("A", 0, 383, d["scrA"], True), ("B", 383, 255, d["scrB"], True), ("C", 638, LC, d["scrC"], False)):
            wrow = wrows[k % 2]
            bw = b_wrow[k % 2]
            k += 1
            for c0 in range(0, n, 508):
                cn = min(508, n - c0)
                pb = pk % 4
                pk += 1
                P.op("pe", I("matmul", C.ps[:, pb, 0:cn], lhsT=tabx[0:33, h, :], rhs=oht[0:33, o0 + c0:o0 + c0 + cn], start=True, stop=True),
                     reads=[bt["tabx"], bt["oht"]], writes=[C.b_ps[pb]])
                if rel:
                    P.op("act", I("activation", out=wrow[:, c0:c0 + cn], in_=C.ps[:, pb, 0:cn], func=AF.Exp, bias=C.ntab31[:, h:h + 1], scale=1.0),
                         reads=[C.b_ps[pb], C.b_const], writes=[bw])
                else:
                    P.op("act", I("activation", out=wrow[:, c0:c0 + cn], in_=C.ps[:, pb, 0:cn], func=AF.Exp),
                         reads=[C.b_ps[pb]], writes=[bw])
            P.dma("sp", scr[h].rearrange("(p n) -> p n", p=128), wrow[:, 0:n], reads=[bw], writes=[b_scr[(nm, h)]])
        P.dma("pool", C.Dtab[:, h, 0:256], bass.AP(d["scrA"].tensor, h * 128 * 383 + 127, [[382, 128], [1, 256]]),
              reads=[b_scr[("A", h)]], writes=[C.b_Dtab[h]])
        P.dma("pool", C.Dtab[:, h, 256:384], bass.AP(d["scrB"].tensor, h * 128 * 255 + 127, [[254, 128], [1, 128]]),
              reads=[b_scr[("B", h)]], writes=[C.b_Dtab[h]])
    C.b_scrC = [b_scr[("C", h)] for h in range(8)]
    P.barrier()


def nsa_phase(P, C, d, l, stop=99):
    A = Alloc(C)
    ksT = A.b(2048).rearrange("p (g t) -> p g t", g=2)
    kwT = A.b(2048).rearrange("p (g t) -> p g t", g=2)
    vtok = A.b(2080).rearrange("p (t a d) -> p t a d", t=16, a=4)
    Wq = A.b(2048).rearrange("p (r k m) -> p r k m", r=4, k=8)
    Woa = A.b(2048).rearrange("p (o k m) -> p o k m", o=8, k=4)
    Ind = A.b(1024)
    ovl = A.b(16)
    valid = A.f(512).rearrange("p (t n) -> p t n", t=16)
    addm = A.f(512).rearrange("p (t n) -> p t n", t=16)
    kcmpT = A.b(128).rearrange("p (g c) -> p g c", g=2)
    vcmp = A.b(64).rearrange("p (g d) -> p g d", g=2)
    b2v = A.f(64)
    gts = A.f(384).rearrange("p (t n) -> p t n", t=16)
    base_q = A.o
    kcT = A.b(1024)
    vcT = A.b(1024)
    wvt = A.b(1120).rearrange("p (k n) -> p k n", k=8)
    w1 = A.b(4096)
    posT = A.b(16)
    w2k = A.b(128).rearrange("p (j m) -> p j m", j=2)
    w2v = A.b(64).rearrange("p (j m) -> p j m", j=2)
    hid = A.b(128).rearrange("p (j c) -> p j c", j=2)
    btmp = A.f(8)
    wring = [A.b(512).rearrange("p (k m) -> p k m", k=8) for _ in range(3)]
    A2 = Alloc(C, base_q)
    qTt = A2.b(1024).rearrange("p (r t) -> p r t", r=4)
    Ec = [A2.b(256) for _ in range(2)]
    b_Ec = [Buf("Ec0"), Buf("Ec1")]
    e32 = [A2.f(512) for _ in range(2)]
    pn_off = A2.o
    pn = A2.b(2048).rearrange("p (h t) -> p h t", h=8)
    pn_f32 = fview(C, pn_off, 2048).rearrange("p (j t) -> p j t", j=4)
    rZ = A2.f(512)
    negmT = A2.b(512).rearrange("p (g t) -> p g t", g=2)
    pexp = [A2.b(256) for _ in range(3)]
    oacc = A2.f(2048).rearrange("p (j f) -> p j f", j=4)
    on = A2.b(1024).rearrange("p (j f) -> p j f", j=4)
    mixT = A2.b(1024).rearrange("p (k t) -> p k t", k=4)
    sc = A2.f(256).rearrange("p (s n) -> p s n", s=8)
    mx = A2.f(64).rearrange("p (s n) -> p s n", s=8)
    negm = A2.b(128).rearrange("p (s n) -> p s n", s=8)
    coef = A2.f(8)
    ssq = A2.f(8)
    junk = C.arena[:, 0:0]
    vec = C.vecs[:, l, :]
    rows = C.rows[:, l, :]

    b = {n: Buf(n) for n in ("ksT kwT vtok Wq Woa cst kcmpT vcmp b2v gts kcT vcT wvt w1 posT w2 hid btmp qTt Ec pn rZ "
                             "negmT oacc on mixT sc mx negm coef ssq junk").split()}
    b_w = [Buf("w%d" % i) for i in range(3)]
    b_e32 = [Buf("e32%d" % i) for i in range(2)]
    b_pexp = [Buf("pexp%d" % i) for i in range(3)]
    bps = C.b_ps
    psb = lambda k: C.ps[:, k, :]
    psbf = lambda k: C.ps[:, k, :].bitcast(BF16)
    cnt = {"w": 0, "ev": 0, "pe": 0, "sb": 0}

    def wload(src):
        s = cnt["w"] % 3
        cnt["w"] += 1
        P.dma("pool", wring[s].rearrange("p k m -> p (k m)"), src, writes=[b_w[s]])
        return s

    def evac(out, in_, reads, writes, **kw):
        cnt["ev"] += 1
        if cnt["ev"] % 2:
            P.op("act", I("activation", out=out, in_=in_, func=AF.Copy, **kw), reads=reads, writes=writes)
        else:
            if "scale" in kw:
                P.op("dve", I("tensor_scalar", out=out, in0=in_, scalar1=kw["scale"], scalar2=None, op0=ALU.mult), reads=reads, writes=writes)
            else:
                P.op("dve", I("tensor_copy", out=out, in_=in_), reads=reads, writes=writes)

    P.dma("pool", Ind[0:32, :], d["ind"], writes=[b["cst"]])
    P.dma("pool", ovl[0:127, :], d["ovl"], writes=[b["cst"]])
    P.dma("sp", valid.rearrange("p t n -> p (t n)"), d["valid"], writes=[b["cst"]])
    P.dma("sp", addm.rearrange("p t n -> p (t n)"), d["addm"], writes=[b["cst"]])
    P.dma("sp", b2v, d["b2v"][l:l + 1, :].partition_broadcast(128), writes=[b["b2v"]])
    for pr in range(4):
        P.dma("pool", Wq[:, pr, :, :].rearrange("p k m -> p (k m)"), d["wfm"][l, pr], writes=[b["Wq"]])
    for m in range(8):
        P.dma("pool", Woa[:, m, :, :].rearrange("p k m -> p (k m)"), d["wmo"][l, m][:, 0:512], writes=[b["Woa"]])
    P.dma("pool", wvt[:, 0:4, :].rearrange("p k n -> p (k n)"), d["wv"][l][:, 0:1120], writes=[b["wvt"]])
    P.dma("pool", wvt[:, 4:8, :].rearrange("p k n -> p (k n)"), d["wv"][l][:, 1120:2240], writes=[b["wvt"]])
    P.op("pool", I("memset", vtok[:, :, :, 64:65], 1.0), writes=[b["vtok"]])
    for ci, dst, bn in ((4, ksT[:, 0, :], "ksT"), (5, ksT[:, 1, :], "ksT"), (6, kwT[:, 0, :], "kwT"), (7, kwT[:, 1, :], "kwT"),
                        (8, kcT, "kcT"), (9, vcT, "vcT")):
        s = wload(d["wfm"][l, ci])
        for tt in range(4):
            ts = slice(tt * 512, (tt + 1) * 512)
            pb = 6 + cnt["pe"] % 2
            cnt["pe"] += 1
            for kc in range(8):
                P.op("pe", I("matmul", psb(pb), lhsT=wring[s][:, kc, :], rhs=C.xnT[:, kc, ts], start=(kc == 0), stop=(kc == 7)),
                     reads=[b_w[s], C.b_xn[tt]], writes=[bps[pb]])
            evac(dst[:, ts], psb(pb), [bps[pb]], [b[bn]])
    for ti in range(16):
        tk = slice(ti * 128, (ti + 1) * 128)
        pb = 4 + ti % 2
        for kc in range(8):
            P.op("pe", I("matmul", C.ps[:, pb, 0:280], lhsT=C.xnT[:, kc, tk], rhs=wvt[:, kc, :], start=(kc == 0), stop=(kc == 7)),
                 reads=[b["wvt"], C.b_xn[ti // 4]], writes=[bps[pb]])
        evac(vtok[:, ti, :, 0:64], C.ps[:, pb, 0:256].rearrange("p (a d) -> p a d", a=4), [bps[pb]], [b["vtok"]])
        P.op("act", I("activation", out=gts[:, ti, :], in_=C.ps[:, pb, 256:280], func=AF.Sigmoid), reads=[bps[pb]], writes=[b["gts"]])
    if stop <= 1:
        P.barrier()
        return
    P.dma("pool", w2k.rearrange("p j m -> p (j m)"), d["cw2k"][l], writes=[b["w2"]])
    P.dma("pool", w2v.rearrange("p j m -> p (j m)"), d["cw2v"][l], writes=[b["w2"]])
    for x in range(2):
        src = kcT if x == 0 else vcT
        bsrc = b["kcT"] if x == 0 else b["vcT"]
        for half in range(2):
            for i in range(4):
                P.dma("pool", w1[half * 64:(half + 1) * 64, i * 2048:(i + 1) * 2048], d["cw1"][l, x][:, i * 2048:(i + 1) * 2048], writes=[b["w1"]])
            P.dma("pool", posT[half * 64:(half + 1) * 64, :], d["cpos"][l, x], writes=[b["posT"]])
        for g in range(2):
            hs = slice(g * 64, (g + 1) * 64)
            for jc in range(2):
                pb = 6 + cnt["pe"] % 2
                cnt["pe"] += 1
                for ll in range(32):
                    P.op("pe", I("matmul", C.ps[:, pb, 0:127], lhsT=w1[hs, ll * 256 + jc * 128:ll * 256 + (jc + 1) * 128],
                                 rhs=src[hs, ll:ll + 2017:16], start=(ll == 0), stop=(ll == 31)),
                         reads=[b["w1"], bsrc], writes=[bps[pb]])
                for ll in range(32):
                    P.op("pe", I("matmul", C.ps[:, pb, 128:129], lhsT=w1[hs, ll * 256 + jc * 128:ll * 256 + (jc + 1) * 128],
                                 rhs=posT[hs, ll:ll + 1], start=(ll == 0), stop=(ll == 31)),
                         reads=[b["w1"], b["posT"]], writes=[bps[pb]])
                P.op("dve", I("tensor_tensor", out=btmp[:, 0:1], in0=C.ps[:, pb, 128:129], in1=vec[:, V_B1 + x * 2 + jc:V_B1 + x * 2 + jc + 1], op=ALU.add),
                     reads=[bps[pb], C.b_const], writes=[b["btmp"]])
                P.op("act", I("activation", out=hid[:, jc, 0:127], in_=C.ps[:, pb, 0:127], func=AF.Silu, bias=btmp[:, 0:1], scale=1.0),
                     reads=[bps[pb], b["btmp"]], writes=[b["hid"]])
            pb = 4 + g
            if x == 0:
                for jc in range(2):
                    P.op("pe", I("matmul", C.ps[:, pb, 0:127], lhsT=w2k[:, jc, :], rhs=hid[:, jc, 0:127], start=(jc == 0), stop=(jc == 1)),
                         reads=[b["w2"], b["hid"]], writes=[bps[pb]])
                P.op("dve", I("tensor_scalar", out=kcmpT[:, g, 0:127], in0=C.ps[:, pb, 0:127], scalar1=vec[:, V_B2K:V_B2K + 1], scalar2=None, op0=ALU.add),
                     reads=[bps[pb], C.b_const], writes=[b["kcmpT"]])
            else:
                for jc in range(2):
                    P.op("pe", I("matmul", C.ps[0:127, pb, 0:64], lhsT=hid[:, jc, 0:127], rhs=w2v[:, jc, :], start=(jc == 0), stop=(jc == 1)),
                         reads=[b["w2"], b["hid"]], writes=[bps[pb]])
                P.op("dve", I("tensor_tensor", out=vcmp[0:127, g, :], in0=C.ps[0:127, pb, 0:64], in1=b2v[0:127, :], op=ALU.add),
                     reads=[bps[pb], b["b2v"]], writes=[b["vcmp"]])
    P.barrier()

    if stop <= 2:
        return

    def combine(h, pb, j0, j1, gcol, first, qt):
        po = C.ps[:, pb, 0:260].rearrange("p (j d) -> p j d", j=4)
        gsl = gts[:, qt * 4 + j0:qt * 4 + j1 + 1, gcol + h]
        if gcol == 0:
            cf = gsl
            rd = [b["gts"]]
        else:
            P.op("dve", I("reciprocal", out=coef[:, j0:j1 + 1], in_=po[:, j0:j1 + 1, 64]), reads=[bps[pb]], writes=[b["coef"]])
            P.op("dve", I("tensor_tensor", out=coef[:, j0:j1 + 1], in0=coef[:, j0:j1 + 1], in1=gsl, op=ALU.mult),
                 reads=[b["coef"], b["gts"]], writes=[b["coef"]])
            cf = coef[:, j0:j1 + 1]
            rd = [b["coef"]]
        for j in range(j0, j1 + 1):
            dst = oacc[:, j, h * 64:(h + 1) * 64]
            if first:
                P.op("dve", I("tensor_scalar", out=dst, in0=po[:, j, 0:64], scalar1=cf[:, j - j0:j - j0 + 1], scalar2=None, op0=ALU.mult),
                     reads=[bps[pb]] + rd, writes=[b["oacc"]])
            else:
                P.op("dve", I("scalar_tensor_tensor", out=dst, in0=po[:, j, 0:64], scalar=cf[:, j - j0:j - j0 + 1], in1=dst,
                              op0=ALU.mult, op1=ALU.add), reads=[bps[pb], b["oacc"]] + rd, writes=[b["oacc"]])

    b_pnh = [Buf("pn%d" % h) for h in range(8)]
    for qt in range(4):
        q0 = qt * 512
        qs = slice(q0, q0 + 512)
        for pr in range(4):
            pb = 6 + cnt["pe"] % 2
            cnt["pe"] += 1
            for kc in range(8):
                P.op("pe", I("matmul", psb(pb), lhsT=Wq[:, pr, kc, :], rhs=C.xnT[:, kc, qs], start=(kc == 0), stop=(kc == 7)),
                     reads=[b["Wq"], C.b_xn[qt]], writes=[bps[pb]])
            evac(qTt[:, pr, :], psb(pb), [bps[pb]], [b["qTt"]], scale=0.125)
        def cmp_s1(h):
            pr, half, g = h // 2, h % 2, h // 4
            hs = slice(half * 64, (half + 1) * 64)
            sb = h % 2
            P.dma("pool", Ec[h % 2][:, :], bass.AP(d["scrC"].tensor, h * 128 * LC + q0 + 2016, [[LC - 16, 128], [1, 512]]),
                  reads=[C.b_scrC[h]], writes=[b_Ec[h % 2]])
            P.op("pe", I("matmul", C.ps[0:127, sb, :], lhsT=kcmpT[hs, g, 0:127], rhs=qTt[hs, pr, :], start=True, stop=True),
                 reads=[b["kcmpT"], b["qTt"]], writes=[bps[sb]])
            P.op("act", I("activation", out=e32[sb][0:127, :], in_=C.ps[0:127, sb, :], func=AF.Exp), reads=[bps[sb]], writes=[b_e32[sb]])
            P.op("dve", I("tensor_tensor", out=pn[0:127, h, :], in0=e32[sb][0:127, :], in1=Ec[h % 2][0:127, :], op=ALU.mult),
                 reads=[b_e32[sb], b_Ec[h % 2]], writes=[b_pnh[h]])

        def cmp_s2(h):
            P.op("pe", I("matmul", psb(5), lhsT=C.ones_bf[0:127, :], rhs=pn[0:127, h, :], start=True, stop=True),
                 reads=[b_pnh[h]], writes=[bps[5]])
            P.op("act", I("activation", out=rZ, in_=psb(5), func=AF.Ln, bias=C.tiny[:, 0:1], scale=1.0), reads=[bps[5]], writes=[b["rZ"]])
            P.op("act", I("activation", out=rZ, in_=rZ, func=AF.Exp, scale=-1.0), reads=[b["rZ"]], writes=[b["rZ"]])
            P.op("dve", I("tensor_tensor", out=pn[:, h, :], in0=pn[:, h, :], in1=rZ, op=ALU.mult),
                 reads=[b_pnh[h], b["rZ"]], writes=[b_pnh[h]])

        def cmp_s3(h):
            g = h // 4
            pb = 2 + h % 2
            for s4 in range(4):
                P.op("pe", I("matmul", C.ps[:, pb, s4 * 65:s4 * 65 + 64], lhsT=pn[0:127, h, s4 * 128:(s4 + 1) * 128], rhs=vcmp[0:127, g, :],
                             start=True, stop=True), reads=[b_pnh[h], b["vcmp"]], writes=[bps[pb]])
            combine(h, pb, 0, 3, 0, True, qt)

        cmp_s1(0)
        cmp_s1(1)
        cmp_s2(0)
        for h in range(8):
            if h + 2 < 8:
                cmp_s1(h + 2)
            if h + 1 < 8:
                cmp_s2(h + 1)
            cmp_s3(h)
        for s4 in range(4):
            for g in range(2):
                for hh in range(4):
                    h = g * 4 + hh
                    P.op("pe", I("matmul", C.ps[:, 4, (s4 * 2 + g) * 32:(s4 * 2 + g + 1) * 32], lhsT=pn[0:127, h, s4 * 128:(s4 + 1) * 128],
                                 rhs=ovl[0:127, :], start=(hh == 0), stop=(hh == 3)), reads=[b_pnh[h], b["cst"]], writes=[bps[4]])

        def attend_all(kT, va, sel, LOOK=2):
            tiles = []
            for h in range(8):
                kts = list(range(0, 4 * qt + 4)) if sel else list(range(max(0, 4 * qt - 4), 4 * qt + 4))
                for kt in kts:
                    tiles.append((h, kt, kts))
            st = {}

            def emit_qk(idx):
                h, kt, kts = tiles[idx]
                pr, half, g = h // 2, h % 2, h // 4
                hs = slice(half * 64, (half + 1) * 64)
                k0 = kt * 128
                dl = q0 - k0
                jlo = max(0, -(dl // 128))
                jhi = 3 if sel else min(3, (512 - dl) // 128)
                cols = slice(jlo * 128, (jhi + 1) * 128)
                sb = idx % 2
                r = idx % 3
                P.op("pe", I("matmul", C.ps[:, sb, cols], lhsT=kT[hs, g, k0:k0 + 128], rhs=qTt[hs, pr, cols], start=True, stop=(not sel)),
                     reads=[b["ksT" if sel else "kwT"], b["qTt"]], writes=[bps[sb]])
                if sel:
                    P.op("pe", I("matmul", C.ps[:, sb, cols], lhsT=Ind[0:32, k0:k0 + 128], rhs=negmT[0:32, g, cols], start=False, stop=True),
                         reads=[b["cst"], b["negmT"]], writes=[bps[sb]])
                P.op("act", I("activation", out=pexp[r][:, cols], in_=C.ps[:, sb, cols], func=AF.Exp, bias=C.tab31[:, h:h + 1], scale=1.0),
                     reads=[bps[sb], C.b_const], writes=[b_pexp[r]])
                for j in range(jlo, jhi + 1):
                    dp = dl + 128 * j
                    tb = {0: 0, 128: 128, 512: 256}.get(dp) if (not sel or dp < 256) else None
                    if tb is not None:
                        P.op("dve", I("tensor_tensor", out=pexp[r][:, j * 128:(j + 1) * 128], in0=pexp[r][:, j * 128:(j + 1) * 128],
                                      in1=C.Dtab[:, h, tb:tb + 128], op=ALU.mult), reads=[b_pexp[r], C.b_Dtab[h]], writes=[b_pexp[r]])
                st[idx] = (r, jlo, jhi)

            def emit_pv(idx):
                h, kt, kts = tiles[idx]
                g = h // 4
                pb = 2 + h % 2
                r, jlo, jhi = st.pop(idx)
                for j in range(jlo, jhi + 1):
                    P.op("pe", I("matmul", C.ps[:, pb, j * 65:(j + 1) * 65], lhsT=pexp[r][:, j * 128:(j + 1) * 128], rhs=vtok[:, kt, va + g, :],
                                 start=(kt == kts[0] and j == jlo), stop=(kt == kts[-1] and j == jhi)),
                         reads=[b_pexp[r], b["vtok"]], writes=[bps[pb]])
                if kt == kts[-1]:
                    combine(h, pb, 0, 3, 8 if sel else 16, False, qt)

            n = len(tiles)
            for idx in range(min(LOOK, n)):
                emit_qk(idx)
            for idx in range(n):
                if idx + LOOK < n:
                    emit_qk(idx + LOOK)
                emit_pv(idx)

        attend_all(kwT, 2, False)
        if stop <= 4:
            break
        P.op("dve", I("tensor_tensor", out=sc.rearrange("p (s g) n -> p s g n", g=2), in0=C.ps[:, 4, 0:256].rearrange("p (s g n) -> p s g n", s=4, g=2),
                      in1=valid[:, qt * 4:(qt + 1) * 4, :].unsqueeze(2).to_broadcast([128, 4, 2, 32]), op=ALU.mult),
             reads=[bps[4], b["cst"]], writes=[b["sc"]])
        P.op("dve", I("tensor_tensor", out=sc.rearrange("p (s g) n -> p s g n", g=2), in0=sc.rearrange("p (s g) n -> p s g n", g=2),
                      in1=addm[:, qt * 4:(qt + 1) * 4, :].unsqueeze(2).to_broadcast([128, 4, 2, 32]), op=ALU.add),
             reads=[b["sc"], b["cst"]], writes=[b["sc"]])
        for sg in range(8):
            P.op("dve", I("max", out=mx[:, sg, :], in_=sc[:, sg, :]), reads=[b["sc"]], writes=[b["mx"]])
        for sg in range(8):
            P.op("dve", I("tensor_scalar", out=negm[:, sg, :], in0=sc[:, sg, :], scalar1=mx[:, sg, 7:8], scalar2=NEG, op0=ALU.is_lt, op1=ALU.mult),
                 reads=[b["sc"], b["mx"]], writes=[b["negm"]])
        for s4 in range(4):
            for g in range(2):
                P.op("pe", I("transpose", psbf(5)[0:32, g * 512 + s4 * 128:g * 512 + (s4 + 1) * 128], negm[:, s4 * 2 + g, :], C.ident_bf),
                     reads=[b["negm"]], writes=[bps[5]])
        P.op("act", I("activation", out=negmT.rearrange("p g t -> p (g t)")[0:32, :], in_=psbf(5)[0:32, :], func=AF.Copy),
             reads=[bps[5]], writes=[b["negmT"]])
        if stop <= 5:
            break
        attend_all(ksT, 0, True)
        for j in range(4):
            P.op("act", I("activation", out=on[:, j, :], in_=oacc[:, j, :], func=AF.Square, accum_out=ssq[:, j:j + 1]),
                 reads=[b["oacc"]], writes=[b["on"], b["ssq"]])
        P.op("act", I("activation", out=ssq[:, 0:4], in_=ssq[:, 0:4], func=AF.Ln, bias=C.eps512[:, 0:1], scale=1.0), reads=[b["ssq"]], writes=[b["ssq"]])
        P.op("act", I("activation", out=ssq[:, 0:4], in_=ssq[:, 0:4], func=AF.Exp, scale=-0.5), reads=[b["ssq"]], writes=[b["ssq"]])
        for j in range(4):
            P.op("dve", I("tensor_scalar", out=on[:, j, :], in0=oacc[:, j, :], scalar1=ssq[:, j:j + 1], scalar2=None, op0=ALU.mult),
                 reads=[b["oacc"], b["ssq"]], writes=[b["on"]])
            pb = 6 + j % 2
            for kc in range(4):
                P.op("pe", I("transpose", psbf(pb)[:, kc * 128:(kc + 1) * 128], on[:, j, kc * 128:(kc + 1) * 128], C.ident_bf),
                     reads=[b["on"]], writes=[bps[pb]])
            P.op("dve", I("tensor_tensor", out=mixT[:, :, j * 128:(j + 1) * 128], in0=psbf(pb)[:, 0:512].rearrange("p (k t) -> p k t", k=4),
                          in1=vec[:, V_NG:V_NG + 4].unsqueeze(2).to_broadcast([128, 4, 128]), op=ALU.mult),
                 reads=[bps[pb], C.b_const], writes=[b["mixT"]])
        for m in range(8):
            pb = 6 + m % 2
            for kc in range(4):
                P.op("pe", I("matmul", psb(pb), lhsT=Woa[:, m, kc, :], rhs=mixT[:, kc, :], start=(kc == 0), stop=(kc == 3)),
                     reads=[b["Woa"], b["mixT"]], writes=[bps[pb]])
            P.op("dve", I("tensor_tensor", out=C.xT[:, m, qs], in0=psb(pb), in1=C.xT[:, m, qs], op=ALU.add),
                 reads=[bps[pb], C.b_xT[qt]], writes=[C.b_xT[qt]])
    P.barrier()


def ple_stage(P, C, d, l, gidx):
    rmsnorm_T(P, C, gidx)
    A = Alloc(C)
    pT = A.b(2048).rearrange("p (k t) -> p k t", k=2)
    Wp = A.b(1024).rearrange("p (m k n) -> p m k n", m=8, k=2)
    wring = [A.b(512).rearrange("p (k m) -> p k m", k=8) for _ in range(3)]
    sg = [A.f(512) for _ in range(2)]
    b_pT, b_Wp = Buf("pT"), Buf("Wp")
    b_w = [Buf("w%d" % i) for i in range(3)]
    b_sg = [Buf("sg%d" % i) for i in range(2)]
    bps = C.b_ps
    for k in range(2):
        P.dma("pool", pT[:, k, :], d["pT"][l][:, k, :], writes=[b_pT])
    P.dma("pool", Wp.rearrange("p m k n -> p (m k n)"), d["wp"][l], writes=[b_Wp])
    step = 0
    for m in range(8):
        s = m % 3
        P.dma("pool", wring[s].rearrange("p k m -> p (k m)"), d["wg"][l, m], writes=[b_w[s]])
        for tt in range(4):
            ts = slice(tt * 512, (tt + 1) * 512)
            pg, pp = step % 2, 2 + step % 2
            k = step % 2
            step += 1
            for kc in range(8):
                P.op("pe", I("matmul", C.ps[:, pg, :], lhsT=wring[s][:, kc, :], rhs=C.xnT[:, kc, ts], start=(kc == 0), stop=(kc == 7)),
                     reads=[b_w[s], C.b_xn[tt]], writes=[bps[pg]])
            for kc in range(2):
                P.op("pe", I("matmul", C.ps[:, pp, :], lhsT=Wp[:, m, kc, :], rhs=pT[:, kc, ts], start=(kc == 0), stop=(kc == 1)),
                     reads=[b_Wp, b_pT], writes=[bps[pp]])
            P.op("act", I("activation", out=sg[k], in_=C.ps[:, pg, :], func=AF.Sigmoid), reads=[bps[pg]], writes=[b_sg[k]])
            P.op("dve", I("tensor_tensor", out=sg[k], in0=sg[k], in1=C.ps[:, pp, :], op=ALU.mult), reads=[b_sg[k], bps[pp]], writes=[b_sg[k]])
            P.op("dve", I("tensor_tensor", out=C.xT[:, m, ts], in0=sg[k], in1=C.xT[:, m, ts], op=ALU.add),
                 reads=[b_sg[k], C.b_xT[tt]], writes=[C.b_xT[tt]])
    P.barrier()


def final_norm_store(P, C, gidx, outT_d):
    ph = OFF_PH + 18432
    sq = bview(C, ph, 2048).rearrange("p (c t) -> p c t", c=8)
    rstd = fview(C, ph + 2048, 2048)
    for tt in range(4):
        ts = slice(tt * 512, (tt + 1) * 512)
        P.op("act", I("activation", out=sq, in_=C.xT[:, :, ts], func=AF.Square),
             reads=[C.b_xT[tt]], writes=[C.b_sq])
        for c in range(8):
            P.op("pe", I("matmul", C.ps[:, tt, :], lhsT=C.ones_bf, rhs=sq[:, c, :],
                                                      start=(c == 0), stop=(c == 7)),
                 reads=[C.b_sq], writes=[C.b_ps[tt]])
    psall = C.ps[:, 0:4, :].rearrange("p a t -> p (a t)")
    P.op("act", I("activation", out=rstd, in_=psall, func=AF.Ln, bias=C.eps1024[:, 0:1], scale=1.0),
         reads=C.b_ps[0:4], writes=[C.b_rstd])
    P.op("act", I("activation", out=rstd, in_=rstd, func=AF.Exp, scale=-0.5),
         reads=[C.b_rstd], writes=[C.b_rstd])
    for tt in range(4):
        ts = slice(tt * 512, (tt + 1) * 512)
        for c in range(8):
            P.op("dve", I("scalar_tensor_tensor",
                out=C.xT[:, c, ts], in0=C.xT[:, c, ts], scalar=C.gains[:, gidx + c:gidx + c + 1],
                in1=rstd[:, ts], op0=ALU.mult, op1=ALU.mult),
                reads=[C.b_xT[tt], C.b_rstd], writes=[C.b_xT[tt]])
        P.dma("sp", outT_d[:, :, ts], C.xT[:, :, ts], reads=[C.b_xT[tt]])


NG_PER_LAYER = 4 * 8


def build_nc(n_layers=DEPTH, stages=("ffn1", "mix", "ffn2", "ple"), final=True, dbg=False, nsa_stop=99):
    nc = bass.Bass("TRN2", target_bir_lowering=False)
    P = Prog(nc)
    C = Ctx()
    C.nsa_stop = nsa_stop
    C.nc = nc
    C.dbg = dbg
    L = DEPTH
    d = {}
    d["xT"] = nc.dram_tensor("xT", [128, 8, SEQ], F32, kind="ExternalInput").ap()
    d["gains"] = nc.dram_tensor("gains", [128, L * NG_PER_LAYER + 8], F32, kind="ExternalInput").ap()
    d["f1_win"] = nc.dram_tensor("f1_win", [L, NJ, 128, 2048], F32, kind="ExternalInput").ap()
    d["f1_wout"] = nc.dram_tensor("f1_wout", [L, 8, 128, D_FF], F32, kind="ExternalInput").ap()
    d["f2_win"] = nc.dram_tensor("f2_win", [L, NJ, 128, 2048], F32, kind="ExternalInput").ap()
    d["f2_wout"] = nc.dram_tensor("f2_wout", [L, 8, 128, D_FF], F32, kind="ExternalInput").ap()
    d["wfm"] = nc.dram_tensor("wfm", [L, NFM, 128, 1024], F32, kind="ExternalInput").ap()
    d["wdt"] = nc.dram_tensor("wdt", [L, 128, 128], F32, kind="ExternalInput").ap()
    d["wmo"] = nc.dram_tensor("wmo", [L, 8, 128, 1536], F32, kind="ExternalInput").ap()
    d["vecs"] = nc.dram_tensor("vecs", [128, L, NVEC], F32, kind="ExternalInput").ap()
    d["rows"] = nc.dram_tensor("rows", [1, L * 48], F32, kind="ExternalInput").ap()
    d["cst"] = nc.dram_tensor("cst", [128, 512], F32, kind="ExternalInput").ap()
    d["rel_table"] = nc.dram_tensor("rel_table", [32, 8], F32, kind="ExternalInput").ap()
    d["oht"] = nc.dram_tensor("oht", [33, 383 + 255 + LC], F32, kind="ExternalInput").ap()
    d["ind"] = nc.dram_tensor("ind", [32, SEQ], F32, kind="ExternalInput").ap()
    d["ovl"] = nc.dram_tensor("ovl", [127, 32], F32, kind="ExternalInput").ap()
    d["valid"] = nc.dram_tensor("valid", [128, 512], F32, kind="ExternalInput").ap()
    d["addm"] = nc.dram_tensor("addm", [128, 512], F32, kind="ExternalInput").ap()
    d["b2v"] = nc.dram_tensor("b2v", [L, 64], F32, kind="ExternalInput").ap()
    d["wv"] = nc.dram_tensor("wv", [L, 128, 2240], F32, kind="ExternalInput").ap()
    d["cw1"] = nc.dram_tensor("cw1", [L, 2, 64, 8192], F32, kind="ExternalInput").ap()
    d["cpos"] = nc.dram_tensor("cpos", [L, 2, 64, 32], F32, kind="ExternalInput").ap()
    d["cw2k"] = nc.dram_tensor("cw2k", [L, 128, 256], F32, kind="ExternalInput").ap()
    d["cw2v"] = nc.dram_tensor("cw2v", [L, 128, 128], F32, kind="ExternalInput").ap()
    d["pT"] = nc.dram_tensor("pT", [L, 128, 2, SEQ], F32, kind="ExternalInput").ap()
    d["wg"] = nc.dram_tensor("wg", [L, 8, 128, 1024], F32, kind="ExternalInput").ap()
    d["wp"] = nc.dram_tensor("wp", [L, 128, 2048], F32, kind="ExternalInput").ap()
    d["scrA"] = nc.dram_tensor("scrA", [8, 128 * 383], F32).ap()
    d["scrB"] = nc.dram_tensor("scrB", [8, 128 * 255], F32).ap()
    d["scrC"] = nc.dram_tensor("scrC", [8, 128 * LC], F32).ap()
    outT = nc.dram_tensor("outT", [128, 8, SEQ], F32, kind="ExternalOutput").ap()

    with ExitStack() as st:
        C.arena = st.enter_context(nc.sbuf_tensor("arena", [128, ARENA_WORDS], F32))
        C.ps = st.enter_context(nc.psum_tensor("ps", [128, 8, 512], F32))
        C.xT = C.arena[:, OFF_XT:OFF_XT + 16384].rearrange("p (c t) -> p c t", c=8)
        C.xnT = bview(C, OFF_XN, 8192).rearrange("p (c t) -> p c t", c=8)
        ngc = L * NG_PER_LAYER + 8
        C.gains = fview(C, OFF_CONST, ngc)
        C.eps1024 = fview(C, OFF_CONST + ngc, 1)
        C.eps512 = fview(C, OFF_CONST + ngc + 1, 1)
        C.one_c = fview(C, OFF_CONST + ngc + 2, 1)
        C.ones_bf = bview(C, OFF_CONST + ngc + 8, 64)
        co = Alloc(C, OFF_CONST + ngc + 8 + 64)
        C.ident_bf = co.b(64)
        C.U_f = co.f(128)
        C.SL_f = co.f(128)
        C.ones_f = co.f(128)
        C.vecs = co.f(L * NVEC).rearrange("p (l v) -> p l v", l=L)
        C.rows = co.f(L * 48).rearrange("p (l v) -> p l v", l=L)
        C.tab31 = co.f(8)
        C.ntab31 = co.f(8)
        C.tiny = co.f(1)
        C.Dtab = co.b(1536).rearrange("p (h m) -> p h m", h=8)
        assert co.o <= OFF_PH, co.o
        C.b_xT = [Buf("xT%d" % i) for i in range(4)]
        C.b_xn = [Buf("xn%d" % i) for i in range(4)]
        C.b_ps = [Buf("ps%d" % i) for i in range(8)]
        C.b_sq = Buf("sq")
        C.b_rstd = Buf("rstd")
        b_const = Buf("const")
        C.b_const = b_const
        C.b_Dtab = [Buf("Dtab%d" % h) for h in range(8)]

        for tt in range(4):
            ts = slice(tt * 512, (tt + 1) * 512)
            P.dma("sp", C.xT[:, :, ts], d["xT"][:, :, ts], writes=[C.b_xT[tt]])
        P.dma("sp", C.gains, d["gains"], writes=[b_const])
        P.op("dve", I("tensor_scalar", out=C.gains, in0=C.gains, scalar1=32.0, scalar2=None, op0=ALU.mult),
             reads=[b_const], writes=[b_const])
        P.op("dve", I("memset", C.eps1024, 1024.0 * EPS), writes=[b_const])
        P.op("dve", I("memset", C.ones_bf, 1.0), writes=[b_const])
        P.op("dve", I("memset", C.eps512, 512.0 * EPS), writes=[b_const])
        P.op("dve", I("memset", C.one_c, 1.0), writes=[b_const])
        P.op("dve", I("memset", C.tiny, 1e-30), writes=[b_const])
        P.dma("sp", C.arena[:, OFF_CONST + ngc + 8 + 64 + 64:OFF_CONST + ngc + 8 + 64 + 64 + 384], d["cst"][:, 0:384], writes=[b_const])
        tmp_id = fview(C, OFF_PH, 128)
        P.dma("sp", tmp_id, d["cst"][:, 384:512], writes=[b_const])
        P.op("dve", I("tensor_copy", out=C.ident_bf, in_=tmp_id), reads=[b_const], writes=[b_const])
        P.dma("sp", C.vecs.rearrange("p l v -> p (l v)"), d["vecs"].rearrange("p l v -> p (l v)"), writes=[b_const])
        P.dma("sp", C.rows.rearrange("p l v -> p (l v)"), d["rows"].partition_broadcast(128), writes=[b_const])
        s512 = math.sqrt(512.0)
        P.op("dve", I("tensor_scalar", out=C.vecs[:, :, V_SG:V_SG + 12], in0=C.vecs[:, :, V_SG:V_SG + 12], scalar1=s512,
                                              scalar2=None, op0=ALU.mult), reads=[b_const], writes=[b_const])
        P.barrier()
        if "mix" in stages or "nsa" in stages or "nsatab" in stages:
            nsa_tables(P, C, d)

        for l in range(n_layers):
            g0 = l * NG_PER_LAYER
            if "ffn1" in stages:
                ffn_stage(P, C, d["f1_win"], d["f1_wout"], l, g0 + 0)
            if "mix" in stages or "ssd" in stages or "nsa" in stages:
                rmsnorm_T(P, C, g0 + 8)
                P.barrier()
            if "mix" in stages or "nsa" in stages:
                nsa_phase(P, C, d, l, stop=C.nsa_stop)
            if "mix" in stages or "ssd" in stages:
                ssd_phase(P, C, d, l)
            if "ffn2" in stages:
                ffn_stage(P, C, d["f2_win"], d["f2_wout"], l, g0 + 16)
            if "ple" in stages:
                ple_stage(P, C, d, l, g0 + 24)
        if final:
            final_norm_store(P, C, L * NG_PER_LAYER, outT)
        else:
            for tt in range(4):
                ts = slice(tt * 512, (tt + 1) * 512)
                P.dma("sp", outT[:, :, ts], C.xT[:, :, ts], reads=[C.b_xT[tt]])
        P.emit()
    return nc


def _fm(v):
    return np.ascontiguousarray(v.reshape(-1, 128).T)


def prep_shared(inp):
    L = DEPTH
    sh = {}
    g = np.zeros((128, L * NG_PER_LAYER + 8), np.float32)
    for l in range(L):
        for k, nm in enumerate(("ffn1_norm", "mix_norm", "ffn2_norm", "ple_norm")):
            g[:, l * NG_PER_LAYER + k * 8:l * NG_PER_LAYER + k * 8 + 8] = _fm(np.asarray(inp[nm][l]))
    g[:, L * NG_PER_LAYER:] = _fm(np.asarray(inp["final_norm"]))
    sh["gains"] = g
    for pre, a, b in (("f1", "ffn1_w_in", "ffn1_w_out"), ("f2", "ffn2_w_in", "ffn2_w_out")):
        wi = np.asarray(inp[a])
        wi = wi.reshape(L, 8, 128, 2, NJ, 128)
        sh[pre + "_win"] = np.ascontiguousarray(wi.transpose(0, 4, 2, 3, 1, 5)).reshape(L, NJ, 128, 2048)
        wo = np.asarray(inp[b])
        wo = wo.reshape(L, NJ, 128, 8, 128)
        sh[pre + "_wout"] = np.ascontiguousarray(wo.transpose(0, 3, 2, 1, 4)).reshape(L, 8, 128, D_FF)
    W = np.asarray(inp["w_mix_in"])
    cols = []
    for pr in range(4):
        cols.append(np.arange(pr * 128, (pr + 1) * 128))
    for base in (768, 1024):
        for g in range(2):
            c = np.arange(base + g * 64, base + (g + 1) * 64)
            cols.append(np.concatenate([c, c]))
    cols.append(np.arange(512, 640))
    cols.append(np.arange(640, 768))
    for c in range(8):
        cols.append(np.arange(1304 + c * 128, 1304 + (c + 1) * 128))
    for c in range(12):
        cols.append(np.arange(2328 + c * 128, 2328 + (c + 1) * 128))
    cols = np.stack(cols, 0)
    Wk = W.reshape(L, 8, 128, 3880)
    wfm = Wk[:, :, :, cols]
    sh["wfm"] = np.ascontiguousarray(wfm.transpose(0, 3, 2, 1, 4)).reshape(L, NFM, 128, 1024)
    sh["wdt"] = np.ascontiguousarray(Wk[:, :, :, 3864:3880].transpose(0, 2, 1, 3)).reshape(L, 128, 128)
    wo = np.asarray(inp["w_mix_out"]).reshape(L, 12, 128, 8, 128)
    sh["wmo"] = np.ascontiguousarray(wo.transpose(0, 3, 2, 1, 4)).reshape(L, 8, 128, 1536)
    vecs = np.zeros((128, L, NVEC), np.float32)
    for l in range(L):
        cw = np.asarray(inp["conv_w"][l])
        vecs[:, l, V_CW:V_CW + 48] = cw.reshape(4, 12, 128).transpose(2, 1, 0).reshape(128, 48)
        vecs[:, l, V_CB:V_CB + 12] = np.asarray(inp["conv_b"][l]).reshape(12, 128).T
        vecs[:, l, V_SG:V_SG + 8] = np.asarray(inp["ssm_out_norm"][l]).reshape(8, 128).T
        vecs[:, l, V_NG:V_NG + 4] = np.asarray(inp["nsa_out_norm"][l]).reshape(4, 128).T
        vecs[:, l, V_B1:V_B1 + 4] = np.asarray(inp["cmp_b1"][l]).reshape(4, 128).T
    sh["vecs"] = vecs
    rows = np.zeros((1, L * 48), np.float32)
    for l in range(L):
        rows[0, l * 48:l * 48 + 16] = np.asarray(inp["dt_bias"][l])
        rows[0, l * 48 + 16:l * 48 + 32] = np.asarray(inp["a_log"][l])
        rows[0, l * 48 + 32:l * 48 + 48] = np.asarray(inp["d_skip"][l])
    sh["rows"] = rows
    ii = np.arange(128)
    cst = np.zeros((128, 512), np.float32)
    cst[:, 0:128] = (ii[:, None] <= ii[None, :])
    cst[:, 128:256] = (ii[:, None] > ii[None, :])
    cst[:, 256:384] = 1.0
    cst[:, 384:512] = np.eye(128)
    sh["cst"] = cst
    sh["rel_table"] = np.ascontiguousarray(np.asarray(inp["rel_table"], np.float32))
    def onehot(dvals, validm):
        o = np.zeros((33, len(dvals)), np.float32)
        bk = t5_bucket_np(dvals)
        for n in range(len(dvals)):
            if validm[n]:
                o[bk[n], n] = 1.0
            else:
                o[32, n] = 1.0
        return o
    dA = np.arange(383) - 127
    dB = np.arange(255) + 385
    dC = np.arange(LC) - 2047
    sh["oht"] = np.concatenate([onehot(dA, dA >= 0), onehot(dB, dB < 512), onehot(dC, dC >= 0)], axis=1)
    kk = np.arange(SEQ)
    sh["ind"] = (kk[None, :] // 64 == np.arange(32)[:, None]).astype(np.float32)
    cc = np.arange(127)
    nn = np.arange(32)
    sh["ovl"] = ((16 * cc[:, None] <= 64 * nn[None, :] + 63) & (16 * cc[:, None] + 31 >= 64 * nn[None, :])).astype(np.float32)
    t = (np.arange(16)[None, :] * 128 + np.arange(128)[:, None])
    cur = (t // 64)[:, :, None]
    blk = nn[None, None, :]
    vld = (blk <= cur)
    forced = ((blk == 0) | (blk == cur) | (blk == cur - 1))
    sh["valid"] = vld.astype(np.float32).reshape(128, 512)
    sh["addm"] = np.where(vld, np.where(forced, 1e4, 0.0), -1e4).astype(np.float32).reshape(128, 512)
    sh["b2v"] = np.ascontiguousarray(np.asarray(inp["cmp_b2"])[:, 1, :])
    tcols = np.concatenate([np.arange(896, 1024), np.arange(1152, 1280), np.arange(1280, 1304)])
    sh["wv"] = np.ascontiguousarray(Wk[:, :, :, tcols].transpose(0, 2, 1, 3)).reshape(L, 128, 2240)
    w1 = np.asarray(inp["cmp_w1"]).reshape(L, 2, 32, 64, 256)
    sh["cw1"] = np.ascontiguousarray(w1.transpose(0, 1, 3, 2, 4)).reshape(L, 2, 64, 8192)
    sh["cpos"] = np.ascontiguousarray(np.asarray(inp["cmp_pos"]).transpose(0, 1, 3, 2))
    w2 = np.asarray(inp["cmp_w2"]).reshape(L, 2, 2, 128, 64)
    w2k = w2[:, 0].transpose(0, 2, 1, 3)
    sh["cw2k"] = np.ascontiguousarray(np.concatenate([w2k, w2k], axis=-1)).reshape(L, 128, 256)
    sh["cw2v"] = np.ascontiguousarray(w2[:, 1].transpose(0, 2, 1, 3)).reshape(L, 128, 128)
    b2k = np.asarray(inp["cmp_b2"])[:, 0, :]
    for l in range(L):
        sh["vecs"][:, l, V_B2K] = np.concatenate([b2k[l], b2k[l]])
    wg = np.asarray(inp["ple_gate_w"]).reshape(L, 8, 128, 8, 128)
    sh["wg"] = np.ascontiguousarray(wg.transpose(0, 3, 2, 1, 4)).reshape(L, 8, 128, 1024)
    wp = np.asarray(inp["ple_proj_w"]).reshape(L, 2, 128, 8, 128)
    sh["wp"] = np.ascontiguousarray(wp.transpose(0, 2, 3, 1, 4)).reshape(L, 128, 2048)
    return sh


def prep_core(inp, b):
    x = np.asarray(inp["x"][b])
    xT = np.ascontiguousarray(x.T.reshape(8, 128, SEQ).transpose(1, 0, 2))
    p = np.asarray(inp["p"][:, b])
    pT = np.ascontiguousarray(p.transpose(0, 2, 1).reshape(DEPTH, 2, 128, SEQ).transpose(0, 2, 1, 3))
    return {"xT": xT, "pT": pT}


_NC_CACHE = {}


def kernel(**inputs):
    key = "full"
    if key not in _NC_CACHE:
        _NC_CACHE[key] = build_nc()
    nc = _NC_CACHE[key]
    sh = prep_shared(inputs)
    in_maps = []
    for b in range(8):
        m = dict(sh)
        m.update(prep_core(inputs, b))
        in_maps.append(m)
    res = run_bass_kernel_spmd(nc, in_maps, core_ids=list(range(8)))
    outs = []
    for b in range(8):
        oT = np.asarray(res.results[b]["outT"])
        outs.append(oT.transpose(2, 1, 0).reshape(SEQ, D_MODEL))
    return np.stack(outs, 0).astype(np.float32)
```
